# Optimizing a Trainium2 kernel written in Bass

```python
import math
import jax, jax.numpy as jnp
from jax import lax
import numpy as np

D_MODEL = 1024
BATCH = 4
SEQ = 8192
DEPTH = 2

N_META = 16
EPS = 1e-6
LRU_WIDTH = D_MODEL
LRU_BLOCKS = 4
LRU_BLOCK = LRU_WIDTH // LRU_BLOCKS
CONV_WIDTH = 4
LRU_C = 8.0
SSM_WIDTH = D_MODEL // 2
SSM_GROUP = 16
SSM_GROUPS = SSM_WIDTH // SSM_GROUP
SSM_STATE = 64
EVEN_IN = 2 * LRU_WIDTH + SSM_WIDTH
EVEN_MIX = LRU_WIDTH + SSM_WIDTH
D_FF = 3 * D_MODEL
GLA_HEADS = 4
GLA_DK = D_MODEL // 2 // GLA_HEADS
GLA_DV = D_MODEL // GLA_HEADS
GLA_KEY = GLA_HEADS * GLA_DK
GLA_VAL = GLA_HEADS * GLA_DV
GLA_RANK = 16
GLA_TAU = 16.0
GLA_CHUNK = 64
ODD_IN = 2 * GLA_KEY + 2 * GLA_VAL + GLA_RANK
N_EXPERTS = 8
TOP_K = 2
D_FF_EXPERT = 7 * D_MODEL // 2
N_EVEN = (DEPTH + 1) // 2
N_ODD = DEPTH // 2

kernel_name = "hybrid_rglru_s5_gla_moe_meta"

F32 = jnp.float32


def rms_norm(x, g):
    xf = x.astype(F32)
    y = xf * lax.rsqrt(jnp.mean(xf * xf, axis=-1, keepdims=True) + EPS)
    return (y * g.astype(F32)).astype(x.dtype)


def swiglu(h, w_gate, w_up, w_down):
    return (jax.nn.silu(h @ w_gate) * (h @ w_up)) @ w_down


def causal_conv(x, w, b):
    K = w.shape[0]
    L = x.shape[1]
    xp = jnp.pad(x, ((0, 0), (K - 1, 0), (0, 0)))
    return b + sum(w[k] * xp[:, k:k + L] for k in range(K))


def _real_combine(e1, e2):
    a1, b1 = e1
    a2, b2 = e2
    return a1 * a2, a2 * b1 + b2


def _complex_combine(e1, e2):
    ar1, ai1, br1, bi1 = e1
    ar2, ai2, br2, bi2 = e2
    return (ar1 * ar2 - ai1 * ai2,
            ar1 * ai2 + ai1 * ar2,
            ar2 * br1 - ai2 * bi1 + br2,
            ar2 * bi1 + ai2 * br1 + bi2)


def rg_lru(x, gate_a_w, gate_a_b, gate_x_w, gate_x_b, lam):
    B_, L, _ = x.shape
    xb = x.reshape(B_, L, LRU_BLOCKS, LRU_BLOCK)
    r = jax.nn.sigmoid(jnp.einsum('blhi,hij->blhj', xb, gate_a_w).reshape(B_, L, LRU_WIDTH) + gate_a_b)
    i = jax.nn.sigmoid(jnp.einsum('blhi,hij->blhj', xb, gate_x_w).reshape(B_, L, LRU_WIDTH) + gate_x_b)
    log_a = -LRU_C * r.astype(F32) * jax.nn.softplus(-lam.astype(F32))
    a = jnp.exp(log_a)
    b = jnp.sqrt(-jnp.expm1(2.0 * log_a)) * (i * x).astype(F32)
    _, h = lax.associative_scan(_real_combine, (a, b), axis=1)
    return h.astype(x.dtype)


def s5_ssm(u, lam_re, lam_im, log_dt, b_re, b_im, c_re, c_im, d, glu_w, glu_b):
    B_, L, _ = u.shape
    lr, li = lam_re.astype(F32), lam_im.astype(F32)
    dt = jnp.exp(log_dt.astype(F32))[:, None]
    mag = jnp.exp(dt * lr)
    ar, ai = mag * jnp.cos(dt * li), mag * jnp.sin(dt * li)
    den = lr * lr + li * li
    nr = ar - 1.0
    zr = (nr * lr + ai * li) / den
    zi = (ai * lr - nr * li) / den
    br_ = zr[..., None] * b_re.astype(F32) - zi[..., None] * b_im.astype(F32)
    bi_ = zr[..., None] * b_im.astype(F32) + zi[..., None] * b_re.astype(F32)
    ug = u.reshape(B_, L, SSM_GROUPS, SSM_GROUP).astype(F32)
    xr = jnp.einsum('blgh,gnh->blgn', ug, br_)
    xi = jnp.einsum('blgh,gnh->blgn', ug, bi_)
    a_shape = (1, L, SSM_GROUPS, SSM_STATE)
    ar_t = jnp.broadcast_to(ar[None, None], a_shape)
    ai_t = jnp.broadcast_to(ai[None, None], a_shape)
    _, _, hr, hi = lax.associative_scan(_complex_combine, (ar_t, ai_t, xr, xi), axis=1)
    y = (jnp.einsum('blgn,ghn->blgh', hr, c_re.astype(F32))
         - jnp.einsum('blgn,ghn->blgh', hi, c_im.astype(F32)))
    y = y.reshape(B_, L, SSM_WIDTH) + d.astype(F32) * u.astype(F32)
    g = jax.nn.gelu(y).astype(u.dtype)
    return g * jax.nn.sigmoid(g @ glu_w + glu_b)


def even_mixer(h, w_in, conv_w, conv_b, gate_a_w, gate_a_b, gate_x_w, gate_x_b, lru_lambda,
               lam_re, lam_im, log_dt, b_re, b_im, c_re, c_im, d, glu_w, glu_b, w_out):
    z = h @ w_in
    gate_branch = z[..., :LRU_WIDTH]
    rec_branch = z[..., LRU_WIDTH:2 * LRU_WIDTH]
    ssm_in = z[..., 2 * LRU_WIDTH:]
    rec = rg_lru(causal_conv(rec_branch, conv_w, conv_b), gate_a_w, gate_a_b, gate_x_w, gate_x_b, lru_lambda)
    a_out = jax.nn.gelu(gate_branch) * rec
    b_out = s5_ssm(ssm_in, lam_re, lam_im, log_dt, b_re, b_im, c_re, c_im, d, glu_w, glu_b)
    return (jnp.concatenate([a_out, b_out.astype(h.dtype)], axis=-1) @ w_out).astype(h.dtype)


def gla_chunk(S, q, k, v, lg):
    size = q.shape[2]
    b = jnp.cumsum(lg, axis=2)
    causal = jnp.tril(jnp.ones((size, size), dtype=bool))[:, :, None]
    diff = b[:, :, :, None, :] - b[:, :, None, :, :]
    decay = jnp.exp(jnp.where(causal, diff, -jnp.inf))
    att = jnp.einsum('bhtk,bhsk,bhtsk->bhts', q, k, decay)
    o = jnp.einsum('bhts,bhsv->bhtv', att, v) + jnp.einsum('bhtk,bhkv->bhtv', q * jnp.exp(b), S)
    b_last = b[:, :, -1:, :]
    S_new = (jnp.exp(b_last[:, :, 0, :])[..., None] * S
             + jnp.einsum('bhsk,bhsv->bhkv', k * jnp.exp(b_last - b), v))
    return S_new, o


def gla_mixer(h, w_in, gate_w2, gate_b, head_norm, w_out):
    B_, L, _ = h.shape
    z = h @ w_in
    q = z[..., :GLA_KEY] * (GLA_DK ** -0.5)
    k = z[..., GLA_KEY:2 * GLA_KEY]
    v = z[..., 2 * GLA_KEY:2 * GLA_KEY + GLA_VAL]
    g = z[..., 2 * GLA_KEY + GLA_VAL:2 * GLA_KEY + 2 * GLA_VAL]
    glr = z[..., 2 * GLA_KEY + 2 * GLA_VAL:]
    lg = jax.nn.log_sigmoid((glr @ gate_w2 + gate_b).astype(F32)) / GLA_TAU

    def heads(t, dh):
        return t.reshape(B_, L, GLA_HEADS, dh).transpose(0, 2, 1, 3).astype(F32)

    q, k, lg, v = heads(q, GLA_DK), heads(k, GLA_DK), heads(lg, GLA_DK), heads(v, GLA_DV)
    S0 = jnp.zeros((B_, GLA_HEADS, GLA_DK, GLA_DV), F32)
    S1, o_meta = gla_chunk(S0, q[:, :, :N_META], k[:, :, :N_META], v[:, :, :N_META], lg[:, :, :N_META])
    n_chunks = (L - N_META) // GLA_CHUNK

    def to_chunks(t):
        return t[:, :, N_META:].reshape(B_, GLA_HEADS, n_chunks, GLA_CHUNK, t.shape[-1]).transpose(2, 0, 1, 3, 4)

    _, o_real = lax.scan(lambda S, xs: gla_chunk(S, *xs), S1,
                         (to_chunks(q), to_chunks(k), to_chunks(v), to_chunks(lg)))
    o_real = o_real.transpose(1, 2, 0, 3, 4).reshape(B_, GLA_HEADS, L - N_META, GLA_DV)
    o = jnp.concatenate([o_meta, o_real], axis=2).transpose(0, 2, 1, 3)
    o = o * lax.rsqrt(jnp.mean(o * o, axis=-1, keepdims=True) + EPS)
    o = o.reshape(B_, L, GLA_VAL) * head_norm.astype(F32)
    o = o.astype(h.dtype) * jax.nn.silu(g)
    return o @ w_out


def moe_swiglu(h, router_w, w_gate, w_up, w_down):
    logits = (h @ router_w).astype(F32)
    top_v, top_i = lax.top_k(logits, TOP_K)
    top_w = jax.nn.softmax(top_v, axis=-1)
    gates = jnp.sum(jax.nn.one_hot(top_i, N_EXPERTS, dtype=F32) * top_w[..., None], axis=-2)
    out = jnp.zeros_like(h)
    for e in range(N_EXPERTS):
        out = out + gates[..., e:e + 1].astype(h.dtype) * swiglu(h, w_gate[e], w_up[e], w_down[e])
    return out


def _normal(key, shape, scale):
    return scale * jax.random.normal(key, shape, F32)


def setup_inputs(seed: int = 0) -> dict:
    key = jax.random.key(seed)
    ks = iter(jax.random.split(key, 48))
    D, E, O = D_MODEL, N_EVEN, N_ODD
    G, N, H = SSM_GROUPS, SSM_STATE, SSM_GROUP
    x = _normal(next(ks), (BATCH, SEQ, D), 1.0)
    meta_tokens = _normal(next(ks), (N_META, D), 1.0)
    ev_norm_mix = 1.0 + _normal(next(ks), (E, D), 0.02)
    ev_w_in = _normal(next(ks), (E, D, EVEN_IN), D ** -0.5)
    ev_conv_w = _normal(next(ks), (E, CONV_WIDTH, LRU_WIDTH), CONV_WIDTH ** -0.5)
    ev_conv_b = _normal(next(ks), (E, LRU_WIDTH), 0.01)
    ev_gate_a_w = _normal(next(ks), (E, LRU_BLOCKS, LRU_BLOCK, LRU_BLOCK), LRU_BLOCK ** -0.5)
    ev_gate_a_b = _normal(next(ks), (E, LRU_WIDTH), 0.01)
    ev_gate_x_w = _normal(next(ks), (E, LRU_BLOCKS, LRU_BLOCK, LRU_BLOCK), LRU_BLOCK ** -0.5)
    ev_gate_x_b = _normal(next(ks), (E, LRU_WIDTH), 0.01)
    a_c = jax.random.uniform(next(ks), (E, LRU_WIDTH), F32, minval=0.9, maxval=0.999)
    s = a_c ** (1.0 / LRU_C)
    ev_lru_lambda = jnp.log(s) - jnp.log1p(-s)
    ev_ssm_lambda_re = -0.5 + _normal(next(ks), (E, G, N), 0.01)
    ev_ssm_lambda_im = jnp.pi * jnp.arange(N, dtype=F32) + _normal(next(ks), (E, G, N), 0.01)
    ev_ssm_log_dt = jax.random.uniform(next(ks), (E, G), F32, minval=math.log(1e-3), maxval=math.log(1e-1))
    ev_ssm_b_re = _normal(next(ks), (E, G, N, H), (2 * H) ** -0.5)
    ev_ssm_b_im = _normal(next(ks), (E, G, N, H), (2 * H) ** -0.5)
    ev_ssm_c_re = _normal(next(ks), (E, G, H, N), N ** -0.5)
    ev_ssm_c_im = _normal(next(ks), (E, G, H, N), N ** -0.5)
    ev_ssm_d = _normal(next(ks), (E, SSM_WIDTH), 1.0)
    ev_ssm_glu_w = _normal(next(ks), (E, SSM_WIDTH, SSM_WIDTH), SSM_WIDTH ** -0.5)
    ev_ssm_glu_b = _normal(next(ks), (E, SSM_WIDTH), 0.01)
    ev_w_out = _normal(next(ks), (E, EVEN_MIX, D), EVEN_MIX ** -0.5)
    ev_norm_ffn = 1.0 + _normal(next(ks), (E, D), 0.02)
    ev_ffn_w_gate = _normal(next(ks), (E, D, D_FF), D ** -0.5)
    ev_ffn_w_up = _normal(next(ks), (E, D, D_FF), D ** -0.5)
    ev_ffn_w_down = _normal(next(ks), (E, D_FF, D), D_FF ** -0.5)
    od_norm_mix = 1.0 + _normal(next(ks), (O, D), 0.02)
    od_w_in = _normal(next(ks), (O, D, ODD_IN), D ** -0.5)
    od_gla_gate_w2 = _normal(next(ks), (O, GLA_RANK, GLA_KEY), GLA_RANK ** -0.5)
    od_gla_gate_b = _normal(next(ks), (O, GLA_KEY), 0.1)
    od_gla_norm = 1.0 + _normal(next(ks), (O, GLA_VAL), 0.02)
    od_w_out = _normal(next(ks), (O, GLA_VAL, D), GLA_VAL ** -0.5)
    od_norm_ffn = 1.0 + _normal(next(ks), (O, D), 0.02)
    od_router_w = _normal(next(ks), (O, D, N_EXPERTS), D ** -0.5)
    od_moe_w_gate = _normal(next(ks), (O, N_EXPERTS, D, D_FF_EXPERT), D ** -0.5)
    od_moe_w_up = _normal(next(ks), (O, N_EXPERTS, D, D_FF_EXPERT), D ** -0.5)
    od_moe_w_down = _normal(next(ks), (O, N_EXPERTS, D_FF_EXPERT, D), D_FF_EXPERT ** -0.5)
    final_norm = 1.0 + _normal(next(ks), (D,), 0.02)
    return {
        "x": x, "meta_tokens": meta_tokens,
        "ev_norm_mix": ev_norm_mix, "ev_w_in": ev_w_in, "ev_conv_w": ev_conv_w, "ev_conv_b": ev_conv_b,
        "ev_gate_a_w": ev_gate_a_w, "ev_gate_a_b": ev_gate_a_b, "ev_gate_x_w": ev_gate_x_w,
        "ev_gate_x_b": ev_gate_x_b, "ev_lru_lambda": ev_lru_lambda,
        "ev_ssm_lambda_re": ev_ssm_lambda_re, "ev_ssm_lambda_im": ev_ssm_lambda_im,
        "ev_ssm_log_dt": ev_ssm_log_dt, "ev_ssm_b_re": ev_ssm_b_re, "ev_ssm_b_im": ev_ssm_b_im,
        "ev_ssm_c_re": ev_ssm_c_re, "ev_ssm_c_im": ev_ssm_c_im, "ev_ssm_d": ev_ssm_d,
        "ev_ssm_glu_w": ev_ssm_glu_w, "ev_ssm_glu_b": ev_ssm_glu_b, "ev_w_out": ev_w_out,
        "ev_norm_ffn": ev_norm_ffn, "ev_ffn_w_gate": ev_ffn_w_gate, "ev_ffn_w_up": ev_ffn_w_up,
        "ev_ffn_w_down": ev_ffn_w_down,
        "od_norm_mix": od_norm_mix, "od_w_in": od_w_in, "od_gla_gate_w2": od_gla_gate_w2,
        "od_gla_gate_b": od_gla_gate_b, "od_gla_norm": od_gla_norm, "od_w_out": od_w_out,
        "od_norm_ffn": od_norm_ffn, "od_router_w": od_router_w, "od_moe_w_gate": od_moe_w_gate,
        "od_moe_w_up": od_moe_w_up, "od_moe_w_down": od_moe_w_down,
        "final_norm": final_norm,
    }


def reference(x, meta_tokens,
              ev_norm_mix, ev_w_in, ev_conv_w, ev_conv_b, ev_gate_a_w, ev_gate_a_b, ev_gate_x_w,
              ev_gate_x_b, ev_lru_lambda, ev_ssm_lambda_re, ev_ssm_lambda_im, ev_ssm_log_dt,
              ev_ssm_b_re, ev_ssm_b_im, ev_ssm_c_re, ev_ssm_c_im, ev_ssm_d, ev_ssm_glu_w, ev_ssm_glu_b,
              ev_w_out, ev_norm_ffn, ev_ffn_w_gate, ev_ffn_w_up, ev_ffn_w_down,
              od_norm_mix, od_w_in, od_gla_gate_w2, od_gla_gate_b, od_gla_norm, od_w_out,
              od_norm_ffn, od_router_w, od_moe_w_gate, od_moe_w_up, od_moe_w_down,
              final_norm):
    B_ = x.shape[0]
    meta = jnp.broadcast_to(meta_tokens.astype(x.dtype)[None], (B_, N_META, D_MODEL))
    h = jnp.concatenate([meta, x], axis=1)
    for layer in range(DEPTH):
        j = layer // 2
        if layer % 2 == 0:
            h = h + even_mixer(rms_norm(h, ev_norm_mix[j]), ev_w_in[j], ev_conv_w[j], ev_conv_b[j],
                               ev_gate_a_w[j], ev_gate_a_b[j], ev_gate_x_w[j], ev_gate_x_b[j],
                               ev_lru_lambda[j], ev_ssm_lambda_re[j], ev_ssm_lambda_im[j],
                               ev_ssm_log_dt[j], ev_ssm_b_re[j], ev_ssm_b_im[j], ev_ssm_c_re[j],
                               ev_ssm_c_im[j], ev_ssm_d[j], ev_ssm_glu_w[j], ev_ssm_glu_b[j], ev_w_out[j])
            h = h + swiglu(rms_norm(h, ev_norm_ffn[j]), ev_ffn_w_gate[j], ev_ffn_w_up[j], ev_ffn_w_down[j])
        else:
            h = h + gla_mixer(rms_norm(h, od_norm_mix[j]), od_w_in[j], od_gla_gate_w2[j],
                              od_gla_gate_b[j], od_gla_norm[j], od_w_out[j])
            h = h + moe_swiglu(rms_norm(h, od_norm_ffn[j]), od_router_w[j], od_moe_w_gate[j],
                               od_moe_w_up[j], od_moe_w_down[j])
    return rms_norm(h, final_norm)[:, N_META:]
```

```python
from contextlib import ExitStack
import math
import numpy as np
import concourse.bass as bass
import concourse.mybir as mybir
from concourse.bass_utils import run_bass_kernel_spmd

F32 = mybir.dt.float32
F32R = mybir.dt.float32r
I32 = mybir.dt.int32
AF = mybir.ActivationFunctionType
ALU = mybir.AluOpType
OUTKEYS = ("out", "accum_out", "ap")

D = 1024
KD = 8
NMETA = 16
SEQ = 8192
BATCH = 4
EPS = 1e-6
NEXP = 8
NSLOT = 5
DFE = 3584
DFF = 3072


class Sem:
    def __init__(self, P, name):
        self.h = P.ctx.enter_context(P.nc.semaphore(name))
        self.val = 0


class V:
    __slots__ = ("buf", "ap")

    def __init__(self, buf, ap):
        self.buf = buf
        self.ap = ap


class Buf:
    def __init__(self, P, name, shape, dtype, space="sbuf", alias=None):
        self.psum = (space == "psum")
        if alias is not None:
            self.t = alias
        elif space == "sbuf":
            self.t = P.ctx.enter_context(P.nc.sbuf_tensor(name, list(shape), dtype))
        else:
            self.t = P.ctx.enter_context(P.nc.psum_tensor(name, list(shape), dtype))
        self.lastw = None
        self.reads = []

    @property
    def f(self):
        return _F32View(self)

    def __getitem__(self, idx):
        return V(self, self.t[idx])

    def v(self, ap):
        return V(self, ap)


class _F32View:
    def __init__(self, b):
        self.b = b

    def __getitem__(self, idx):
        return V(self.b, self.b.t[idx].bitcast(F32))


class Eng:
    def __init__(self, P, name):
        self.sem = Sem(P, "s_" + name)
        self.seen = {}
        self.q = []


class Prog:
    def __init__(self, nc):
        self.nc = nc
        self.ctx = ExitStack()
        self.E = {n: Eng(self, n) for n in ("pe", "dve", "act", "pool", "sp")}

    def buf(self, name, shape, dtype=F32, space="sbuf", alias=None):
        return Buf(self, name, shape, dtype, space, alias)

    def _waits(self, E, R, W, skip_own):
        waits = {}

        def need(tk):
            if tk is not None and waits.get(tk[0], 0) < tk[1]:
                waits[tk[0]] = tk[1]

        for v in R:
            need(v.buf.lastw)
            if v.buf.psum:
                for t in v.buf.reads:
                    if t[0] is not E.sem:
                        need(t)
        for v in W:
            need(v.buf.lastw)
            for t in v.buf.reads:
                need(t)
        wl = []
        for s, val in waits.items():
            if skip_own and s is E.sem:
                continue
            if E.seen.get(s, 0) < val:
                E.seen[s] = val
                wl.append((s, val))
        return wl

    def op(self, eng, meth, *args, writes=(), reads=(), **kw):
        E = self.E[eng]
        W = [v for k, v in kw.items() if isinstance(v, V) and k in OUTKEYS] + list(writes)
        R = [v for k, v in kw.items() if isinstance(v, V) and k not in OUTKEYS] + list(reads)
        import os
        wl = self._waits(E, R, W, eng == "pe" or bool(os.environ.get("SKIPOWN")))
        E.sem.val += 1
        tk = (E.sem, E.sem.val)
        kw2 = {k: (v.ap if isinstance(v, V) else v) for k, v in kw.items()}
        E.q.append((meth, args, kw2, wl, (E.sem, 1)))
        for v in R:
            v.buf.reads.append(tk)
        for v in W:
            v.buf.lastw = tk
            v.buf.reads = []
        return tk

    def dma(self, eng, out, in_, sem, **kw):
        E = self.E[eng]
        W = [out] if isinstance(out, V) else []
        R = [in_] if isinstance(in_, V) else []
        wl = self._waits(E, R, W, False)
        sem.val += 16
        tk = (sem, sem.val)
        kw2 = dict(kw)
        kw2["out"] = out.ap if isinstance(out, V) else out
        kw2["in_"] = in_.ap if isinstance(in_, V) else in_
        E.q.append(("dma_start", (), kw2, wl, (sem, 16)))
        for v in R:
            v.buf.reads.append(tk)
        for v in W:
            v.buf.lastw = tk
            v.buf.reads = []
        return tk

    def wait(self, eng, tk):
        E = self.E[eng]
        if E.seen.get(tk[0], 0) < tk[1]:
            E.seen[tk[0]] = tk[1]
            E.q.append((None, (), {}, [tk], None))

    def build(self):
        nc = self.nc
        with nc.Block() as block:
            def mk(E):
                def body(e):
                    for meth, args, kw, wl, inc in E.q:
                        for s, val in wl:
                            e.wait_ge(s.h, val)
                        if meth is None:
                            continue
                        ins = getattr(e, meth)(*args, **kw)
                        if inc is not None:
                            ins.then_inc(inc[0].h, inc[1])
                return body
            for name, reg in (("pe", block.tensor), ("dve", block.vector), ("act", block.scalar),
                              ("pool", block.gpsimd), ("sp", block.sync)):
                if self.E[name].q:
                    reg(mk(self.E[name]))
        self.ctx.close()


BIGW = {
    "ev_w_in": (1024, 2560), "ev_gate_a_w": (1024, 256), "ev_gate_x_w": (1024, 256),
    "ev_ssm_glu_w": (512, 512), "ev_w_out": (1536, 1024),
    "ev_ffn_w_gate": (1024, 3072), "ev_ffn_w_up": (1024, 3072), "ev_ffn_w_down": (3072, 1024),
    "od_w_in": (1024, 3088), "od_gla_gate_w2": (16, 512), "od_w_out": (1024, 1024),
    "od_moe_w_gate": (8 * 1024, 3584), "od_moe_w_up": (8 * 1024, 3584), "od_moe_w_down": (8 * 3584, 1024),
}
VEC1024 = ["ev_norm_mix", "ev_conv_b", "ev_gate_a_b", "ev_gate_x_b", "ev_lru_lambda", "ev_norm_ffn",
           "od_norm_mix", "od_gla_norm", "od_norm_ffn", "final_norm"]
VEC512 = ["ev_ssm_d", "ev_ssm_glu_b", "od_gla_gate_b"]


def make_blocks(nch):
    first = min(7, nch)
    blocks = [[16] + [64] * first]
    rem = nch - first
    a = rem // 8
    while a >= 0 and (rem - 8 * a) % 7 != 0:
        a -= 1
    if a < 0:
        a, b7 = 0, 0
        blocks += [[64] * 7] * (rem // 7)
        if rem % 7:
            blocks.append([64] * (rem % 7))
        return blocks
    b7 = (rem - 8 * a) // 7
    blocks += [[64] * 8] * a + [[64] * 7] * b7
    return blocks


class K:
    def __init__(self, nchunks, stage_stop=99, dump_h=False):
        self.nchunks = nchunks
        self.stage_stop = stage_stop
        self.dump_h = dump_h
        self.blocks = make_blocks(nchunks)
        self.ntok = 2 * (NMETA + 64 * nchunks)
        self.npass = NMETA + 64 * nchunks
        nc = bass.Bass("TRN2", target_bir_lowering=False)
        nc.dge_precook = False
        self.nc = nc
        self.dr = {}
        dr = self.dr
        dr["tok"] = nc.dram_tensor("tok", [self.ntok, D], F32, kind="ExternalInput").ap()
        import os
        for n, s in BIGW.items():
            if os.environ.get("NOBIGW"):
                continue
            dr[n] = nc.dram_tensor(n, list(s), F32R, kind="ExternalInput").ap()
        for n in VEC1024:
            dr[n] = nc.dram_tensor(n, [1024], F32, kind="ExternalInput").ap()
        for n in VEC512:
            dr[n] = nc.dram_tensor(n, [512], F32, kind="ExternalInput").ap()
        dr["ev_conv_w"] = nc.dram_tensor("ev_conv_w", [4, 1024], F32, kind="ExternalInput").ap()
        dr["od_router_w"] = nc.dram_tensor("od_router_w", [1024, 8], F32, kind="ExternalInput").ap()
        for n in ("ev_ssm_lambda_re", "ev_ssm_lambda_im"):
            dr[n] = nc.dram_tensor(n, [32, 64], F32, kind="ExternalInput").ap()
        dr["ev_ssm_log_dt"] = nc.dram_tensor("ev_ssm_log_dt", [32], F32, kind="ExternalInput").ap()
        for n in ("ev_ssm_b_re", "ev_ssm_b_im"):
            dr[n] = nc.dram_tensor(n, [32, 64, 16], F32, kind="ExternalInput").ap()
        for n in ("ev_ssm_c_re", "ev_ssm_c_im"):
            dr[n] = nc.dram_tensor(n, [32, 16, 64], F32, kind="ExternalInput").ap()
        dr["c_ident"] = nc.dram_tensor("c_ident", [128, 128], F32, kind="ExternalInput").ap()
        dr["c_onesr"] = nc.dram_tensor("c_onesr", [128, 128], F32R, kind="ExternalInput").ap()
        dr["c_maskpar"] = nc.dram_tensor("c_maskpar", [128, 3], F32, kind="ExternalInput").ap()
        dr["c_triu"] = nc.dram_tensor("c_triu", [64, 64], F32, kind="ExternalInput").ap()
        dr["c_rmask"] = nc.dram_tensor("c_rmask", [128, 528], F32, kind="ExternalInput").ap()
        dr["c_flag"] = nc.dram_tensor("c_flag", [128, 2], F32, kind="ExternalInput").ap()
        dr["out"] = nc.dram_tensor("out", [64 * nchunks, D], F32, kind="ExternalOutput").ap()
        self.P = Prog(nc)
        self.alloc()
        self.prologue()
        pos = 0
        for bi, chunks in enumerate(self.blocks):
            self.block(pos, pos, chunks, hist=True, blend=False)
            pos += sum(chunks)
        self.main_start()
        mpos = 0
        for bi, chunks in enumerate(self.blocks):
            self.block(pos, mpos, chunks, hist=False, blend=(bi == 0))
            pos += sum(chunks)
            mpos += sum(chunks)
        for os_ in self.osem:
            self.P.wait("act", (os_, os_.val))
        self.P.build()

    def alloc(self):
        P = self.P
        b = P.buf
        ht = P.ctx.enter_context(P.nc.sbuf_tensor("h", [128, KD, 512], F32))
        self.ht = ht
        hnt = P.ctx.enter_context(P.nc.sbuf_tensor("hn", [128, KD, 512], F32R))
        import os
        NA = 8 if os.environ.get("SMALLA") else 28
        At = P.ctx.enter_context(P.nc.sbuf_tensor("A", [128, NA, 512], F32R))
        self.At = At
        self.h = [b(f"h{k}", None, None, alias=ht[:, k, :]) for k in range(KD)]
        self.hn = [b(f"hn{k}", None, None, alias=hnt[:, k, :]) for k in range(KD)]
        self.A = [b(f"A{k}", None, None, alias=At[:, k % NA, :]) for k in range(28)]
        self.slots = [b(f"slot{i}", [128, 2048], F32R) for i in range(NSLOT)]
        self.slot_sem = [Sem(P, f"slsem{i}") for i in range(NSLOT)]
        self.slot_i = 0
        self.ps = [b(f"ps{i}", [128, 512], F32, "psum") for i in range(8)]
        self.pool = [0, 1, 2, 3, 4, 5]
        self.ps_i = 0
        self.psm_i = 0
        self.tmp = [b(f"tmp{i}", [128, 512], F32) for i in range(6)]
        self.tmp_i = 0
        self.xt = [b(f"xt{i}", [128, 1024], F32) for i in range(2)]
        self.xt_sem = [Sem(P, f"xtsem{i}") for i in range(2)]
        self.osem = [Sem(P, f"osem{i}") for i in range(2)]
        self.psem = Sem(P, "psem")
        self.io_i = 0
        self.vec = {n: b("p_" + n, [128, 8], F32) for n in VEC1024}
        self.vec5 = {n: b("p_" + n, [128, 4], F32) for n in VEC512}
        self.convw = b("convw", [128, 4, 8], F32)
        self.rw = b("rw", [128, 8, 8], F32)
        self.rwg = b("rwg", [128, 8, 8], F32)
        self.ident = b("ident", [128, 128], F32)
        self.onesr = b("onesr", [128, 128], F32R)
        self.maskpar = b("maskpar", [128, 3], F32)
        self.triu = b("triu", [64, 64], F32)
        self.rmask = b("rmask", [128, 528], F32)
        self.w2 = b("w2", [16, 512], F32R)
        self.nc8 = b("nc8", [128, 8], F32)
        self.nc16 = b("nc16", [128, 8], F32)
        self.rec = b("rec", [128, 8, 3 + 512], F32)
        self.lru_state = b("lru_state", [128, 8], F32)
        self.Bc = b("Bc", [128, 4, 2, 128], F32R)
        self.Cc = b("Cc", [128, 16, 2, 64], F32)
        self.Bc3 = b("Bc3", [128, 4, 2, 128], F32R)
        self.lam_pw = b("lam_pw", [128, 16, 10, 3], F32)
        self.s5_state = b("s5_state", [128, 16, 2], F32)
        self.S = [b(f"S{i}", [128, 256], F32) for i in range(4)]
        self.Srab = [b(f"Srab{i}", [128, 256], F32R) for i in range(2)]
        self.S2 = b("Stmp2", [128, 256], F32)
        self.small = [b(f"small{i}", [128, 16], F32) for i in range(8)]
        self.small_i = 0
        self.epsb = b("epsb", [128, 1], F32)
        self.oneb = b("oneb", [128, 1], F32)
        self.ones8 = b("ones8", [8, 128], F32)
        self.vt = b("vt", None, None, alias=self.At[0:64, 12:16, :].rearrange("p a b -> p (a b)").rearrange("p (c v) -> p c v", c=8))
        self.qt = self.A[10]
        self.kt = self.A[11]
        self.att2 = [b(f"att{i}", [64, 64], F32R) for i in range(2)]
        self.ktT = b("ktT", None, None, alias=self.At[0:64, 16:18, :].rearrange("p a b -> p (a b)").rearrange("p (c v) -> p c v", c=8))
        self.gbc = b("gbc", [128, 512], F32)
        self.flag = b("flag", [128, 2], F32)
        self.gkeep = b("gkeep", [8, 512], F32)
        self.prev_n = None
        self.scr_off = 0

    def scratch(self, name, npart, width, dtype=F32):
        regs = [(self.ht[:, :, :].rearrange("p a b -> p (a b)"), 4096),
                (self.rec.t[:, :, :].rearrange("p a b -> p (a b)"), 8 * 515)]
        if not hasattr(self, "scr_offs"):
            self.scr_offs = [0, 0]
        for ri, (flat, cap) in enumerate(regs):
            off = self.scr_offs[ri]
            if off + width <= cap:
                self.scr_offs[ri] = off + width
                ap = flat[0:npart, off:off + width]
                if dtype is not F32:
                    ap = ap.bitcast(dtype)
                bf = self.P.buf(name, None, None, alias=ap)
                self.scr_list.append(bf)
                return bf
        raise AssertionError("scratch full")

    def T(self):
        t = self.tmp[self.tmp_i % len(self.tmp)]
        self.tmp_i += 1
        return t

    def SM(self):
        t = self.small[self.small_i % len(self.small)]
        self.small_i += 1
        return t

    def PS(self):
        t = self.ps[self.pool[self.ps_i % len(self.pool)]]
        self.ps_i += 1
        return t

    def PSM(self):
        t = self.ps[6 + self.psm_i % 2]
        self.psm_i += 1
        return t

    def next_slot(self):
        i = self.slot_i % NSLOT
        self.slot_i += 1
        return self.slots[i], self.slot_sem[i]

    def kslab(self, name, k0, kc, c0, ncols, row0=0):
        assert kc * ncols <= 2048
        w = self.dr[name]
        rows = w[row0 + k0 * 128: row0 + (k0 + kc) * 128, c0:c0 + ncols].rearrange("(k p) c -> p k c", p=128)
        s, sem = self.next_slot()
        sv = s.t[:, 0:kc * ncols].rearrange("p (k c) -> p k c", k=kc)
        self.P.dma("sp", s.v(sv), rows, sem)
        return s, sv

    def prologue(self):
        P, dr = self.P, self.dr
        ps = self.psem
        self.scr_list = []
        loaded = []
        import os
        if os.environ.get("MINPRO"):
            P.dma("sp", self.ident[:, :], dr["c_ident"], ps)
            P.dma("sp", self.onesr[:, :], dr["c_onesr"], ps)
            self.ident.lastw = (ps, ps.val)
            self.onesr.lastw = (ps, ps.val)
            return

        def ld(dst, src, **kw):
            P.dma("sp", dst, src, ps, **kw)
            loaded.append(dst.buf)
        nrow = 8 * len(VEC1024) + 4 * len(VEC512) + 32
        vstage = self.scratch("vstage", nrow, 128)
        r0 = 0
        self.vrows = {}
        for n in VEC1024:
            ld(vstage[r0:r0 + 8, :], dr[n].rearrange("(k p) -> k p", p=128))
            self.vrows[n] = (r0, 8)
            r0 += 8
        for n in VEC512:
            ld(vstage[r0:r0 + 4, :], dr[n].rearrange("(k p) -> k p", p=128))
            self.vrows[n] = (r0, 4)
            r0 += 4
        ld(vstage[r0:r0 + 32, :], dr["ev_conv_w"].rearrange("t (k p) -> (t k) p", p=128))
        self.vrows["ev_conv_w"] = (r0, 32)
        self.vstage = vstage
        ld(self.rw[:, :, :], dr["od_router_w"].rearrange("(k p) e -> p k e", p=128))
        ld(self.ident[:, :], dr["c_ident"])
        ld(self.onesr[:, :], dr["c_onesr"])
        ld(self.maskpar[:, :], dr["c_maskpar"])
        ld(self.triu[:, :], dr["c_triu"])
        ld(self.rmask[:, :], dr["c_rmask"])
        ld(self.w2[:, :], dr["od_gla_gate_w2"])
        ld(self.flag[:, :], dr["c_flag"])
        g = {}
        for n in ("ev_ssm_lambda_re", "ev_ssm_lambda_im"):
            t = self.scratch("g32_" + n, 32, 64)
            ld(t[:, :], dr[n])
            g[n] = t
            t2 = self.scratch("p16_" + n, 16, 128)
            ld(t2[:, :], dr[n].rearrange("(q r) n -> q (r n)", r=2))
            g[n + "_p16"] = t2
        self.g32 = g
        ldt32 = self.scratch("ldt32", 32, 1)
        ld(ldt32[:, :], dr["ev_ssm_log_dt"].rearrange("(g o) -> g o", o=1))
        ldt16 = self.scratch("ldt16", 16, 2)
        ld(ldt16[:, :], dr["ev_ssm_log_dt"].rearrange("(q r) -> q r", r=2))
        bre = self.scratch("bre", 64, 512)
        bim = self.scratch("bim", 64, 512)
        ld(bre.v(bre.t.rearrange("p (g h) -> p g h", g=32)), dr["ev_ssm_b_re"].rearrange("g n h -> n g h"))
        ld(bim.v(bim.t.rearrange("p (g h) -> p g h", g=32)), dr["ev_ssm_b_im"].rearrange("g n h -> n g h"))
        cnat = self.scratch("cnat", 128, 512)
        cv = cnat.t.rearrange("p (c j n) -> p c j n", c=4, j=2)
        ld(cnat.v(cv[:, :, 0, :]), dr["ev_ssm_c_re"].rearrange("(c g) h n -> (g h) c n", c=4))
        ld(cnat.v(cv[:, :, 1, :]), dr["ev_ssm_c_im"].rearrange("(c g) h n -> (g h) c n", c=4))
        fin = (ps, ps.val)
        for bf in loaded:
            bf.lastw = fin

        op = P.op
        op("dve", "memset", ap=self.epsb[:, :], constant=EPS)
        op("dve", "memset", ap=self.oneb[:, :], constant=1.0)
        op("dve", "memset", ap=self.ones8[:, :], constant=1.0)
        op("dve", "memset", ap=self.lru_state[:, :], constant=0.0)
        op("dve", "memset", ap=self.s5_state[:, :, :], constant=0.0)
        for i in range(4):
            op("dve", "memset", ap=self.S[i][:, :], constant=0.0)
        pv = self.PSM()
        nrow = 8 * len(VEC1024) + 4 * len(VEC512) + 32
        op("pe", "transpose", out=pv[:, 0:nrow], in_=self.vstage[:, :], identity=self.ident[0:nrow, 0:nrow])
        for n in VEC1024:
            r0, nr = self.vrows[n]
            op("dve", "tensor_copy", out=self.vec[n][:, :], in_=pv[:, r0:r0 + nr])
        for n in VEC512:
            r0, nr = self.vrows[n]
            op("dve", "tensor_copy", out=self.vec5[n][:, :], in_=pv[:, r0:r0 + nr])
        r0, nr = self.vrows["ev_conv_w"]
        op("dve", "tensor_copy", out=self.convw[:, :, :], in_=pv.v(pv.t[:, r0:r0 + 32].rearrange("p (t k) -> p t k", t=4)))
        gff = self.vec["od_norm_ffn"]
        for k in range(8):
            op("dve", "tensor_scalar", out=self.rwg[:, k, :], in0=self.rw[:, k, :], scalar1=gff[:, k:k + 1], scalar2=None, op0=ALU.mult)
        lam = self.vec["ev_lru_lambda"]
        t1, t2 = self.SM(), self.SM()
        op("act", "activation", out=t1[:, 0:8], in_=lam[:, :], func=AF.Abs)
        op("act", "activation", out=t1[:, 0:8], in_=t1[:, 0:8], func=AF.Exp, scale=-1.0)
        op("act", "activation", out=t1[:, 0:8], in_=t1[:, 0:8], func=AF.Ln, bias=self.oneb[:, 0:1], scale=1.0)
        op("dve", "tensor_scalar", out=t2[:, 0:8], in0=lam[:, :], scalar1=-1.0, scalar2=0.0, op0=ALU.mult, op1=ALU.max)
        op("dve", "tensor_tensor", out=t1[:, 0:8], in0=t1[:, 0:8], in1=t2[:, 0:8], op=ALU.add)
        op("dve", "tensor_scalar", out=self.nc8[:, :], in0=t1[:, 0:8], scalar1=-8.0, scalar2=None, op0=ALU.mult)
        op("dve", "tensor_scalar", out=self.nc16[:, :], in0=t1[:, 0:8], scalar1=-16.0, scalar2=None, op0=ALU.mult)
        import os
        if not os.environ.get('SKIP_S5P'):
            self.s5_prologue(ldt32, ldt16, bre, bim, cnat)
        for bf in self.scr_list:
            for a in self.h + [self.rec]:
                if bf.lastw is not None:
                    a.reads.append(bf.lastw)
                a.reads.extend(bf.reads)
        op("dve", "memset", ap=self.rec[:, :, :], constant=0.0)

    def lbar(self, lr, li, dtcol, npart, width, name):
        P = self.P
        op = P.op
        mk = lambda s: self.scratch(f"{name}_{s}", npart, width)
        dlr, dli, mag, sn, cs, ar, ai = mk("dlr"), mk("dli"), mk("mag"), mk("sn"), mk("cs"), mk("ar"), mk("ai")
        for sl, sc in dtcol:
            op("dve", "tensor_scalar", out=dlr[:, sl], in0=lr[:, sl], scalar1=sc, scalar2=None, op0=ALU.mult)
            op("dve", "tensor_scalar", out=dli[:, sl], in0=li[:, sl], scalar1=sc, scalar2=None, op0=ALU.mult)
        op("act", "activation", out=mag[:, :], in_=dlr[:, :], func=AF.Exp)
        ki = self.scratch(name + "_ki", npart, width, dtype=I32)
        kf = mk("kf")
        op("dve", "tensor_scalar", out=kf[:, :], in0=dli[:, :], scalar1=1.0 / (2 * math.pi), scalar2=None, op0=ALU.mult)
        op("dve", "tensor_copy", out=ki[:, :], in_=kf[:, :])
        op("dve", "tensor_copy", out=kf[:, :], in_=ki[:, :])
        r = mk("r")
        op("dve", "scalar_tensor_tensor", out=r[:, :], in0=kf[:, :], scalar=-2 * math.pi, in1=dli[:, :], op0=ALU.mult, op1=ALU.add)

        def wrap(tag, src, shift):
            y, m = mk("wy" + tag), mk("wm" + tag)
            op("dve", "tensor_scalar", out=y[:, :], in0=src[:, :], scalar1=shift, scalar2=None, op0=ALU.add)
            for _ in range(2):
                op("dve", "tensor_scalar", out=m[:, :], in0=y[:, :], scalar1=math.pi, scalar2=-2 * math.pi, op0=ALU.is_gt, op1=ALU.mult)
                op("dve", "tensor_tensor", out=y[:, :], in0=y[:, :], in1=m[:, :], op=ALU.add)
                op("dve", "tensor_scalar", out=m[:, :], in0=y[:, :], scalar1=-math.pi, scalar2=2 * math.pi, op0=ALU.is_lt, op1=ALU.mult)
                op("dve", "tensor_tensor", out=y[:, :], in0=y[:, :], in1=m[:, :], op=ALU.add)
            op("dve", "tensor_scalar", out=y[:, :], in0=y[:, :], scalar1=math.pi, scalar2=-math.pi, op0=ALU.min, op1=ALU.max)
            return y
        ys = wrap("s", r, 0.0)
        yc = wrap("c", r, math.pi / 2)
        op("act", "activation", out=sn[:, :], in_=ys[:, :], func=AF.Sin)
        op("act", "activation", out=cs[:, :], in_=yc[:, :], func=AF.Sin)
        op("dve", "tensor_tensor", out=ar[:, :], in0=mag[:, :], in1=cs[:, :], op=ALU.mult)
        op("dve", "tensor_tensor", out=ai[:, :], in0=mag[:, :], in1=sn[:, :], op=ALU.mult)
        return ar, ai

    def s5_prologue(self, ldt32, ldt16, bre, bim, cnat):
        P = self.P
        op = P.op
        g = self.g32
        dt32 = self.scratch("dt32", 32, 1)
        dt16 = self.scratch("dt16", 16, 2)
        op("act", "activation", out=dt32[:, :], in_=ldt32[:, :], func=AF.Exp)
        op("act", "activation", out=dt16[:, :], in_=ldt16[:, :], func=AF.Exp)
        lr, li = g["ev_ssm_lambda_re"], g["ev_ssm_lambda_im"]
        ar, ai = self.lbar(lr, li, [(slice(0, 64), dt32[:, 0:1])], 32, 64, "L32")
        mk = lambda s: self.scratch("z32_" + s, 32, 64)
        den, t, nr, zr, zi = mk("den"), mk("t"), mk("nr"), mk("zr"), mk("zi")
        op("dve", "tensor_tensor", out=den[:, :], in0=lr[:, :], in1=lr[:, :], op=ALU.mult)
        op("dve", "tensor_tensor", out=t[:, :], in0=li[:, :], in1=li[:, :], op=ALU.mult)
        op("dve", "tensor_tensor", out=den[:, :], in0=den[:, :], in1=t[:, :], op=ALU.add)
        op("dve", "reciprocal", out=den[:, :], in_=den[:, :])
        op("dve", "tensor_scalar", out=nr[:, :], in0=ar[:, :], scalar1=-1.0, scalar2=None, op0=ALU.add)
        op("dve", "tensor_tensor", out=zr[:, :], in0=nr[:, :], in1=lr[:, :], op=ALU.mult)
        op("dve", "tensor_tensor", out=t[:, :], in0=ai[:, :], in1=li[:, :], op=ALU.mult)
        op("dve", "tensor_tensor", out=zr[:, :], in0=zr[:, :], in1=t[:, :], op=ALU.add)
        op("dve", "tensor_tensor", out=zr[:, :], in0=zr[:, :], in1=den[:, :], op=ALU.mult)
        op("dve", "tensor_tensor", out=zi[:, :], in0=ai[:, :], in1=lr[:, :], op=ALU.mult)
        op("dve", "tensor_tensor", out=t[:, :], in0=nr[:, :], in1=li[:, :], op=ALU.mult)
        op("dve", "tensor_tensor", out=zi[:, :], in0=zi[:, :], in1=t[:, :], op=ALU.subtract)
        op("dve", "tensor_tensor", out=zi[:, :], in0=zi[:, :], in1=den[:, :], op=ALU.mult)
        zT = self.scratch("zT", 64, 96)
        for j, src in enumerate((zr, zi)):
            pt = self.PSM()
            op("pe", "transpose", out=pt[0:64, 0:32], in_=src[:, :], identity=self.ident[0:32, 0:32])
            op("dve", "tensor_copy", out=zT[:, 32 * j:32 * j + 32], in_=pt[0:64, 0:32])
        op("dve", "tensor_scalar", out=zT[:, 64:96], in0=zT[:, 32:64], scalar1=-1.0, scalar2=None, op0=ALU.mult)
        Bre = self.scratch("Bre", 64, 512)
        Bim = self.scratch("Bim", 64, 512)
        tt = self.scratch("tt", 64, 16)
        for gi in range(32):
            gs = slice(16 * gi, 16 * gi + 16)
            op("dve", "tensor_scalar", out=tt[:, :], in0=bre[:, gs], scalar1=zT[:, gi:gi + 1], scalar2=None, op0=ALU.mult)
            op("dve", "scalar_tensor_tensor", out=Bre[:, gs], in0=bim[:, gs], scalar=zT[:, 64 + gi:65 + gi], in1=tt[:, :], op0=ALU.mult, op1=ALU.add)
            op("dve", "tensor_scalar", out=tt[:, :], in0=bim[:, gs], scalar1=zT[:, gi:gi + 1], scalar2=None, op0=ALU.mult)
            op("dve", "scalar_tensor_tensor", out=Bim[:, gs], in0=bre[:, gs], scalar=zT[:, 32 + gi:33 + gi], in1=tt[:, :], op0=ALU.mult, op1=ALU.add)
        for c in range(4):
            for j, src in enumerate((Bre, Bim)):
                pt = self.PSM()
                op("pe", "transpose", out=pt[:, 0:64], in_=src[:, 128 * c:128 * c + 128], identity=self.ident[0:64, 0:64])
                for par in range(2):
                    op("dve", "tensor_scalar", out=self.Bc[:, c, j, 64 * par:64 * par + 64], in0=pt[:, 0:64],
                       scalar1=self.maskpar[:, par:par + 1], scalar2=None, op0=ALU.mult)
        for c in range(4):
            for j in range(2):
                op("dve", "tensor_scalar", out=self.Bc3[:, c, j, :], in0=self.Bc.f[:, c, j, :],
                   scalar1=self.maskpar[:, 2:3], scalar2=None, op0=ALU.mult)
        op("dve", "memset", ap=self.Cc[:, :, :, :], constant=0.0)
        pre2 = self.scratch("pre2", 128, 128)
        for c in range(4):
            for j in range(2):
                for par in range(2):
                    op("dve", "tensor_scalar", out=pre2[:, 64 * par:64 * par + 64], in0=cnat[:, (c * 2 + j) * 64:(c * 2 + j) * 64 + 64],
                       scalar1=self.maskpar[:, par:par + 1], scalar2=(1.0 if j == 0 else -1.0), op0=ALU.mult, op1=ALU.mult)
                pt = self.PSM()
                op("pe", "transpose", out=pt[:, 0:128], in_=pre2[:, :], identity=self.ident[:, :])
                for r in range(4):
                    op("dve", "tensor_copy", out=self.Cc[:, 4 * c + r, j, 32 * (r % 2):32 * (r % 2) + 32],
                       in_=pt[:, 32 * r:32 * r + 32])
        lr16, li16 = g["ev_ssm_lambda_re_p16"], g["ev_ssm_lambda_im_p16"]
        ar16, ai16 = self.lbar(lr16, li16, [(slice(0, 64), dt16[:, 0:1]), (slice(64, 128), dt16[:, 1:2])], 16, 128, "L16")
        lp = self.lam_pw
        for j, src in enumerate((ar16, ai16)):
            pt = self.PSM()
            op("pe", "transpose", out=pt[:, 0:16], in_=src[:, :], identity=self.ident[0:16, 0:16])
            op("dve", "tensor_copy", out=lp[:, :, 0, j], in_=pt[:, 0:16])
        t1 = self.scratch("lp_t1", 128, 16)
        t2 = self.scratch("lp_t2", 128, 16)
        for lv in range(1, 10):
            op("dve", "tensor_tensor", out=t1[:, :], in0=lp[:, :, lv - 1, 0], in1=lp[:, :, lv - 1, 0], op=ALU.mult)
            op("dve", "tensor_tensor", out=t2[:, :], in0=lp[:, :, lv - 1, 1], in1=lp[:, :, lv - 1, 1], op=ALU.mult)
            op("dve", "tensor_tensor", out=lp[:, :, lv, 0], in0=t1[:, :], in1=t2[:, :], op=ALU.subtract)
            op("dve", "tensor_tensor", out=t1[:, :], in0=lp[:, :, lv - 1, 0], in1=lp[:, :, lv - 1, 1], op=ALU.mult)
            op("dve", "tensor_scalar", out=lp[:, :, lv, 1], in0=t1[:, :], scalar1=2.0, scalar2=None, op0=ALU.mult)
        op("dve", "tensor_scalar", out=lp[:, :, :, 2], in0=lp[:, :, :, 1], scalar1=-1.0, scalar2=None, op0=ALU.mult)

    def rmsnorm(self, n, gname, src=None, dst=None, dst_f32=False):
        P = self.P
        op = P.op
        src = src or self.h
        dst = dst or self.hn
        A = self.A
        for k in range(KD):
            op("act", "activation", out=A[20 + k][:, 0:n], in_=src[k][:, 0:n], func=AF.Square)
        pss = self.PSM()
        for k in range(KD):
            op("pe", "matmul", out=pss[:, 0:n], lhsT=self.onesr[:, :], rhs=A[20 + k][:, 0:n], start=(k == 0), stop=(k == KD - 1))
        rstd = self.T()
        op("act", "activation", out=rstd[:, 0:n], in_=pss[:, 0:n], func=AF.Sqrt, scale=1.0 / 1024.0, bias=self.epsb[:, 0:1])
        op("dve", "reciprocal", out=rstd[:, 0:n], in_=rstd[:, 0:n])
        g = self.vec[gname]
        for k in range(KD):
            o = dst[k].f[:, 0:n] if dst_f32 else dst[k][:, 0:n]
            op("dve", "scalar_tensor_tensor", out=o, in0=src[k][:, 0:n], scalar=g[:, k:k + 1],
               in1=rstd[:, 0:n], op0=ALU.mult, op1=ALU.mult)
        return rstd

    def load_tokens(self, pos, n):
        P = self.P
        op = P.op
        import os
        t0 = 0
        nlim = int(os.environ.get('LOAD_TOK', '100000'))
        while t0 < min(n, nlim):
            nt = min(128, n - t0)
            i = self.io_i % 2
            self.io_i += 1
            xt = self.xt[i]
            P.dma("act", xt[0:nt, :], self.dr["tok"][pos + t0:pos + t0 + nt, :], self.xt_sem[i])
            lmode = int(os.environ.get("LOAD_MODE", "0")) if t0 > 0 else 0
            for half in range(2):
                if lmode == 1:
                    continue
                pt = self.PS()
                for j in range(4):
                    k = half * 4 + j
                    op("pe", "transpose", out=pt[:, j * 128:j * 128 + nt], in_=xt[0:nt, k * 128:(k + 1) * 128],
                       identity=self.ident[0:nt, 0:nt])
                if lmode == 2:
                    continue
                for j in range(4):
                    k = half * 4 + j
                    dstv = self.h[k][:, t0:t0 + nt]
                    if lmode == 3:
                        dstv = self.tmp[k % 6][:, 0:nt]
                    if lmode == 4:
                        dstv = self.h[k][:, 0:nt]
                    if lmode == 5 and j % 2 == 1:
                        continue
                    if lmode == 6 and j % 2 == 0:
                        continue
                    use_dve = (half == 0)
                    if use_dve:
                        op("dve", "tensor_copy", out=dstv, in_=pt[:, j * 128:j * 128 + nt])
                    else:
                        op("act", "copy", out=dstv, in_=pt[:, j * 128:j * 128 + nt])
            t0 += nt

    def store_tokens(self, src, pos, n, f32view=False):
        P = self.P
        op = P.op
        import os
        t0 = 0
        nlim = int(os.environ.get('STORE_TOK', '100000'))
        while t0 < min(n, nlim):
            nt = min(128, n - t0)
            i = self.io_i % 2
            self.io_i += 1
            ot = self.xt[i]
            for half in range(2):
                pt = self.PS()
                for j in range(4):
                    k = half * 4 + j
                    sv = src[k].f[:, t0:t0 + nt] if f32view else src[k][:, t0:t0 + nt]
                    op("pe", "transpose", out=pt[0:nt, j * 128:(j + 1) * 128], in_=sv, identity=self.ident[:, :])
                if half == 0:
                    op("dve", "tensor_copy", out=ot[0:nt, 0:512], in_=pt[0:nt, :])
                else:
                    op("act", "copy", out=ot[0:nt, 512:1024], in_=pt[0:nt, :])
            lo = pos + t0
            skip = max(0, NMETA - lo)
            if skip < nt:
                a = skip
                while a < nt:
                    bnd = min(nt, (a // 32 + 1) * 32) if a % 32 else nt
                    P.dma("act", self.dr["out"][lo + a - NMETA: lo + bnd - NMETA, :], ot[a:bnd, :], self.osem[i])
                    a = bnd
            t0 += nt

    def main_start(self):
        op = self.P.op
        g = self.flag[:, 1:2]
        op("dve", "tensor_scalar", out=self.lru_state[:, :], in0=self.lru_state[:, :], scalar1=g, scalar2=None, op0=ALU.mult)
        op("dve", "tensor_scalar", out=self.s5_state[:, :, :], in0=self.s5_state[:, :, :], scalar1=g, scalar2=None, op0=ALU.mult)
        for i in range(4):
            op("dve", "tensor_scalar", out=self.S[i][:, :], in0=self.S[i][:, :], scalar1=g, scalar2=None, op0=ALU.mult)
        pn = self.prev_n
        op("dve", "tensor_scalar", out=self.rec[:, :, pn:pn + 3], in0=self.rec[:, :, pn:pn + 3], scalar1=g, scalar2=None, op0=ALU.mult)

    def block(self, pos, mpos, chunks, hist, blend):
        n = sum(chunks)
        self.load_tokens(pos, n)
        if self.stage_stop >= 1:
            self.layer0_mixer(n, blend)
        if self.stage_stop >= 2:
            self.ffn0(n)
        if self.stage_stop >= 3:
            self.gla(n, chunks, hist, blend)
        if hist:
            return
        if self.stage_stop >= 4:
            self.moe(n)
        if not self.dump_h:
            self.rmsnorm(n, "final_norm", dst=self.h)
        self.store_tokens(self.h, mpos, n)

    def mm_out(self, n, lhs_list, rhs_list, ps=None):
        ps = ps or self.PS()
        nk = len(lhs_list)
        for k in range(nk):
            self.P.op("pe", "matmul", out=ps[:, 0:n], lhsT=lhs_list[k], rhs=rhs_list[k], start=(k == 0), stop=(k == nk - 1))
        return ps

    def layer0_mixer(self, n, blend=False):
        P = self.P
        op = P.op
        A, hn, h = self.A, self.hn, self.h
        self.rmsnorm(n, "ev_norm_mix")
        rec = self.rec
        if self.prev_n is not None:
            op("act", "copy", out=rec[:, :, 0:3], in_=rec[:, :, self.prev_n:self.prev_n + 3])
        self.prev_n = n
        rhs = [hn[k][:, 0:n] for k in range(8)]
        for sl in range(10):
            s, sv = self.kslab("ev_w_in", 0, 8, sl * 256, 256)
            for m in range(2):
                mc = sl * 2 + m
                ps = self.mm_out(n, [s.v(sv[:, k, m * 128:(m + 1) * 128]) for k in range(8)], rhs)
                if mc < 8:
                    op("act", "activation", out=A[mc][:, 0:n], in_=ps[:, 0:n], func=AF.Gelu)
                elif mc < 16:
                    op("dve", "tensor_copy", out=rec[:, mc - 8, 3:3 + n], in_=ps[:, 0:n])
                else:
                    op("dve", "tensor_copy", out=A[8 + mc - 16][:, 0:n], in_=ps[:, 0:n])
        cw, cb = self.convw, self.vec["ev_conv_b"]
        for k in range(KD):
            t = self.T()
            op("dve", "tensor_scalar", out=t[:, 0:n], in0=rec[:, k, 0:n], scalar1=cw[:, 0, k:k + 1], scalar2=cb[:, k:k + 1], op0=ALU.mult, op1=ALU.add)
            op("dve", "scalar_tensor_tensor", out=t[:, 0:n], in0=rec[:, k, 1:1 + n], scalar=cw[:, 1, k:k + 1], in1=t[:, 0:n], op0=ALU.mult, op1=ALU.add)
            op("dve", "scalar_tensor_tensor", out=t[:, 0:n], in0=rec[:, k, 2:2 + n], scalar=cw[:, 2, k:k + 1], in1=t[:, 0:n], op0=ALU.mult, op1=ALU.add)
            op("dve", "scalar_tensor_tensor", out=A[12 + k][:, 0:n], in0=rec[:, k, 3:3 + n], scalar=cw[:, 3, k:k + 1], in1=t[:, 0:n], op0=ALU.mult, op1=ALU.add)
        sa, sav = self.kslab("ev_gate_a_w", 0, 8, 0, 256)
        sx, sxv = self.kslab("ev_gate_x_w", 0, 8, 0, 256)
        ba, bx = self.vec["ev_gate_a_b"], self.vec["ev_gate_x_b"]
        for j in range(KD):
            hb = j // 2
            cols = slice((j % 2) * 128, (j % 2) * 128 + 128)
            xin = [A[12 + 2 * hb + kk][:, 0:n] for kk in range(2)]
            psa = self.mm_out(n, [sa.v(sav[:, 2 * hb + kk, cols]) for kk in range(2)], xin)
            psx = self.mm_out(n, [sx.v(sxv[:, 2 * hb + kk, cols]) for kk in range(2)], xin)
            r, a_t, s_t, i_t = self.T(), self.T(), self.T(), self.T()
            op("act", "activation", out=r[:, 0:n], in_=psa[:, 0:n], func=AF.Sigmoid, bias=ba[:, j:j + 1], scale=1.0)
            op("act", "activation", out=i_t[:, 0:n], in_=psx[:, 0:n], func=AF.Sigmoid, bias=bx[:, j:j + 1], scale=1.0)
            op("act", "activation", out=a_t[:, 0:n], in_=r[:, 0:n], func=AF.Exp, scale=self.nc8[:, j:j + 1])
            op("act", "activation", out=s_t[:, 0:n], in_=r[:, 0:n], func=AF.Exp, scale=self.nc16[:, j:j + 1])
            op("act", "activation", out=s_t[:, 0:n], in_=s_t[:, 0:n], func=AF.Sqrt, scale=-1.0, bias=self.oneb[:, 0:1])
            op("dve", "tensor_tensor", out=i_t[:, 0:n], in0=i_t[:, 0:n], in1=A[12 + j].f[:, 0:n], op=ALU.mult)
            op("dve", "tensor_tensor", out=i_t[:, 0:n], in0=i_t[:, 0:n], in1=s_t[:, 0:n], op=ALU.mult)
            ls = self.lru_state[:, j:j + 1]
            if blend:
                op("dve", "tensor_tensor_scan", out=r[:, 0:16], data0=a_t[:, 0:16], data1=i_t[:, 0:16],
                   initial=ls, op0=ALU.mult, op1=ALU.add)
                tb = self.SM()
                op("dve", "tensor_scalar", out=tb[:, 0:1], in0=r[:, 15:16], scalar1=self.flag[:, 0:1], scalar2=None, op0=ALU.mult)
                op("dve", "scalar_tensor_tensor", out=ls, in0=ls, scalar=self.flag[:, 1:2], in1=tb[:, 0:1], op0=ALU.mult, op1=ALU.add)
                op("dve", "tensor_tensor_scan", out=r[:, 16:n], data0=a_t[:, 16:n], data1=i_t[:, 16:n],
                   initial=ls, op0=ALU.mult, op1=ALU.add)
            else:
                op("dve", "tensor_tensor_scan", out=r[:, 0:n], data0=a_t[:, 0:n], data1=i_t[:, 0:n],
                   initial=ls, op0=ALU.mult, op1=ALU.add)
            op("act", "copy", out=self.lru_state[:, j:j + 1], in_=r[:, n - 1:n])
            op("dve", "tensor_tensor", out=A[j][:, 0:n], in0=A[j].f[:, 0:n], in1=r[:, 0:n], op=ALU.mult)
        self.s5(n, blend)
        mixr = [A[k][:, 0:n] for k in range(8)] + [A[20 + k][:, 0:n] for k in range(4)]
        for cg in range(4):
            pss = [self.PS(), self.PS()]
            for part in range(2):
                s, sv = self.kslab("ev_w_out", part * 6, 6, cg * 256, 256)
                for mm in range(2):
                    for k in range(6):
                        kk = part * 6 + k
                        op("pe", "matmul", out=pss[mm][:, 0:n], lhsT=s.v(sv[:, k, mm * 128:(mm + 1) * 128]), rhs=mixr[kk],
                           start=(kk == 0), stop=(kk == 11))
            for mm in range(2):
                m = cg * 2 + mm
                op("dve", "tensor_tensor", out=h[m][:, 0:n], in0=pss[mm][:, 0:n], in1=h[m][:, 0:n], op=ALU.add)

    def s5(self, n, blend=False):
        P = self.P
        op = P.op
        A = self.A
        lp = self.lam_pw
        nlev = 0
        while (1 << nlev) < n:
            nlev += 1
        yps = self.ps[0:4]
        saved_pool = self.pool
        self.pool = [4, 5]
        st = self.s5_state
        for pi in range(16):
            c, r = pi // 4, pi % 4
            prow = slice(32 * r, 32 * r + 32)
            za = [self.T(), self.T()]
            zb = [self.T(), self.T()]
            for j in range(2):
                psx = self.PS()
                if r < 3:
                    op("pe", "matmul", out=psx[:, 0:n], lhsT=self.Bc[prow, c, j, :], rhs=A[8 + c][prow, 0:n], start=True, stop=True)
                else:
                    op("pe", "matmul", out=psx[:, 0:n], lhsT=self.Bc3[64:128, c, j, :], rhs=A[8 + c][64:128, 0:n], start=True, stop=True)
                op("act", "copy", out=za[j][:, 0:n], in_=psx[:, 0:n])
            def seg(c0, c1):
                op("dve", "scalar_tensor_tensor", out=za[0][:, c0:c0 + 1], in0=st[:, pi, 0:1], scalar=lp[:, pi, 0, 0:1], in1=za[0][:, c0:c0 + 1], op0=ALU.mult, op1=ALU.add)
                op("dve", "scalar_tensor_tensor", out=za[0][:, c0:c0 + 1], in0=st[:, pi, 1:2], scalar=lp[:, pi, 0, 2:3], in1=za[0][:, c0:c0 + 1], op0=ALU.mult, op1=ALU.add)
                op("dve", "scalar_tensor_tensor", out=za[1][:, c0:c0 + 1], in0=st[:, pi, 1:2], scalar=lp[:, pi, 0, 0:1], in1=za[1][:, c0:c0 + 1], op0=ALU.mult, op1=ALU.add)
                op("dve", "scalar_tensor_tensor", out=za[1][:, c0:c0 + 1], in0=st[:, pi, 0:1], scalar=lp[:, pi, 0, 1:2], in1=za[1][:, c0:c0 + 1], op0=ALU.mult, op1=ALU.add)
                src, dst = za, zb
                lv = 0
                while (1 << lv) < (c1 - c0):
                    d = 1 << lv
                    pr, pim, npi = lp[:, pi, lv, 0:1], lp[:, pi, lv, 1:2], lp[:, pi, lv, 2:3]
                    op("act", "copy", out=dst[0][:, c0:c0 + d], in_=src[0][:, c0:c0 + d])
                    op("act", "copy", out=dst[1][:, c0:c0 + d], in_=src[1][:, c0:c0 + d])
                    op("dve", "scalar_tensor_tensor", out=dst[0][:, c0 + d:c1], in0=src[0][:, c0:c1 - d], scalar=pr, in1=src[0][:, c0 + d:c1], op0=ALU.mult, op1=ALU.add)
                    op("dve", "scalar_tensor_tensor", out=dst[0][:, c0 + d:c1], in0=src[1][:, c0:c1 - d], scalar=npi, in1=dst[0][:, c0 + d:c1], op0=ALU.mult, op1=ALU.add)
                    op("dve", "scalar_tensor_tensor", out=dst[1][:, c0 + d:c1], in0=src[1][:, c0:c1 - d], scalar=pr, in1=src[1][:, c0 + d:c1], op0=ALU.mult, op1=ALU.add)
                    op("dve", "scalar_tensor_tensor", out=dst[1][:, c0 + d:c1], in0=src[0][:, c0:c1 - d], scalar=pim, in1=dst[1][:, c0 + d:c1], op0=ALU.mult, op1=ALU.add)
                    src, dst = dst, src
                    lv += 1
                return src
            if blend:
                sa = seg(0, 16)
                for j in range(2):
                    tb = self.SM()
                    op("dve", "tensor_scalar", out=tb[:, 0:1], in0=sa[j][:, 15:16], scalar1=self.flag[:, 0:1], scalar2=None, op0=ALU.mult)
                    op("dve", "scalar_tensor_tensor", out=st[:, pi, j:j + 1], in0=st[:, pi, j:j + 1], scalar=self.flag[:, 1:2], in1=tb[:, 0:1], op0=ALU.mult, op1=ALU.add)
                src = seg(16, n)
                if sa is not src:
                    for j in range(2):
                        op("act", "copy", out=src[j][:, 0:16], in_=sa[j][:, 0:16])
            else:
                src = seg(0, n)
            op("act", "copy", out=st[:, pi, 0:1], in_=src[0][:, n - 1:n])
            op("act", "copy", out=st[:, pi, 1:2], in_=src[1][:, n - 1:n])
            orow = slice(64 * (r // 2), 64 * (r // 2) + 64)
            for j in range(2):
                op("pe", "matmul", out=yps[c][orow, 0:n], lhsT=self.Cc[:, pi, j, :], rhs=src[j][:, 0:n],
                   start=(j == 0 and r % 2 == 0), stop=(j == 1 and r % 2 == 1))
        self.pool = saved_pool
        dv = self.vec5["ev_ssm_d"]
        for c in range(4):
            t = self.T()
            op("dve", "scalar_tensor_tensor", out=t[:, 0:n], in0=A[8 + c].f[:, 0:n], scalar=dv[:, c:c + 1], in1=yps[c][:, 0:n], op0=ALU.mult, op1=ALU.add)
            op("act", "activation", out=A[24 + c][:, 0:n], in_=t[:, 0:n], func=AF.Gelu)
        s, sv = self.kslab("ev_ssm_glu_w", 0, 4, 0, 512)
        gb = self.vec5["ev_ssm_glu_b"]
        for m in range(4):
            ps = self.mm_out(n, [s.v(sv[:, k, m * 128:(m + 1) * 128]) for k in range(4)], [A[24 + k][:, 0:n] for k in range(4)])
            t = self.T()
            op("act", "activation", out=t[:, 0:n], in_=ps[:, 0:n], func=AF.Sigmoid, bias=gb[:, m:m + 1], scale=1.0)
            op("dve", "tensor_tensor", out=A[20 + m][:, 0:n], in0=A[24 + m].f[:, 0:n], in1=t[:, 0:n], op=ALU.mult)

    def swiglu_dense(self, n, wg, wu, wd, nf, nparts, row0_gu=0, row0_d=0, gate_bc=None):
        P = self.P
        op = P.op
        A, hn, h = self.A, self.hn, self.h
        rhs = [hn[k][:, 0:n] for k in range(8)]
        for f0 in range(0, nf, 2):
            sg, sgv = self.kslab(wg, 0, 8, f0 * 128, 256, row0=row0_gu)
            su, suv = self.kslab(wu, 0, 8, f0 * 128, 256, row0=row0_gu)
            for m in range(2):
                f = f0 + m
                pg = self.mm_out(n, [sg.v(sgv[:, k, m * 128:(m + 1) * 128]) for k in range(8)], rhs)
                pu = self.mm_out(n, [su.v(suv[:, k, m * 128:(m + 1) * 128]) for k in range(8)], rhs)
                t = self.T()
                op("act", "activation", out=t[:, 0:n], in_=pg[:, 0:n], func=AF.Silu)
                op("dve", "tensor_tensor", out=A[f][:, 0:n], in0=pu[:, 0:n], in1=t[:, 0:n], op=ALU.mult)
        kp = nf // nparts
        for cg in range(4):
            pss = [self.PS(), self.PS()]
            for part in range(nparts):
                s, sv = self.kslab(wd, part * kp, kp, cg * 256, 256, row0=row0_d)
                for mm in range(2):
                    for k in range(kp):
                        f = part * kp + k
                        op("pe", "matmul", out=pss[mm][:, 0:n], lhsT=s.v(sv[:, k, mm * 128:(mm + 1) * 128]), rhs=A[f][:, 0:n],
                           start=(f == 0), stop=(f == nf - 1))
            for mm in range(2):
                m = cg * 2 + mm
                if gate_bc is None:
                    op("dve", "tensor_tensor", out=h[m][:, 0:n], in0=pss[mm][:, 0:n], in1=h[m][:, 0:n], op=ALU.add)
                else:
                    t = self.T()
                    op("dve", "tensor_tensor", out=t[:, 0:n], in0=pss[mm][:, 0:n], in1=gate_bc[:, 0:n], op=ALU.mult)
                    op("dve", "tensor_tensor", out=h[m][:, 0:n], in0=t[:, 0:n], in1=h[m][:, 0:n], op=ALU.add)

    def ffn0(self, n):
        self.rmsnorm(n, "ev_norm_ffn")
        self.swiglu_dense(n, "ev_ffn_w_gate", "ev_ffn_w_up", "ev_ffn_w_down", 24, 3)

    def gla(self, n, chunks, hist=False, blend=False):
        P = self.P
        op = P.op
        A, hn, h = self.A, self.hn, self.h
        self.rmsnorm(n, "od_norm_mix")
        rhs = [hn[k][:, 0:n] for k in range(8)]
        s, sv = self.kslab("od_w_in", 0, 8, 3072, 16)
        psg = self.PSM()
        for k in range(8):
            op("pe", "matmul", out=psg[0:16, 0:n], lhsT=s.v(sv[:, k, :]), rhs=rhs[k], start=(k == 0), stop=(k == 7))
        glr = A[27]
        op("dve", "tensor_copy", out=glr[0:16, 0:n], in_=psg[0:16, 0:n])
        gbv = self.vec5["od_gla_gate_b"]
        hnorm = self.vec["od_gla_norm"]
        rm0 = 0 if chunks[0] == 16 else 16
        saved_pool = self.pool
        for hd in range(4):
            po = [self.ps[(hd % 2) * 2], self.ps[(hd % 2) * 2 + 1]]
            self.pool = [4, 5] + ([2, 3] if hd % 2 == 0 else [0, 1])
            sqk, sem = self.next_slot()
            sqkv = sqk.t[:, 0:2048].rearrange("p (k c) -> p k c", k=8)
            w = self.dr["od_w_in"]
            if not hist:
                P.dma("sp", sqk.v(sqkv[:, :, 0:128]), w[:, hd * 128:hd * 128 + 128].rearrange("(k p) c -> p k c", p=128), sem)
            P.dma("sp", sqk.v(sqkv[:, :, 128:256]), w[:, 512 + hd * 128:512 + hd * 128 + 128].rearrange("(k p) c -> p k c", p=128), sem)
            svv, svvv = self.kslab("od_w_in", 0, 8, 1024 + hd * 256, 256)
            pq = None if hist else self.mm_out(n, [sqk.v(sqkv[:, k, 0:128]) for k in range(8)], rhs)
            pk = self.mm_out(n, [sqk.v(sqkv[:, k, 128:256]) for k in range(8)], rhs)
            pl = self.PS()
            op("pe", "matmul", out=pl[:, 0:n], lhsT=self.w2[:, hd * 128:(hd + 1) * 128], rhs=glr[0:16, 0:n], start=True, stop=True)
            xs, ax, mn, eb = self.T(), self.T(), self.T(), self.T()
            op("dve", "tensor_scalar", out=xs[:, 0:n], in0=pl[:, 0:n], scalar1=gbv[:, hd:hd + 1], scalar2=None, op0=ALU.add)
            op("act", "activation", out=ax[:, 0:n], in_=xs[:, 0:n], func=AF.Abs)
            op("act", "activation", out=ax[:, 0:n], in_=ax[:, 0:n], func=AF.Exp, scale=-1.0)
            op("act", "activation", out=ax[:, 0:n], in_=ax[:, 0:n], func=AF.Ln, bias=self.oneb[:, 0:1], scale=1.0)
            op("dve", "tensor_scalar", out=mn[:, 0:n], in0=xs[:, 0:n], scalar1=0.0, scalar2=None, op0=ALU.min)
            op("dve", "tensor_tensor", out=mn[:, 0:n], in0=mn[:, 0:n], in1=ax[:, 0:n], op=ALU.subtract)
            b16 = xs
            op("dve", "tensor_tensor_scan", out=b16[:, 0:n], data0=self.rmask[:, rm0:rm0 + n], data1=mn[:, 0:n], initial=0.0, op0=ALU.mult, op1=ALU.add)
            enb = ax
            op("act", "activation", out=eb[:, 0:n], in_=b16[:, 0:n], func=AF.Exp, scale=1.0 / 16.0)
            op("act", "activation", out=enb[:, 0:n], in_=b16[:, 0:n], func=AF.Exp, scale=-1.0 / 16.0)
            qt, kt = self.qt, self.kt
            if not hist:
                op("dve", "scalar_tensor_tensor", out=qt[:, 0:n], in0=pq[:, 0:n], scalar=128.0 ** -0.5, in1=eb[:, 0:n], op0=ALU.mult, op1=ALU.mult)
            op("dve", "tensor_tensor", out=kt[:, 0:n], in0=pk[:, 0:n], in1=enb[:, 0:n], op=ALU.mult)
            vt = self.vt
            c0 = 0
            for ci, cs in enumerate(chunks):
                pv = self.PS()
                for k in range(8):
                    op("pe", "matmul", out=pv[0:cs, 0:256], lhsT=hn[k][:, c0:c0 + cs], rhs=svv.v(svvv[:, k, :]), start=(k == 0), stop=(k == 7))
                op("act", "copy", out=vt[0:cs, ci, :], in_=pv[0:cs, 0:256])
                c0 += cs
            S = self.S[hd]
            Srab, ktT, S2 = self.Srab, self.ktT, self.S2
            c0 = 0
            for ci, cs in enumerate(chunks):
                pt = self.PS()
                op("pe", "transpose", out=pt[0:cs, 0:128], in_=kt.f[:, c0:c0 + cs], identity=self.ident[:, :])
                op("act", "copy", out=ktT[0:cs, ci, :], in_=pt[0:cs, 0:128])
                c0 += cs
            cur = 0
            op("dve", "tensor_copy", out=Srab[0][:, :], in_=S[:, :])
            c0 = 0
            for ci, cs in enumerate(chunks):
                cl = slice(c0, c0 + cs)
                last = eb[:, c0 + cs - 1:c0 + cs]
                if not hist:
                    pa = self.PS()
                    op("pe", "matmul", out=pa[0:cs, 0:cs], lhsT=kt[:, cl], rhs=qt[:, cl], start=True, stop=True)
                    att = self.att2[ci % 2]
                    op("dve", "tensor_tensor", out=att[0:cs, 0:cs], in0=pa[0:cs, 0:cs], in1=self.triu[0:cs, 0:cs], op=ALU.mult)
                    for vc in range(2):
                        op("pe", "matmul", out=po[vc][:, cl], lhsT=vt[0:cs, ci, vc * 128:(vc + 1) * 128], rhs=att[0:cs, 0:cs], start=True, stop=False)
                        op("pe", "matmul", out=po[vc][:, cl], lhsT=Srab[cur][:, vc * 128:(vc + 1) * 128], rhs=qt[:, cl], start=False, stop=True)
                pS = self.PS()
                op("pe", "matmul", out=pS[:, 0:256], lhsT=ktT[0:cs, ci, :], rhs=vt[0:cs, ci, :], start=True, stop=True)
                op("dve", "tensor_tensor", out=S2[:, :], in0=pS[:, 0:256], in1=S[:, :], op=ALU.add)
                if blend and ci == 0:
                    op("dve", "tensor_scalar", out=S2[:, :], in0=S2[:, :], scalar1=last, scalar2=self.flag[:, 0:1], op0=ALU.mult, op1=ALU.mult)
                    op("dve", "scalar_tensor_tensor", out=S[:, :], in0=S[:, :], scalar=self.flag[:, 1:2], in1=S2[:, :], op0=ALU.mult, op1=ALU.add)
                else:
                    op("dve", "tensor_scalar", out=S[:, :], in0=S2[:, :], scalar1=last, scalar2=None, op0=ALU.mult)
                cur ^= 1
                op("dve", "tensor_copy", out=Srab[cur][:, :], in_=S[:, :])
                c0 += cs
            if hist:
                continue
            for vc in range(2):
                op("act", "activation", out=A[20 + vc][:, 0:n], in_=po[vc][:, 0:n], func=AF.Square)
            pss = self.PSM()
            for vc in range(2):
                op("pe", "matmul", out=pss[:, 0:n], lhsT=self.onesr[:, :], rhs=A[20 + vc][:, 0:n], start=(vc == 0), stop=(vc == 1))
            rs = self.T()
            op("act", "activation", out=rs[:, 0:n], in_=pss[:, 0:n], func=AF.Sqrt, scale=1.0 / 256.0, bias=self.epsb[:, 0:1])
            op("dve", "reciprocal", out=rs[:, 0:n], in_=rs[:, 0:n])
            sg, sgv = self.kslab("od_w_in", 0, 8, 2048 + hd * 256, 256)
            for vc in range(2):
                m = hd * 2 + vc
                on = self.T()
                op("dve", "scalar_tensor_tensor", out=on[:, 0:n], in0=po[vc][:, 0:n], scalar=hnorm[:, m:m + 1], in1=rs[:, 0:n], op0=ALU.mult, op1=ALU.mult)
                pg = self.mm_out(n, [sg.v(sgv[:, k, vc * 128:(vc + 1) * 128]) for k in range(8)], rhs)
                sgt = self.T()
                op("act", "activation", out=sgt[:, 0:n], in_=pg[:, 0:n], func=AF.Silu)
                op("dve", "tensor_tensor", out=A[m][:, 0:n], in0=on[:, 0:n], in1=sgt[:, 0:n], op=ALU.mult)
        self.pool = saved_pool
        if hist:
            return
        for cg in range(4):
            s, sv = self.kslab("od_w_out", 0, 8, cg * 256, 256)
            for mm in range(2):
                m = cg * 2 + mm
                ps = self.mm_out(n, [s.v(sv[:, k, mm * 128:(mm + 1) * 128]) for k in range(8)], [A[k][:, 0:n] for k in range(8)])
                op("dve", "tensor_tensor", out=h[m][:, 0:n], in0=ps[:, 0:n], in1=h[m][:, 0:n], op=ALU.add)

    def moe(self, n):
        P = self.P
        op = P.op
        hn = self.hn
        rstd = self.rmsnorm(n, "od_norm_ffn")
        pl = self.PSM()
        for k in range(8):
            op("pe", "matmul", out=pl[0:8, 0:n], lhsT=self.rwg[:, k, :], rhs=self.h[k][:, 0:n], start=(k == 0), stop=(k == 7))
        lfm, gfm = self.T(), self.T()
        op("dve", "memset", ap=lfm[0:64, 0:n], constant=0.0)
        op("dve", "tensor_copy", out=lfm[0:8, 0:n], in_=pl[0:8, 0:n])
        op("dve", "tensor_copy", out=lfm[32:33, 0:n], in_=rstd[32:33, 0:n])
        t0 = 0
        while t0 < n:
            nt = min(128, n - t0)
            pt = self.PSM()
            op("pe", "transpose", out=pt[0:nt, 0:33], in_=lfm[0:33, t0:t0 + nt], identity=self.ident[0:33, 0:33])
            lt, mx, g1, g2 = self.SM(), self.SM(), self.SM(), self.SM()
            op("dve", "tensor_copy", out=mx[0:nt, 12:13], in_=pt[0:nt, 32:33])
            op("dve", "tensor_scalar", out=lt[0:nt, 0:8], in0=pt[0:nt, 0:8], scalar1=mx[0:nt, 12:13], scalar2=None, op0=ALU.mult)
            op("dve", "max", out=mx[0:nt, 0:8], in_=lt[0:nt, 0:8])
            op("dve", "tensor_tensor", out=mx[0:nt, 8:9], in0=mx[0:nt, 1:2], in1=mx[0:nt, 0:1], op=ALU.subtract)
            op("act", "activation", out=mx[0:nt, 8:9], in_=mx[0:nt, 8:9], func=AF.Exp)
            op("dve", "tensor_scalar", out=mx[0:nt, 9:10], in0=mx[0:nt, 8:9], scalar1=1.0, scalar2=None, op0=ALU.add)
            op("dve", "reciprocal", out=mx[0:nt, 9:10], in_=mx[0:nt, 9:10])
            op("dve", "tensor_tensor", out=mx[0:nt, 10:11], in0=mx[0:nt, 8:9], in1=mx[0:nt, 9:10], op=ALU.mult)
            op("dve", "tensor_scalar", out=g1[0:nt, 0:8], in0=lt[0:nt, 0:8], scalar1=mx[0:nt, 0:1], scalar2=mx[0:nt, 9:10], op0=ALU.is_equal, op1=ALU.mult)
            op("dve", "tensor_scalar", out=g2[0:nt, 0:8], in0=lt[0:nt, 0:8], scalar1=mx[0:nt, 1:2], scalar2=mx[0:nt, 10:11], op0=ALU.is_equal, op1=ALU.mult)
            op("dve", "tensor_tensor", out=g1[0:nt, 0:8], in0=g1[0:nt, 0:8], in1=g2[0:nt, 0:8], op=ALU.add)
            pt2 = self.PSM()
            op("pe", "transpose", out=pt2[0:8, 0:nt], in_=g1[0:nt, 0:8], identity=self.ident[0:nt, 0:nt])
            op("dve", "tensor_copy", out=gfm[0:8, t0:t0 + nt], in_=pt2[0:8, 0:nt])
            t0 += nt
        gkeep = self.gkeep
        op("dve", "tensor_copy", out=gkeep[0:8, 0:n], in_=gfm[0:8, 0:n])
        for e in range(NEXP):
            gm = self.T()
            op("dve", "tensor_scalar", out=gm[0:8, 0:n], in0=gkeep[0:8, 0:n], scalar1=self.ident[0:8, e:e + 1], scalar2=None, op0=ALU.mult)
            pb = self.PSM()
            op("pe", "matmul", out=pb[:, 0:n], lhsT=self.ones8[:, :], rhs=gm[0:8, 0:n], start=True, stop=True)
            gbc = self.gbc
            op("act", "copy", out=gbc[:, 0:n], in_=pb[:, 0:n])
            self.swiglu_dense(n, "od_moe_w_gate", "od_moe_w_up", "od_moe_w_down", 28, 4,
                              row0_gu=e * 1024, row0_d=e * DFE, gate_bc=gbc)


def make_consts():
    ident = np.eye(128, dtype=np.float32)
    ones = np.ones((128, 128), np.float32)
    p = np.arange(128)
    maskpar = np.stack([((p // 16) % 2 == 0), ((p // 16) % 2 == 1), (p >= 96)], axis=1).astype(np.float32)
    s = np.arange(64)
    triu = (s[:, None] <= s[None, :]).astype(np.float32)
    rm = np.ones((128, 528), np.float32)
    rm[:, 0] = 0
    for c in range(16, 528, 64):
        rm[:, c] = 0
    return {"c_ident": ident, "c_onesr": ones, "c_maskpar": maskpar, "c_triu": triu, "c_rmask": rm}


def make_weight_map(inp):
    f = lambda a: np.ascontiguousarray(np.asarray(a, dtype=np.float32))
    m = {}
    m["ev_w_in"] = f(inp["ev_w_in"][0])
    m["ev_gate_a_w"] = f(inp["ev_gate_a_w"][0].reshape(1024, 256))
    m["ev_gate_x_w"] = f(inp["ev_gate_x_w"][0].reshape(1024, 256))
    m["ev_ssm_glu_w"] = f(inp["ev_ssm_glu_w"][0])
    m["ev_w_out"] = f(inp["ev_w_out"][0])
    m["ev_ffn_w_gate"] = f(inp["ev_ffn_w_gate"][0])
    m["ev_ffn_w_up"] = f(inp["ev_ffn_w_up"][0])
    m["ev_ffn_w_down"] = f(inp["ev_ffn_w_down"][0])
    m["od_w_in"] = f(inp["od_w_in"][0])
    m["od_gla_gate_w2"] = f(inp["od_gla_gate_w2"][0])
    m["od_w_out"] = f(inp["od_w_out"][0])
    m["od_moe_w_gate"] = f(inp["od_moe_w_gate"][0].reshape(8 * 1024, 3584))
    m["od_moe_w_up"] = f(inp["od_moe_w_up"][0].reshape(8 * 1024, 3584))
    m["od_moe_w_down"] = f(inp["od_moe_w_down"][0].reshape(8 * 3584, 1024))
    for n in VEC1024:
        m[n] = f(inp[n]).reshape(1024)
    for n in VEC512:
        m[n] = f(inp[n]).reshape(512)
    m["ev_conv_w"] = f(inp["ev_conv_w"][0])
    m["od_router_w"] = f(inp["od_router_w"][0])
    for n in ("ev_ssm_lambda_re", "ev_ssm_lambda_im", "ev_ssm_b_re", "ev_ssm_b_im", "ev_ssm_c_re", "ev_ssm_c_im"):
        m[n] = f(inp[n][0])
    m["ev_ssm_log_dt"] = f(inp["ev_ssm_log_dt"][0])
    m.update(make_consts())
    return m


_CACHE = {}


def kernel(**inputs):
    x = np.asarray(inputs["x"], dtype=np.float32)
    meta = np.asarray(inputs["meta_tokens"], dtype=np.float32)
    nchunks = SEQ // 2 // 64
    half = SEQ // 2
    if "k" not in _CACHE:
        _CACHE["k"] = K(nchunks)
    k = _CACHE["k"]
    wm = make_weight_map(inputs)
    in_maps = []
    npass = NMETA + half
    for c in range(8):
        b, hf = c // 2, c % 2
        m = dict(wm)
        first = np.concatenate([meta, x[b, :half]], axis=0)
        if hf == 0:
            tok = np.concatenate([np.zeros((npass, D), np.float32), first], axis=0)
            flag = np.tile(np.array([[1.0, 0.0]], np.float32), (128, 1))
        else:
            tok = np.concatenate([first, x[b, half - NMETA:half], x[b, half:]], axis=0)
            flag = np.tile(np.array([[0.0, 1.0]], np.float32), (128, 1))
        m["tok"] = np.ascontiguousarray(tok)
        m["c_flag"] = flag
        in_maps.append(m)
    res = run_bass_kernel_spmd(k.nc, in_maps, core_ids=list(range(8)))
    out = np.empty((BATCH, SEQ, D), np.float32)
    for c in range(8):
        b, hf = c // 2, c % 2
        out[b, hf * half:(hf + 1) * half] = res.results[c]["out"]
    return out
```

```python
from contextlib import ExitStack
import math
import numpy as np
import concourse.bass as bass
import concourse.mybir as mybir
from concourse.bass_utils import run_bass_kernel_spmd

F32 = mybir.dt.float32
F32R = mybir.dt.float32r
I32 = mybir.dt.int32
AF = mybir.ActivationFunctionType
ALU = mybir.AluOpType
OUTKEYS = ("out", "accum_out", "ap")

D = 1024
KD = 8
NMETA = 16
SEQ = 8192
BATCH = 4
EPS = 1e-6
NEXP = 8
NSLOT = 5
RELAX_DVE_OWN = True
DFE = 3584
DFF = 3072


class Sem:
    def __init__(self, P, name):
        self.h = P.ctx.enter_context(P.nc.semaphore(name))
        self.val = 0


class V:
    __slots__ = ("buf", "ap")

    def __init__(self, buf, ap):
        self.buf = buf
        self.ap = ap


class Buf:
    def __init__(self, P, name, shape, dtype, space="sbuf", alias=None):
        self.psum = (space == "psum")
        if alias is not None:
            self.t = alias
        elif space == "sbuf":
            self.t = P.ctx.enter_context(P.nc.sbuf_tensor(name, list(shape), dtype))
        else:
            self.t = P.ctx.enter_context(P.nc.psum_tensor(name, list(shape), dtype))
        self.lastw = None
        self.reads = []
        self.small_tk = set()

    @property
    def f(self):
        return _F32View(self)

    def __getitem__(self, idx):
        return V(self, self.t[idx])

    def v(self, ap):
        return V(self, ap)


class _F32View:
    def __init__(self, b):
        self.b = b

    def __getitem__(self, idx):
        return V(self.b, self.b.t[idx].bitcast(F32))


class Eng:
    def __init__(self, P, name):
        self.sem = Sem(P, "s_" + name)
        self.seen = {}
        self.q = []


class Prog:
    def __init__(self, nc):
        self.nc = nc
        self.ctx = ExitStack()
        self.E = {n: Eng(self, n) for n in ("pe", "dve", "act", "pool", "sp")}

    def buf(self, name, shape, dtype=F32, space="sbuf", alias=None):
        return Buf(self, name, shape, dtype, space, alias)

    def _waits(self, E, R, W, skip_own, relax_own=False):
        waits = {}
        own_needed = [False]

        def need(tk, buf=None):
            if tk is None:
                return
            if tk[0] is E.sem and relax_own and buf is not None and tk in buf.small_tk:
                own_needed[0] = True
            if waits.get(tk[0], 0) < tk[1]:
                waits[tk[0]] = tk[1]

        for v in R:
            need(v.buf.lastw, v.buf)
            if v.buf.psum:
                for t in v.buf.reads:
                    if t[0] is not E.sem:
                        need(t)
        for v in W:
            need(v.buf.lastw, v.buf)
            for t in v.buf.reads:
                need(t, v.buf)
        wl = []
        for s, val in waits.items():
            if s is E.sem:
                if skip_own:
                    continue
                if relax_own and not own_needed[0]:
                    continue
            if E.seen.get(s, 0) < val:
                E.seen[s] = val
                wl.append((s, val))
        return wl

    def op(self, eng, meth, *args, writes=(), reads=(), **kw):
        E = self.E[eng]
        W = [v for k, v in kw.items() if isinstance(v, V) and k in OUTKEYS] + list(writes)
        R = [v for k, v in kw.items() if isinstance(v, V) and k not in OUTKEYS] + list(reads)

        def fsize(v):
            n = 1
            for st_, cnt in list(v.ap.ap)[1:]:
                n *= cnt
            return n
        SCAL = ("scalar", "scalar1", "scalar2", "bias", "scale", "initial")
        mainv = [v for k, v in kw.items() if isinstance(v, V) and k not in SCAL] + list(writes) + list(reads)
        small = any(fsize(v) < 128 for v in mainv)
        relax = (eng == "dve") and RELAX_DVE_OWN and not small
        wl = self._waits(E, R, W, eng == "pe", relax_own=relax)
        E.sem.val += 1
        tk = (E.sem, E.sem.val)
        kw2 = {k: (v.ap if isinstance(v, V) else v) for k, v in kw.items()}
        E.q.append((meth, args, kw2, wl, (E.sem, 1)))
        for v in R:
            v.buf.reads.append(tk)
            if small:
                v.buf.small_tk.add(tk)
        for v in W:
            v.buf.lastw = tk
            v.buf.reads = []
            v.buf.small_tk = {tk} if small else set()
        return tk

    def dma(self, eng, out, in_, sem, **kw):
        E = self.E[eng]
        W = [out] if isinstance(out, V) else []
        R = [in_] if isinstance(in_, V) else []
        wl = self._waits(E, R, W, False)
        sem.val += 16
        tk = (sem, sem.val)
        kw2 = dict(kw)
        kw2["out"] = out.ap if isinstance(out, V) else out
        kw2["in_"] = in_.ap if isinstance(in_, V) else in_
        E.q.append(("dma_start", (), kw2, wl, (sem, 16)))
        for v in R:
            v.buf.reads.append(tk)
        for v in W:
            v.buf.lastw = tk
            v.buf.reads = []
        return tk

    def wait(self, eng, tk):
        E = self.E[eng]
        if E.seen.get(tk[0], 0) < tk[1]:
            E.seen[tk[0]] = tk[1]
            E.q.append((None, (), {}, [tk], None))

    def build(self):
        nc = self.nc
        with nc.Block() as block:
            def mk(E):
                def body(e):
                    for meth, args, kw, wl, inc in E.q:
                        for s, val in wl:
                            e.wait_ge(s.h, val)
                        if meth is None:
                            continue
                        ins = getattr(e, meth)(*args, **kw)
                        if inc is not None:
                            ins.then_inc(inc[0].h, inc[1])
                return body
            for name, reg in (("pe", block.tensor), ("dve", block.vector), ("act", block.scalar),
                              ("pool", block.gpsimd), ("sp", block.sync)):
                if self.E[name].q:
                    reg(mk(self.E[name]))
        self.ctx.close()


BIGW = {
    "ev_w_in": (1024, 2560), "ev_gate_a_w": (1024, 256), "ev_gate_x_w": (1024, 256),
    "ev_ssm_glu_w": (512, 512), "ev_w_out": (1536, 1024),
    "ev_ffn_w_gate": (1024, 3072), "ev_ffn_w_up": (1024, 3072), "ev_ffn_w_down": (3072, 1024),
    "od_w_in": (1024, 3088), "od_gla_gate_w2": (16, 512), "od_w_out": (1024, 1024),
    "od_moe_w_gate": (8 * 1024, 3584), "od_moe_w_up": (8 * 1024, 3584), "od_moe_w_down": (8 * 3584, 1024),
}
VEC1024 = ["ev_norm_mix", "ev_conv_b", "ev_gate_a_b", "ev_gate_x_b", "ev_lru_lambda", "ev_norm_ffn",
           "od_norm_mix", "od_gla_norm", "od_norm_ffn", "final_norm"]
VEC512 = ["ev_ssm_d", "ev_ssm_glu_b", "od_gla_gate_b"]


def make_blocks(nch):
    first = min(7, nch)
    blocks = [[16] + [64] * first]
    rem = nch - first
    a = rem // 8
    while a >= 0 and (rem - 8 * a) % 7 != 0:
        a -= 1
    if a < 0:
        a, b7 = 0, 0
        blocks += [[64] * 7] * (rem // 7)
        if rem % 7:
            blocks.append([64] * (rem % 7))
        return blocks
    b7 = (rem - 8 * a) // 7
    blocks += [[64] * 8] * a + [[64] * 7] * b7
    return blocks


class K:
    def __init__(self, nchunks, stage_stop=99, dump_h=False):
        self.nchunks = nchunks
        self.stage_stop = stage_stop
        self.dump_h = dump_h
        self.blocks = make_blocks(nchunks)
        self.ntok = 2 * (NMETA + 64 * nchunks)
        self.npass = NMETA + 64 * nchunks
        nc = bass.Bass("TRN2", target_bir_lowering=False)
        nc.dge_precook = False
        self.nc = nc
        self.dr = {}
        dr = self.dr
        dr["tok"] = nc.dram_tensor("tok", [self.ntok, D], F32, kind="ExternalInput").ap()
        import os
        for n, s in BIGW.items():
            if os.environ.get("NOBIGW"):
                continue
            dr[n] = nc.dram_tensor(n, list(s), F32R, kind="ExternalInput").ap()
        for n in VEC1024:
            dr[n] = nc.dram_tensor(n, [1024], F32, kind="ExternalInput").ap()
        for n in VEC512:
            dr[n] = nc.dram_tensor(n, [512], F32, kind="ExternalInput").ap()
        dr["ev_conv_w"] = nc.dram_tensor("ev_conv_w", [4, 1024], F32, kind="ExternalInput").ap()
        dr["od_router_w"] = nc.dram_tensor("od_router_w", [1024, 8], F32, kind="ExternalInput").ap()
        for n in ("ev_ssm_lambda_re", "ev_ssm_lambda_im"):
            dr[n] = nc.dram_tensor(n, [32, 64], F32, kind="ExternalInput").ap()
        dr["ev_ssm_log_dt"] = nc.dram_tensor("ev_ssm_log_dt", [32], F32, kind="ExternalInput").ap()
        for n in ("ev_ssm_b_re", "ev_ssm_b_im"):
            dr[n] = nc.dram_tensor(n, [32, 64, 16], F32, kind="ExternalInput").ap()
        for n in ("ev_ssm_c_re", "ev_ssm_c_im"):
            dr[n] = nc.dram_tensor(n, [32, 16, 64], F32, kind="ExternalInput").ap()
        dr["c_ident"] = nc.dram_tensor("c_ident", [128, 128], F32, kind="ExternalInput").ap()
        dr["c_onesr"] = nc.dram_tensor("c_onesr", [128, 128], F32R, kind="ExternalInput").ap()
        dr["c_maskpar"] = nc.dram_tensor("c_maskpar", [128, 3], F32, kind="ExternalInput").ap()
        dr["c_triu"] = nc.dram_tensor("c_triu", [64, 64], F32, kind="ExternalInput").ap()
        dr["c_rmask"] = nc.dram_tensor("c_rmask", [128, 528], F32, kind="ExternalInput").ap()
        dr["c_flag"] = nc.dram_tensor("c_flag", [128, 2], F32, kind="ExternalInput").ap()
        dr["out"] = nc.dram_tensor("out", [64 * nchunks, D], F32, kind="ExternalOutput").ap()
        self.P = Prog(nc)
        self.alloc()
        self.prologue()
        pos = 0
        for bi, chunks in enumerate(self.blocks):
            self.block(pos, pos, chunks, hist=True, blend=False)
            pos += sum(chunks)
        self.main_start()
        mpos = 0
        for bi, chunks in enumerate(self.blocks):
            self.block(pos, mpos, chunks, hist=False, blend=(bi == 0))
            pos += sum(chunks)
            mpos += sum(chunks)
        for os_ in self.osem:
            self.P.wait("act", (os_, os_.val))
        self.P.build()

    def alloc(self):
        P = self.P
        b = P.buf
        ht = P.ctx.enter_context(P.nc.sbuf_tensor("h", [128, KD, 512], F32))
        self.ht = ht
        hnt = P.ctx.enter_context(P.nc.sbuf_tensor("hn", [128, KD, 512], F32R))
        import os
        NA = 8 if os.environ.get("SMALLA") else 28
        At = P.ctx.enter_context(P.nc.sbuf_tensor("A", [128, NA, 512], F32R))
        self.At = At
        self.h = [b(f"h{k}", None, None, alias=ht[:, k, :]) for k in range(KD)]
        self.hn = [b(f"hn{k}", None, None, alias=hnt[:, k, :]) for k in range(KD)]
        self.A = [b(f"A{k}", None, None, alias=At[:, k % NA, :]) for k in range(28)]
        self.slots = [b(f"slot{i}", [128, 2048], F32R) for i in range(NSLOT)]
        self.slot_sem = [Sem(P, f"slsem{i}") for i in range(NSLOT)]
        self.slot_i = 0
        self.ps = [b(f"ps{i}", [128, 512], F32, "psum") for i in range(8)]
        self.pool = [0, 1, 2, 3, 4, 5]
        self.ps_i = 0
        self.psm_i = 0
        self.tmp = [b(f"tmp{i}", [128, 512], F32) for i in range(6)]
        self.tmp_i = 0
        self.xt = [b(f"xt{i}", [128, 1024], F32) for i in range(2)]
        self.xt_sem = [Sem(P, f"xtsem{i}") for i in range(2)]
        self.osem = [Sem(P, f"osem{i}") for i in range(2)]
        self.psem = Sem(P, "psem")
        self.io_i = 0
        self.vec = {n: b("p_" + n, [128, 8], F32) for n in VEC1024}
        self.vec5 = {n: b("p_" + n, [128, 4], F32) for n in VEC512}
        self.convw = b("convw", [128, 4, 8], F32)
        self.rw = b("rw", [128, 8, 8], F32)
        self.rwg = b("rwg", [128, 8, 8], F32)
        self.ident = b("ident", [128, 128], F32)
        self.onesr = b("onesr", [128, 128], F32R)
        self.maskpar = b("maskpar", [128, 3], F32)
        self.triu = b("triu", [64, 64], F32)
        self.rmask = b("rmask", [128, 528], F32)
        self.w2 = b("w2", [16, 512], F32R)
        self.nc8 = b("nc8", [128, 8], F32)
        self.nc16 = b("nc16", [128, 8], F32)
        self.rec = b("rec", [128, 8, 3 + 512], F32)
        self.lru_state = b("lru_state", [128, 8], F32)
        self.Bc = b("Bc", [128, 4, 2, 128], F32R)
        self.Cc = b("Cc", [128, 16, 2, 64], F32)
        self.Bc3 = b("Bc3", [128, 4, 2, 128], F32R)
        self.lam_pw = b("lam_pw", [128, 16, 10, 3], F32)
        self.s5_state = b("s5_state", [128, 16, 2], F32)
        self.S = [b(f"S{i}", [128, 256], F32) for i in range(4)]
        self.Srab = [b(f"Srab{i}", [128, 256], F32R) for i in range(2)]
        self.S2 = b("Stmp2", [128, 256], F32)
        self.small = [b(f"small{i}", [128, 16], F32) for i in range(8)]
        self.small_i = 0
        self.epsb = b("epsb", [128, 1], F32)
        self.oneb = b("oneb", [128, 1], F32)
        self.ones8 = b("ones8", [8, 128], F32)
        self.vt = b("vt", None, None, alias=self.At[0:64, 12:16, :].rearrange("p a b -> p (a b)").rearrange("p (c v) -> p c v", c=8))
        self.qt = self.A[10]
        self.kt = self.A[11]
        self.att2 = [b(f"att{i}", [64, 64], F32R) for i in range(2)]
        self.ktT = b("ktT", None, None, alias=self.At[0:64, 16:18, :].rearrange("p a b -> p (a b)").rearrange("p (c v) -> p c v", c=8))
        self.gbc = b("gbc", [128, 512], F32)
        self.flag = b("flag", [128, 2], F32)
        self.gkeep = b("gkeep", [8, 512], F32)
        self.prev_n = None
        self.scr_off = 0

    def scratch(self, name, npart, width, dtype=F32):
        regs = [(self.ht[:, :, :].rearrange("p a b -> p (a b)"), 4096),
                (self.rec.t[:, :, :].rearrange("p a b -> p (a b)"), 8 * 515)]
        if not hasattr(self, "scr_offs"):
            self.scr_offs = [0, 0]
        for ri, (flat, cap) in enumerate(regs):
            off = self.scr_offs[ri]
            if off + width <= cap:
                self.scr_offs[ri] = off + width
                ap = flat[0:npart, off:off + width]
                if dtype is not F32:
                    ap = ap.bitcast(dtype)
                bf = self.P.buf(name, None, None, alias=ap)
                self.scr_list.append(bf)
                return bf
        raise AssertionError("scratch full")

    def T(self):
        t = self.tmp[self.tmp_i % len(self.tmp)]
        self.tmp_i += 1
        return t

    def SM(self):
        t = self.small[self.small_i % len(self.small)]
        self.small_i += 1
        return t

    def PS(self):
        t = self.ps[self.pool[self.ps_i % len(self.pool)]]
        self.ps_i += 1
        return t

    def PSM(self):
        t = self.ps[6 + self.psm_i % 2]
        self.psm_i += 1
        return t

    def next_slot(self):
        i = self.slot_i % NSLOT
        self.slot_i += 1
        return self.slots[i], self.slot_sem[i]

    def kslab(self, name, k0, kc, c0, ncols, row0=0):
        assert kc * ncols <= 2048
        w = self.dr[name]
        rows = w[row0 + k0 * 128: row0 + (k0 + kc) * 128, c0:c0 + ncols].rearrange("(k p) c -> p k c", p=128)
        s, sem = self.next_slot()
        sv = s.t[:, 0:kc * ncols].rearrange("p (k c) -> p k c", k=kc)
        self.P.dma("sp", s.v(sv), rows, sem)
        return s, sv

    def prologue(self):
        P, dr = self.P, self.dr
        ps = self.psem
        self.scr_list = []
        loaded = []
        import os
        if os.environ.get("MINPRO"):
            P.dma("sp", self.ident[:, :], dr["c_ident"], ps)
            P.dma("sp", self.onesr[:, :], dr["c_onesr"], ps)
            self.ident.lastw = (ps, ps.val)
            self.onesr.lastw = (ps, ps.val)
            return

        def ld(dst, src, **kw):
            P.dma("sp", dst, src, ps, **kw)
            loaded.append(dst.buf)
        nrow = 8 * len(VEC1024) + 4 * len(VEC512) + 32
        vstage = self.scratch("vstage", nrow, 128)
        r0 = 0
        self.vrows = {}
        for n in VEC1024:
            ld(vstage[r0:r0 + 8, :], dr[n].rearrange("(k p) -> k p", p=128))
            self.vrows[n] = (r0, 8)
            r0 += 8
        for n in VEC512:
            ld(vstage[r0:r0 + 4, :], dr[n].rearrange("(k p) -> k p", p=128))
            self.vrows[n] = (r0, 4)
            r0 += 4
        ld(vstage[r0:r0 + 32, :], dr["ev_conv_w"].rearrange("t (k p) -> (t k) p", p=128))
        self.vrows["ev_conv_w"] = (r0, 32)
        self.vstage = vstage
        ld(self.rw[:, :, :], dr["od_router_w"].rearrange("(k p) e -> p k e", p=128))
        ld(self.ident[:, :], dr["c_ident"])
        ld(self.onesr[:, :], dr["c_onesr"])
        ld(self.maskpar[:, :], dr["c_maskpar"])
        ld(self.triu[:, :], dr["c_triu"])
        ld(self.rmask[:, :], dr["c_rmask"])
        ld(self.w2[:, :], dr["od_gla_gate_w2"])
        ld(self.flag[:, :], dr["c_flag"])
        g = {}
        for n in ("ev_ssm_lambda_re", "ev_ssm_lambda_im"):
            t = self.scratch("g32_" + n, 32, 64)
            ld(t[:, :], dr[n])
            g[n] = t
            t2 = self.scratch("p16_" + n, 16, 128)
            ld(t2[:, :], dr[n].rearrange("(q r) n -> q (r n)", r=2))
            g[n + "_p16"] = t2
        self.g32 = g
        ldt32 = self.scratch("ldt32", 32, 1)
        ld(ldt32[:, :], dr["ev_ssm_log_dt"].rearrange("(g o) -> g o", o=1))
        ldt16 = self.scratch("ldt16", 16, 2)
        ld(ldt16[:, :], dr["ev_ssm_log_dt"].rearrange("(q r) -> q r", r=2))
        bre = self.scratch("bre", 64, 512)
        bim = self.scratch("bim", 64, 512)
        ld(bre.v(bre.t.rearrange("p (g h) -> p g h", g=32)), dr["ev_ssm_b_re"].rearrange("g n h -> n g h"))
        ld(bim.v(bim.t.rearrange("p (g h) -> p g h", g=32)), dr["ev_ssm_b_im"].rearrange("g n h -> n g h"))
        cnat = self.scratch("cnat", 128, 512)
        cv = cnat.t.rearrange("p (c j n) -> p c j n", c=4, j=2)
        ld(cnat.v(cv[:, :, 0, :]), dr["ev_ssm_c_re"].rearrange("(c g) h n -> (g h) c n", c=4))
        ld(cnat.v(cv[:, :, 1, :]), dr["ev_ssm_c_im"].rearrange("(c g) h n -> (g h) c n", c=4))
        fin = (ps, ps.val)
        for bf in loaded:
            bf.lastw = fin

        op = P.op
        op("dve", "memset", ap=self.epsb[:, :], constant=EPS)
        op("dve", "memset", ap=self.oneb[:, :], constant=1.0)
        op("dve", "memset", ap=self.ones8[:, :], constant=1.0)
        op("dve", "memset", ap=self.lru_state[:, :], constant=0.0)
        op("dve", "memset", ap=self.s5_state[:, :, :], constant=0.0)
        for i in range(4):
            op("dve", "memset", ap=self.S[i][:, :], constant=0.0)
        pv = self.PSM()
        nrow = 8 * len(VEC1024) + 4 * len(VEC512) + 32
        op("pe", "transpose", out=pv[:, 0:nrow], in_=self.vstage[:, :], identity=self.ident[0:nrow, 0:nrow])
        for n in VEC1024:
            r0, nr = self.vrows[n]
            op("dve", "tensor_copy", out=self.vec[n][:, :], in_=pv[:, r0:r0 + nr])
        for n in VEC512:
            r0, nr = self.vrows[n]
            op("dve", "tensor_copy", out=self.vec5[n][:, :], in_=pv[:, r0:r0 + nr])
        r0, nr = self.vrows["ev_conv_w"]
        op("dve", "tensor_copy", out=self.convw[:, :, :], in_=pv.v(pv.t[:, r0:r0 + 32].rearrange("p (t k) -> p t k", t=4)))
        gff = self.vec["od_norm_ffn"]
        for k in range(8):
            op("dve", "tensor_scalar", out=self.rwg[:, k, :], in0=self.rw[:, k, :], scalar1=gff[:, k:k + 1], scalar2=None, op0=ALU.mult)
        lam = self.vec["ev_lru_lambda"]
        t1, t2 = self.SM(), self.SM()
        op("act", "activation", out=t1[:, 0:8], in_=lam[:, :], func=AF.Abs)
        op("act", "activation", out=t1[:, 0:8], in_=t1[:, 0:8], func=AF.Exp, scale=-1.0)
        op("act", "activation", out=t1[:, 0:8], in_=t1[:, 0:8], func=AF.Ln, bias=self.oneb[:, 0:1], scale=1.0)
        op("dve", "tensor_scalar", out=t2[:, 0:8], in0=lam[:, :], scalar1=-1.0, scalar2=0.0, op0=ALU.mult, op1=ALU.max)
        op("dve", "tensor_tensor", out=t1[:, 0:8], in0=t1[:, 0:8], in1=t2[:, 0:8], op=ALU.add)
        op("dve", "tensor_scalar", out=self.nc8[:, :], in0=t1[:, 0:8], scalar1=-8.0, scalar2=None, op0=ALU.mult)
        op("dve", "tensor_scalar", out=self.nc16[:, :], in0=t1[:, 0:8], scalar1=-16.0, scalar2=None, op0=ALU.mult)
        import os
        if not os.environ.get('SKIP_S5P'):
            self.s5_prologue(ldt32, ldt16, bre, bim, cnat)
        for bf in self.scr_list:
            for a in self.h + [self.rec]:
                if bf.lastw is not None:
                    a.reads.append(bf.lastw)
                a.reads.extend(bf.reads)
        op("dve", "memset", ap=self.rec[:, :, :], constant=0.0)

    def lbar(self, lr, li, dtcol, npart, width, name):
        P = self.P
        op = P.op
        mk = lambda s: self.scratch(f"{name}_{s}", npart, width)
        dlr, dli, mag, sn, cs, ar, ai = mk("dlr"), mk("dli"), mk("mag"), mk("sn"), mk("cs"), mk("ar"), mk("ai")
        for sl, sc in dtcol:
            op("dve", "tensor_scalar", out=dlr[:, sl], in0=lr[:, sl], scalar1=sc, scalar2=None, op0=ALU.mult)
            op("dve", "tensor_scalar", out=dli[:, sl], in0=li[:, sl], scalar1=sc, scalar2=None, op0=ALU.mult)
        op("act", "activation", out=mag[:, :], in_=dlr[:, :], func=AF.Exp)
        ki = self.scratch(name + "_ki", npart, width, dtype=I32)
        kf = mk("kf")
        op("dve", "tensor_scalar", out=kf[:, :], in0=dli[:, :], scalar1=1.0 / (2 * math.pi), scalar2=None, op0=ALU.mult)
        op("dve", "tensor_copy", out=ki[:, :], in_=kf[:, :])
        op("dve", "tensor_copy", out=kf[:, :], in_=ki[:, :])
        r = mk("r")
        op("dve", "scalar_tensor_tensor", out=r[:, :], in0=kf[:, :], scalar=-2 * math.pi, in1=dli[:, :], op0=ALU.mult, op1=ALU.add)

        def wrap(tag, src, shift):
            y, m = mk("wy" + tag), mk("wm" + tag)
            op("dve", "tensor_scalar", out=y[:, :], in0=src[:, :], scalar1=shift, scalar2=None, op0=ALU.add)
            for _ in range(2):
                op("dve", "tensor_scalar", out=m[:, :], in0=y[:, :], scalar1=math.pi, scalar2=-2 * math.pi, op0=ALU.is_gt, op1=ALU.mult)
                op("dve", "tensor_tensor", out=y[:, :], in0=y[:, :], in1=m[:, :], op=ALU.add)
                op("dve", "tensor_scalar", out=m[:, :], in0=y[:, :], scalar1=-math.pi, scalar2=2 * math.pi, op0=ALU.is_lt, op1=ALU.mult)
                op("dve", "tensor_tensor", out=y[:, :], in0=y[:, :], in1=m[:, :], op=ALU.add)
            op("dve", "tensor_scalar", out=y[:, :], in0=y[:, :], scalar1=math.pi, scalar2=-math.pi, op0=ALU.min, op1=ALU.max)
            return y
        ys = wrap("s", r, 0.0)
        yc = wrap("c", r, math.pi / 2)
        op("act", "activation", out=sn[:, :], in_=ys[:, :], func=AF.Sin)
        op("act", "activation", out=cs[:, :], in_=yc[:, :], func=AF.Sin)
        op("dve", "tensor_tensor", out=ar[:, :], in0=mag[:, :], in1=cs[:, :], op=ALU.mult)
        op("dve", "tensor_tensor", out=ai[:, :], in0=mag[:, :], in1=sn[:, :], op=ALU.mult)
        return ar, ai

    def s5_prologue(self, ldt32, ldt16, bre, bim, cnat):
        P = self.P
        op = P.op
        g = self.g32
        dt32 = self.scratch("dt32", 32, 1)
        dt16 = self.scratch("dt16", 16, 2)
        op("act", "activation", out=dt32[:, :], in_=ldt32[:, :], func=AF.Exp)
        op("act", "activation", out=dt16[:, :], in_=ldt16[:, :], func=AF.Exp)
        lr, li = g["ev_ssm_lambda_re"], g["ev_ssm_lambda_im"]
        ar, ai = self.lbar(lr, li, [(slice(0, 64), dt32[:, 0:1])], 32, 64, "L32")
        mk = lambda s: self.scratch("z32_" + s, 32, 64)
        den, t, nr, zr, zi = mk("den"), mk("t"), mk("nr"), mk("zr"), mk("zi")
        op("dve", "tensor_tensor", out=den[:, :], in0=lr[:, :], in1=lr[:, :], op=ALU.mult)
        op("dve", "tensor_tensor", out=t[:, :], in0=li[:, :], in1=li[:, :], op=ALU.mult)
        op("dve", "tensor_tensor", out=den[:, :], in0=den[:, :], in1=t[:, :], op=ALU.add)
        op("dve", "reciprocal", out=den[:, :], in_=den[:, :])
        op("dve", "tensor_scalar", out=nr[:, :], in0=ar[:, :], scalar1=-1.0, scalar2=None, op0=ALU.add)
        op("dve", "tensor_tensor", out=zr[:, :], in0=nr[:, :], in1=lr[:, :], op=ALU.mult)
        op("dve", "tensor_tensor", out=t[:, :], in0=ai[:, :], in1=li[:, :], op=ALU.mult)
        op("dve", "tensor_tensor", out=zr[:, :], in0=zr[:, :], in1=t[:, :], op=ALU.add)
        op("dve", "tensor_tensor", out=zr[:, :], in0=zr[:, :], in1=den[:, :], op=ALU.mult)
        op("dve", "tensor_tensor", out=zi[:, :], in0=ai[:, :], in1=lr[:, :], op=ALU.mult)
        op("dve", "tensor_tensor", out=t[:, :], in0=nr[:, :], in1=li[:, :], op=ALU.mult)
        op("dve", "tensor_tensor", out=zi[:, :], in0=zi[:, :], in1=t[:, :], op=ALU.subtract)
        op("dve", "tensor_tensor", out=zi[:, :], in0=zi[:, :], in1=den[:, :], op=ALU.mult)
        zT = self.scratch("zT", 64, 96)
        for j, src in enumerate((zr, zi)):
            pt = self.PSM()
            op("pe", "transpose", out=pt[0:64, 0:32], in_=src[:, :], identity=self.ident[0:32, 0:32])
            op("dve", "tensor_copy", out=zT[:, 32 * j:32 * j + 32], in_=pt[0:64, 0:32])
        op("dve", "tensor_scalar", out=zT[:, 64:96], in0=zT[:, 32:64], scalar1=-1.0, scalar2=None, op0=ALU.mult)
        Bre = self.scratch("Bre", 64, 512)
        Bim = self.scratch("Bim", 64, 512)
        tt = self.scratch("tt", 64, 16)
        for gi in range(32):
            gs = slice(16 * gi, 16 * gi + 16)
            op("dve", "tensor_scalar", out=tt[:, :], in0=bre[:, gs], scalar1=zT[:, gi:gi + 1], scalar2=None, op0=ALU.mult)
            op("dve", "scalar_tensor_tensor", out=Bre[:, gs], in0=bim[:, gs], scalar=zT[:, 64 + gi:65 + gi], in1=tt[:, :], op0=ALU.mult, op1=ALU.add)
            op("dve", "tensor_scalar", out=tt[:, :], in0=bim[:, gs], scalar1=zT[:, gi:gi + 1], scalar2=None, op0=ALU.mult)
            op("dve", "scalar_tensor_tensor", out=Bim[:, gs], in0=bre[:, gs], scalar=zT[:, 32 + gi:33 + gi], in1=tt[:, :], op0=ALU.mult, op1=ALU.add)
        for c in range(4):
            for j, src in enumerate((Bre, Bim)):
                pt = self.PSM()
                op("pe", "transpose", out=pt[:, 0:64], in_=src[:, 128 * c:128 * c + 128], identity=self.ident[0:64, 0:64])
                for par in range(2):
                    op("dve", "tensor_scalar", out=self.Bc[:, c, j, 64 * par:64 * par + 64], in0=pt[:, 0:64],
                       scalar1=self.maskpar[:, par:par + 1], scalar2=None, op0=ALU.mult)
        for c in range(4):
            for j in range(2):
                op("dve", "tensor_scalar", out=self.Bc3[:, c, j, :], in0=self.Bc.f[:, c, j, :],
                   scalar1=self.maskpar[:, 2:3], scalar2=None, op0=ALU.mult)
        op("dve", "memset", ap=self.Cc[:, :, :, :], constant=0.0)
        pre2 = self.scratch("pre2", 128, 128)
        for c in range(4):
            for j in range(2):
                for par in range(2):
                    op("dve", "tensor_scalar", out=pre2[:, 64 * par:64 * par + 64], in0=cnat[:, (c * 2 + j) * 64:(c * 2 + j) * 64 + 64],
                       scalar1=self.maskpar[:, par:par + 1], scalar2=(1.0 if j == 0 else -1.0), op0=ALU.mult, op1=ALU.mult)
                pt = self.PSM()
                op("pe", "transpose", out=pt[:, 0:128], in_=pre2[:, :], identity=self.ident[:, :])
                for r in range(4):
                    op("dve", "tensor_copy", out=self.Cc[:, 4 * c + r, j, 32 * (r % 2):32 * (r % 2) + 32],
                       in_=pt[:, 32 * r:32 * r + 32])
        lr16, li16 = g["ev_ssm_lambda_re_p16"], g["ev_ssm_lambda_im_p16"]
        ar16, ai16 = self.lbar(lr16, li16, [(slice(0, 64), dt16[:, 0:1]), (slice(64, 128), dt16[:, 1:2])], 16, 128, "L16")
        lp = self.lam_pw
        for j, src in enumerate((ar16, ai16)):
            pt = self.PSM()
            op("pe", "transpose", out=pt[:, 0:16], in_=src[:, :], identity=self.ident[0:16, 0:16])
            op("dve", "tensor_copy", out=lp[:, :, 0, j], in_=pt[:, 0:16])
        t1 = self.scratch("lp_t1", 128, 16)
        t2 = self.scratch("lp_t2", 128, 16)
        for lv in range(1, 10):
            op("dve", "tensor_tensor", out=t1[:, :], in0=lp[:, :, lv - 1, 0], in1=lp[:, :, lv - 1, 0], op=ALU.mult)
            op("dve", "tensor_tensor", out=t2[:, :], in0=lp[:, :, lv - 1, 1], in1=lp[:, :, lv - 1, 1], op=ALU.mult)
            op("dve", "tensor_tensor", out=lp[:, :, lv, 0], in0=t1[:, :], in1=t2[:, :], op=ALU.subtract)
            op("dve", "tensor_tensor", out=t1[:, :], in0=lp[:, :, lv - 1, 0], in1=lp[:, :, lv - 1, 1], op=ALU.mult)
            op("dve", "tensor_scalar", out=lp[:, :, lv, 1], in0=t1[:, :], scalar1=2.0, scalar2=None, op0=ALU.mult)
        op("dve", "tensor_scalar", out=lp[:, :, :, 2], in0=lp[:, :, :, 1], scalar1=-1.0, scalar2=None, op0=ALU.mult)

    def rmsnorm(self, n, gname, src=None, dst=None, dst_f32=False):
        P = self.P
        op = P.op
        src = src or self.h
        dst = dst or self.hn
        A = self.A
        for k in range(KD):
            op("act", "activation", out=A[20 + k][:, 0:n], in_=src[k][:, 0:n], func=AF.Square)
        pss = self.PSM()
        for k in range(KD):
            op("pe", "matmul", out=pss[:, 0:n], lhsT=self.onesr[:, :], rhs=A[20 + k][:, 0:n], start=(k == 0), stop=(k == KD - 1))
        rstd = self.T()
        op("act", "activation", out=rstd[:, 0:n], in_=pss[:, 0:n], func=AF.Sqrt, scale=1.0 / 1024.0, bias=self.epsb[:, 0:1])
        op("dve", "reciprocal", out=rstd[:, 0:n], in_=rstd[:, 0:n])
        g = self.vec[gname]
        for k in range(KD):
            o = dst[k].f[:, 0:n] if dst_f32 else dst[k][:, 0:n]
            op("dve", "scalar_tensor_tensor", out=o, in0=src[k][:, 0:n], scalar=g[:, k:k + 1],
               in1=rstd[:, 0:n], op0=ALU.mult, op1=ALU.mult)
        return rstd

    def load_tokens(self, pos, n):
        P = self.P
        op = P.op
        import os
        t0 = 0
        nlim = int(os.environ.get('LOAD_TOK', '100000'))
        while t0 < min(n, nlim):
            nt = min(128, n - t0)
            i = self.io_i % 2
            self.io_i += 1
            xt = self.xt[i]
            P.dma("act", xt[0:nt, :], self.dr["tok"][pos + t0:pos + t0 + nt, :], self.xt_sem[i])
            lmode = int(os.environ.get("LOAD_MODE", "0")) if t0 > 0 else 0
            for half in range(2):
                if lmode == 1:
                    continue
                pt = self.PS()
                for j in range(4):
                    k = half * 4 + j
                    op("pe", "transpose", out=pt[:, j * 128:j * 128 + nt], in_=xt[0:nt, k * 128:(k + 1) * 128],
                       identity=self.ident[0:nt, 0:nt])
                if lmode == 2:
                    continue
                for j in range(4):
                    k = half * 4 + j
                    dstv = self.h[k][:, t0:t0 + nt]
                    if lmode == 3:
                        dstv = self.tmp[k % 6][:, 0:nt]
                    if lmode == 4:
                        dstv = self.h[k][:, 0:nt]
                    if lmode == 5 and j % 2 == 1:
                        continue
                    if lmode == 6 and j % 2 == 0:
                        continue
                    use_dve = (half == 0)
                    if use_dve:
                        op("dve", "tensor_copy", out=dstv, in_=pt[:, j * 128:j * 128 + nt])
                    else:
                        op("act", "copy", out=dstv, in_=pt[:, j * 128:j * 128 + nt])
            t0 += nt

    def store_tokens(self, src, pos, n, f32view=False):
        P = self.P
        op = P.op
        import os
        t0 = 0
        nlim = int(os.environ.get('STORE_TOK', '100000'))
        while t0 < min(n, nlim):
            nt = min(128, n - t0)
            i = self.io_i % 2
            self.io_i += 1
            ot = self.xt[i]
            for half in range(2):
                pt = self.PS()
                for j in range(4):
                    k = half * 4 + j
                    sv = src[k].f[:, t0:t0 + nt] if f32view else src[k][:, t0:t0 + nt]
                    op("pe", "transpose", out=pt[0:nt, j * 128:(j + 1) * 128], in_=sv, identity=self.ident[:, :])
                if half == 0:
                    op("dve", "tensor_copy", out=ot[0:nt, 0:512], in_=pt[0:nt, :])
                else:
                    op("act", "copy", out=ot[0:nt, 512:1024], in_=pt[0:nt, :])
            lo = pos + t0
            skip = max(0, NMETA - lo)
            if skip < nt:
                a = skip
                while a < nt:
                    bnd = min(nt, (a // 32 + 1) * 32) if a % 32 else nt
                    P.dma("act", self.dr["out"][lo + a - NMETA: lo + bnd - NMETA, :], ot[a:bnd, :], self.osem[i])
                    a = bnd
            t0 += nt

    def main_start(self):
        op = self.P.op
        g = self.flag[:, 1:2]
        op("dve", "tensor_scalar", out=self.lru_state[:, :], in0=self.lru_state[:, :], scalar1=g, scalar2=None, op0=ALU.mult)
        op("dve", "tensor_scalar", out=self.s5_state[:, :, :], in0=self.s5_state[:, :, :], scalar1=g, scalar2=None, op0=ALU.mult)
        for i in range(4):
            op("dve", "tensor_scalar", out=self.S[i][:, :], in0=self.S[i][:, :], scalar1=g, scalar2=None, op0=ALU.mult)
        pn = self.prev_n
        op("dve", "tensor_scalar", out=self.rec[:, :, pn:pn + 3], in0=self.rec[:, :, pn:pn + 3], scalar1=g, scalar2=None, op0=ALU.mult)

    def block(self, pos, mpos, chunks, hist, blend):
        n = sum(chunks)
        self.load_tokens(pos, n)
        if self.stage_stop >= 1:
            self.layer0_mixer(n, blend)
        if self.stage_stop >= 2:
            self.ffn0(n)
        if self.stage_stop >= 3:
            self.gla(n, chunks, hist, blend)
        if hist:
            return
        if self.stage_stop >= 4:
            self.moe(n)
        if not self.dump_h:
            self.rmsnorm(n, "final_norm", dst=self.h)
        self.store_tokens(self.h, mpos, n)

    def mm_out(self, n, lhs_list, rhs_list, ps=None):
        ps = ps or self.PS()
        nk = len(lhs_list)
        for k in range(nk):
            self.P.op("pe", "matmul", out=ps[:, 0:n], lhsT=lhs_list[k], rhs=rhs_list[k], start=(k == 0), stop=(k == nk - 1))
        return ps

    def layer0_mixer(self, n, blend=False):
        P = self.P
        op = P.op
        A, hn, h = self.A, self.hn, self.h
        self.rmsnorm(n, "ev_norm_mix")
        rec = self.rec
        if self.prev_n is not None:
            op("act", "copy", out=rec[:, :, 0:3], in_=rec[:, :, self.prev_n:self.prev_n + 3])
        self.prev_n = n
        rhs = [hn[k][:, 0:n] for k in range(8)]
        for sl in range(10):
            s, sv = self.kslab("ev_w_in", 0, 8, sl * 256, 256)
            for m in range(2):
                mc = sl * 2 + m
                ps = self.mm_out(n, [s.v(sv[:, k, m * 128:(m + 1) * 128]) for k in range(8)], rhs)
                if mc < 8:
                    op("act", "activation", out=A[mc][:, 0:n], in_=ps[:, 0:n], func=AF.Gelu)
                elif mc < 16:
                    op("dve", "tensor_copy", out=rec[:, mc - 8, 3:3 + n], in_=ps[:, 0:n])
                else:
                    op("dve", "tensor_copy", out=A[8 + mc - 16][:, 0:n], in_=ps[:, 0:n])
        cw, cb = self.convw, self.vec["ev_conv_b"]
        for k in range(KD):
            t = self.T()
            op("dve", "tensor_scalar", out=t[:, 0:n], in0=rec[:, k, 0:n], scalar1=cw[:, 0, k:k + 1], scalar2=cb[:, k:k + 1], op0=ALU.mult, op1=ALU.add)
            op("dve", "scalar_tensor_tensor", out=t[:, 0:n], in0=rec[:, k, 1:1 + n], scalar=cw[:, 1, k:k + 1], in1=t[:, 0:n], op0=ALU.mult, op1=ALU.add)
            op("dve", "scalar_tensor_tensor", out=t[:, 0:n], in0=rec[:, k, 2:2 + n], scalar=cw[:, 2, k:k + 1], in1=t[:, 0:n], op0=ALU.mult, op1=ALU.add)
            op("dve", "scalar_tensor_tensor", out=A[12 + k][:, 0:n], in0=rec[:, k, 3:3 + n], scalar=cw[:, 3, k:k + 1], in1=t[:, 0:n], op0=ALU.mult, op1=ALU.add)
        sa, sav = self.kslab("ev_gate_a_w", 0, 8, 0, 256)
        sx, sxv = self.kslab("ev_gate_x_w", 0, 8, 0, 256)
        ba, bx = self.vec["ev_gate_a_b"], self.vec["ev_gate_x_b"]
        for j in range(KD):
            hb = j // 2
            cols = slice((j % 2) * 128, (j % 2) * 128 + 128)
            xin = [A[12 + 2 * hb + kk][:, 0:n] for kk in range(2)]
            psa = self.mm_out(n, [sa.v(sav[:, 2 * hb + kk, cols]) for kk in range(2)], xin)
            psx = self.mm_out(n, [sx.v(sxv[:, 2 * hb + kk, cols]) for kk in range(2)], xin)
            r, a_t, s_t, i_t = self.T(), self.T(), self.T(), self.T()
            op("act", "activation", out=r[:, 0:n], in_=psa[:, 0:n], func=AF.Sigmoid, bias=ba[:, j:j + 1], scale=1.0)
            op("act", "activation", out=i_t[:, 0:n], in_=psx[:, 0:n], func=AF.Sigmoid, bias=bx[:, j:j + 1], scale=1.0)
            op("act", "activation", out=a_t[:, 0:n], in_=r[:, 0:n], func=AF.Exp, scale=self.nc8[:, j:j + 1])
            op("act", "activation", out=s_t[:, 0:n], in_=r[:, 0:n], func=AF.Exp, scale=self.nc16[:, j:j + 1])
            op("act", "activation", out=s_t[:, 0:n], in_=s_t[:, 0:n], func=AF.Sqrt, scale=-1.0, bias=self.oneb[:, 0:1])
            op("dve", "tensor_tensor", out=i_t[:, 0:n], in0=i_t[:, 0:n], in1=A[12 + j].f[:, 0:n], op=ALU.mult)
            op("dve", "tensor_tensor", out=i_t[:, 0:n], in0=i_t[:, 0:n], in1=s_t[:, 0:n], op=ALU.mult)
            ls = self.lru_state[:, j:j + 1]
            if blend:
                op("dve", "tensor_tensor_scan", out=r[:, 0:16], data0=a_t[:, 0:16], data1=i_t[:, 0:16],
                   initial=ls, op0=ALU.mult, op1=ALU.add)
                tb = self.SM()
                op("dve", "tensor_scalar", out=tb[:, 0:1], in0=r[:, 15:16], scalar1=self.flag[:, 0:1], scalar2=None, op0=ALU.mult)
                op("dve", "scalar_tensor_tensor", out=ls, in0=ls, scalar=self.flag[:, 1:2], in1=tb[:, 0:1], op0=ALU.mult, op1=ALU.add)
                op("dve", "tensor_tensor_scan", out=r[:, 16:n], data0=a_t[:, 16:n], data1=i_t[:, 16:n],
                   initial=ls, op0=ALU.mult, op1=ALU.add)
            else:
                op("dve", "tensor_tensor_scan", out=r[:, 0:n], data0=a_t[:, 0:n], data1=i_t[:, 0:n],
                   initial=ls, op0=ALU.mult, op1=ALU.add)
            op("act", "copy", out=self.lru_state[:, j:j + 1], in_=r[:, n - 1:n])
            op("dve", "tensor_tensor", out=A[j][:, 0:n], in0=A[j].f[:, 0:n], in1=r[:, 0:n], op=ALU.mult)
        self.s5(n, blend)
        mixr = [A[k][:, 0:n] for k in range(8)] + [A[20 + k][:, 0:n] for k in range(4)]
        for cg in range(4):
            pss = [self.PS(), self.PS()]
            for part in range(2):
                s, sv = self.kslab("ev_w_out", part * 6, 6, cg * 256, 256)
                for mm in range(2):
                    for k in range(6):
                        kk = part * 6 + k
                        op("pe", "matmul", out=pss[mm][:, 0:n], lhsT=s.v(sv[:, k, mm * 128:(mm + 1) * 128]), rhs=mixr[kk],
                           start=(kk == 0), stop=(kk == 11))
            for mm in range(2):
                m = cg * 2 + mm
                op("dve", "tensor_tensor", out=h[m][:, 0:n], in0=pss[mm][:, 0:n], in1=h[m][:, 0:n], op=ALU.add)

    def s5(self, n, blend=False):
        P = self.P
        op = P.op
        A = self.A
        lp = self.lam_pw
        nlev = 0
        while (1 << nlev) < n:
            nlev += 1
        yps = self.ps[0:4]
        saved_pool = self.pool
        self.pool = [4, 5]
        st = self.s5_state
        for pi in range(16):
            c, r = pi // 4, pi % 4
            prow = slice(32 * r, 32 * r + 32)
            za = [self.T(), self.T()]
            zb = [self.T(), self.T()]
            for j in range(2):
                psx = self.PS()
                if r < 3:
                    op("pe", "matmul", out=psx[:, 0:n], lhsT=self.Bc[prow, c, j, :], rhs=A[8 + c][prow, 0:n], start=True, stop=True)
                else:
                    op("pe", "matmul", out=psx[:, 0:n], lhsT=self.Bc3[64:128, c, j, :], rhs=A[8 + c][64:128, 0:n], start=True, stop=True)
                op("act", "copy", out=za[j][:, 0:n], in_=psx[:, 0:n])
            def seg(c0, c1):
                op("dve", "scalar_tensor_tensor", out=za[0][:, c0:c0 + 1], in0=st[:, pi, 0:1], scalar=lp[:, pi, 0, 0:1], in1=za[0][:, c0:c0 + 1], op0=ALU.mult, op1=ALU.add)
                op("dve", "scalar_tensor_tensor", out=za[0][:, c0:c0 + 1], in0=st[:, pi, 1:2], scalar=lp[:, pi, 0, 2:3], in1=za[0][:, c0:c0 + 1], op0=ALU.mult, op1=ALU.add)
                op("dve", "scalar_tensor_tensor", out=za[1][:, c0:c0 + 1], in0=st[:, pi, 1:2], scalar=lp[:, pi, 0, 0:1], in1=za[1][:, c0:c0 + 1], op0=ALU.mult, op1=ALU.add)
                op("dve", "scalar_tensor_tensor", out=za[1][:, c0:c0 + 1], in0=st[:, pi, 0:1], scalar=lp[:, pi, 0, 1:2], in1=za[1][:, c0:c0 + 1], op0=ALU.mult, op1=ALU.add)
                src, dst = za, zb
                lv = 0
                while (1 << lv) < (c1 - c0):
                    d = 1 << lv
                    pr, pim, npi = lp[:, pi, lv, 0:1], lp[:, pi, lv, 1:2], lp[:, pi, lv, 2:3]
                    op("act", "copy", out=dst[0][:, c0:c0 + d], in_=src[0][:, c0:c0 + d])
                    op("act", "copy", out=dst[1][:, c0:c0 + d], in_=src[1][:, c0:c0 + d])
                    op("dve", "scalar_tensor_tensor", out=dst[0][:, c0 + d:c1], in0=src[0][:, c0:c1 - d], scalar=pr, in1=src[0][:, c0 + d:c1], op0=ALU.mult, op1=ALU.add)
                    op("dve", "scalar_tensor_tensor", out=dst[0][:, c0 + d:c1], in0=src[1][:, c0:c1 - d], scalar=npi, in1=dst[0][:, c0 + d:c1], op0=ALU.mult, op1=ALU.add)
                    op("dve", "scalar_tensor_tensor", out=dst[1][:, c0 + d:c1], in0=src[1][:, c0:c1 - d], scalar=pr, in1=src[1][:, c0 + d:c1], op0=ALU.mult, op1=ALU.add)
                    op("dve", "scalar_tensor_tensor", out=dst[1][:, c0 + d:c1], in0=src[0][:, c0:c1 - d], scalar=pim, in1=dst[1][:, c0 + d:c1], op0=ALU.mult, op1=ALU.add)
                    src, dst = dst, src
                    lv += 1
                return src
            if blend:
                sa = seg(0, 16)
                for j in range(2):
                    tb = self.SM()
                    op("dve", "tensor_scalar", out=tb[:, 0:1], in0=sa[j][:, 15:16], scalar1=self.flag[:, 0:1], scalar2=None, op0=ALU.mult)
                    op("dve", "scalar_tensor_tensor", out=st[:, pi, j:j + 1], in0=st[:, pi, j:j + 1], scalar=self.flag[:, 1:2], in1=tb[:, 0:1], op0=ALU.mult, op1=ALU.add)
                src = seg(16, n)
                if sa is not src:
                    for j in range(2):
                        op("act", "copy", out=src[j][:, 0:16], in_=sa[j][:, 0:16])
            else:
                src = seg(0, n)
            op("act", "copy", out=st[:, pi, 0:1], in_=src[0][:, n - 1:n])
            op("act", "copy", out=st[:, pi, 1:2], in_=src[1][:, n - 1:n])
            orow = slice(64 * (r // 2), 64 * (r // 2) + 64)
            for j in range(2):
                op("pe", "matmul", out=yps[c][orow, 0:n], lhsT=self.Cc[:, pi, j, :], rhs=src[j][:, 0:n],
                   start=(j == 0 and r % 2 == 0), stop=(j == 1 and r % 2 == 1))
        self.pool = saved_pool
        dv = self.vec5["ev_ssm_d"]
        for c in range(4):
            t = self.T()
            op("dve", "scalar_tensor_tensor", out=t[:, 0:n], in0=A[8 + c].f[:, 0:n], scalar=dv[:, c:c + 1], in1=yps[c][:, 0:n], op0=ALU.mult, op1=ALU.add)
            op("act", "activation", out=A[24 + c][:, 0:n], in_=t[:, 0:n], func=AF.Gelu)
        s, sv = self.kslab("ev_ssm_glu_w", 0, 4, 0, 512)
        gb = self.vec5["ev_ssm_glu_b"]
        for m in range(4):
            ps = self.mm_out(n, [s.v(sv[:, k, m * 128:(m + 1) * 128]) for k in range(4)], [A[24 + k][:, 0:n] for k in range(4)])
            t = self.T()
            op("act", "activation", out=t[:, 0:n], in_=ps[:, 0:n], func=AF.Sigmoid, bias=gb[:, m:m + 1], scale=1.0)
            op("dve", "tensor_tensor", out=A[20 + m][:, 0:n], in0=A[24 + m].f[:, 0:n], in1=t[:, 0:n], op=ALU.mult)

    def swiglu_dense(self, n, wg, wu, wd, nf, nparts, row0_gu=0, row0_d=0, gate_bc=None):
        P = self.P
        op = P.op
        A, hn, h = self.A, self.hn, self.h
        rhs = [hn[k][:, 0:n] for k in range(8)]
        for f0 in range(0, nf, 2):
            sg, sgv = self.kslab(wg, 0, 8, f0 * 128, 256, row0=row0_gu)
            su, suv = self.kslab(wu, 0, 8, f0 * 128, 256, row0=row0_gu)
            for m in range(2):
                f = f0 + m
                pg = self.mm_out(n, [sg.v(sgv[:, k, m * 128:(m + 1) * 128]) for k in range(8)], rhs)
                pu = self.mm_out(n, [su.v(suv[:, k, m * 128:(m + 1) * 128]) for k in range(8)], rhs)
                t = self.T()
                op("act", "activation", out=t[:, 0:n], in_=pg[:, 0:n], func=AF.Silu)
                op("dve", "tensor_tensor", out=A[f][:, 0:n], in0=pu[:, 0:n], in1=t[:, 0:n], op=ALU.mult)
        kp = nf // nparts
        for cg in range(4):
            pss = [self.PS(), self.PS()]
            for part in range(nparts):
                s, sv = self.kslab(wd, part * kp, kp, cg * 256, 256, row0=row0_d)
                for mm in range(2):
                    for k in range(kp):
                        f = part * kp + k
                        op("pe", "matmul", out=pss[mm][:, 0:n], lhsT=s.v(sv[:, k, mm * 128:(mm + 1) * 128]), rhs=A[f][:, 0:n],
                           start=(f == 0), stop=(f == nf - 1))
            for mm in range(2):
                m = cg * 2 + mm
                if gate_bc is None:
                    op("dve", "tensor_tensor", out=h[m][:, 0:n], in0=pss[mm][:, 0:n], in1=h[m][:, 0:n], op=ALU.add)
                else:
                    t = self.T()
                    op("dve", "tensor_tensor", out=t[:, 0:n], in0=pss[mm][:, 0:n], in1=gate_bc[:, 0:n], op=ALU.mult)
                    op("dve", "tensor_tensor", out=h[m][:, 0:n], in0=t[:, 0:n], in1=h[m][:, 0:n], op=ALU.add)

    def ffn0(self, n):
        self.rmsnorm(n, "ev_norm_ffn")
        self.swiglu_dense(n, "ev_ffn_w_gate", "ev_ffn_w_up", "ev_ffn_w_down", 24, 3)

    def gla(self, n, chunks, hist=False, blend=False):
        P = self.P
        op = P.op
        A, hn, h = self.A, self.hn, self.h
        self.rmsnorm(n, "od_norm_mix")
        rhs = [hn[k][:, 0:n] for k in range(8)]
        s, sv = self.kslab("od_w_in", 0, 8, 3072, 16)
        psg = self.PSM()
        for k in range(8):
            op("pe", "matmul", out=psg[0:16, 0:n], lhsT=s.v(sv[:, k, :]), rhs=rhs[k], start=(k == 0), stop=(k == 7))
        glr = A[27]
        op("dve", "tensor_copy", out=glr[0:16, 0:n], in_=psg[0:16, 0:n])
        gbv = self.vec5["od_gla_gate_b"]
        hnorm = self.vec["od_gla_norm"]
        rm0 = 0 if chunks[0] == 16 else 16
        saved_pool = self.pool
        for hd in range(4):
            po = [self.ps[(hd % 2) * 2], self.ps[(hd % 2) * 2 + 1]]
            self.pool = [4, 5] + ([2, 3] if hd % 2 == 0 else [0, 1])
            sqk, sem = self.next_slot()
            sqkv = sqk.t[:, 0:2048].rearrange("p (k c) -> p k c", k=8)
            w = self.dr["od_w_in"]
            if not hist:
                P.dma("sp", sqk.v(sqkv[:, :, 0:128]), w[:, hd * 128:hd * 128 + 128].rearrange("(k p) c -> p k c", p=128), sem)
            P.dma("sp", sqk.v(sqkv[:, :, 128:256]), w[:, 512 + hd * 128:512 + hd * 128 + 128].rearrange("(k p) c -> p k c", p=128), sem)
            svv, svvv = self.kslab("od_w_in", 0, 8, 1024 + hd * 256, 256)
            pq = None if hist else self.mm_out(n, [sqk.v(sqkv[:, k, 0:128]) for k in range(8)], rhs)
            pk = self.mm_out(n, [sqk.v(sqkv[:, k, 128:256]) for k in range(8)], rhs)
            pl = self.PS()
            op("pe", "matmul", out=pl[:, 0:n], lhsT=self.w2[:, hd * 128:(hd + 1) * 128], rhs=glr[0:16, 0:n], start=True, stop=True)
            xs, ax, mn, eb = self.T(), self.T(), self.T(), self.T()
            op("dve", "tensor_scalar", out=xs[:, 0:n], in0=pl[:, 0:n], scalar1=gbv[:, hd:hd + 1], scalar2=None, op0=ALU.add)
            op("act", "activation", out=ax[:, 0:n], in_=xs[:, 0:n], func=AF.Abs)
            op("act", "activation", out=ax[:, 0:n], in_=ax[:, 0:n], func=AF.Exp, scale=-1.0)
            op("act", "activation", out=ax[:, 0:n], in_=ax[:, 0:n], func=AF.Ln, bias=self.oneb[:, 0:1], scale=1.0)
            op("dve", "tensor_scalar", out=mn[:, 0:n], in0=xs[:, 0:n], scalar1=0.0, scalar2=None, op0=ALU.min)
            op("dve", "tensor_tensor", out=mn[:, 0:n], in0=mn[:, 0:n], in1=ax[:, 0:n], op=ALU.subtract)
            b16 = xs
            op("dve", "tensor_tensor_scan", out=b16[:, 0:n], data0=self.rmask[:, rm0:rm0 + n], data1=mn[:, 0:n], initial=0.0, op0=ALU.mult, op1=ALU.add)
            enb = ax
            op("act", "activation", out=eb[:, 0:n], in_=b16[:, 0:n], func=AF.Exp, scale=1.0 / 16.0)
            op("act", "activation", out=enb[:, 0:n], in_=b16[:, 0:n], func=AF.Exp, scale=-1.0 / 16.0)
            qt, kt = self.qt, self.kt
            if not hist:
                op("dve", "scalar_tensor_tensor", out=qt[:, 0:n], in0=pq[:, 0:n], scalar=128.0 ** -0.5, in1=eb[:, 0:n], op0=ALU.mult, op1=ALU.mult)
            op("dve", "tensor_tensor", out=kt[:, 0:n], in0=pk[:, 0:n], in1=enb[:, 0:n], op=ALU.mult)
            vt = self.vt
            c0 = 0
            for ci, cs in enumerate(chunks):
                pv = self.PS()
                for k in range(8):
                    op("pe", "matmul", out=pv[0:cs, 0:256], lhsT=hn[k][:, c0:c0 + cs], rhs=svv.v(svvv[:, k, :]), start=(k == 0), stop=(k == 7))
                op("act", "copy", out=vt[0:cs, ci, :], in_=pv[0:cs, 0:256])
                c0 += cs
            S = self.S[hd]
            Srab, ktT, S2 = self.Srab, self.ktT, self.S2
            c0 = 0
            for ci, cs in enumerate(chunks):
                pt = self.PS()
                op("pe", "transpose", out=pt[0:cs, 0:128], in_=kt.f[:, c0:c0 + cs], identity=self.ident[:, :])
                op("act", "copy", out=ktT[0:cs, ci, :], in_=pt[0:cs, 0:128])
                c0 += cs
            cur = 0
            op("dve", "tensor_copy", out=Srab[0][:, :], in_=S[:, :])
            c0 = 0
            for ci, cs in enumerate(chunks):
                cl = slice(c0, c0 + cs)
                last = eb[:, c0 + cs - 1:c0 + cs]
                if not hist:
                    pa = self.PS()
                    op("pe", "matmul", out=pa[0:cs, 0:cs], lhsT=kt[:, cl], rhs=qt[:, cl], start=True, stop=True)
                    att = self.att2[ci % 2]
                    op("dve", "tensor_tensor", out=att[0:cs, 0:cs], in0=pa[0:cs, 0:cs], in1=self.triu[0:cs, 0:cs], op=ALU.mult)
                    for vc in range(2):
                        op("pe", "matmul", out=po[vc][:, cl], lhsT=vt[0:cs, ci, vc * 128:(vc + 1) * 128], rhs=att[0:cs, 0:cs], start=True, stop=False)
                        op("pe", "matmul", out=po[vc][:, cl], lhsT=Srab[cur][:, vc * 128:(vc + 1) * 128], rhs=qt[:, cl], start=False, stop=True)
                pS = self.PS()
                op("pe", "matmul", out=pS[:, 0:256], lhsT=ktT[0:cs, ci, :], rhs=vt[0:cs, ci, :], start=True, stop=True)
                op("dve", "tensor_tensor", out=S2[:, :], in0=pS[:, 0:256], in1=S[:, :], op=ALU.add)
                if blend and ci == 0:
                    op("dve", "tensor_scalar", out=S2[:, :], in0=S2[:, :], scalar1=last, scalar2=self.flag[:, 0:1], op0=ALU.mult, op1=ALU.mult)
                    op("dve", "scalar_tensor_tensor", out=S[:, :], in0=S[:, :], scalar=self.flag[:, 1:2], in1=S2[:, :], op0=ALU.mult, op1=ALU.add)
                else:
                    op("dve", "tensor_scalar", out=S[:, :], in0=S2[:, :], scalar1=last, scalar2=None, op0=ALU.mult)
                cur ^= 1
                op("dve", "tensor_copy", out=Srab[cur][:, :], in_=S[:, :])
                c0 += cs
            if hist:
                continue
            for vc in range(2):
                op("act", "activation", out=A[20 + vc][:, 0:n], in_=po[vc][:, 0:n], func=AF.Square)
            pss = self.PSM()
            for vc in range(2):
                op("pe", "matmul", out=pss[:, 0:n], lhsT=self.onesr[:, :], rhs=A[20 + vc][:, 0:n], start=(vc == 0), stop=(vc == 1))
            rs = self.T()
            op("act", "activation", out=rs[:, 0:n], in_=pss[:, 0:n], func=AF.Sqrt, scale=1.0 / 256.0, bias=self.epsb[:, 0:1])
            op("dve", "reciprocal", out=rs[:, 0:n], in_=rs[:, 0:n])
            sg, sgv = self.kslab("od_w_in", 0, 8, 2048 + hd * 256, 256)
            for vc in range(2):
                m = hd * 2 + vc
                on = self.T()
                op("dve", "scalar_tensor_tensor", out=on[:, 0:n], in0=po[vc][:, 0:n], scalar=hnorm[:, m:m + 1], in1=rs[:, 0:n], op0=ALU.mult, op1=ALU.mult)
                pg = self.mm_out(n, [sg.v(sgv[:, k, vc * 128:(vc + 1) * 128]) for k in range(8)], rhs)
                sgt = self.T()
                op("act", "activation", out=sgt[:, 0:n], in_=pg[:, 0:n], func=AF.Silu)
                op("dve", "tensor_tensor", out=A[m][:, 0:n], in0=on[:, 0:n], in1=sgt[:, 0:n], op=ALU.mult)
        self.pool = saved_pool
        if hist:
            return
        for cg in range(4):
            s, sv = self.kslab("od_w_out", 0, 8, cg * 256, 256)
            for mm in range(2):
                m = cg * 2 + mm
                ps = self.mm_out(n, [s.v(sv[:, k, mm * 128:(mm + 1) * 128]) for k in range(8)], [A[k][:, 0:n] for k in range(8)])
                op("dve", "tensor_tensor", out=h[m][:, 0:n], in0=ps[:, 0:n], in1=h[m][:, 0:n], op=ALU.add)

    def moe(self, n):
        P = self.P
        op = P.op
        hn = self.hn
        rstd = self.rmsnorm(n, "od_norm_ffn")
        pl = self.PSM()
        for k in range(8):
            op("pe", "matmul", out=pl[0:8, 0:n], lhsT=self.rwg[:, k, :], rhs=self.h[k][:, 0:n], start=(k == 0), stop=(k == 7))
        lfm, gfm = self.T(), self.T()
        op("dve", "memset", ap=lfm[0:64, 0:n], constant=0.0)
        op("dve", "tensor_copy", out=lfm[0:8, 0:n], in_=pl[0:8, 0:n])
        op("dve", "tensor_copy", out=lfm[32:33, 0:n], in_=rstd[32:33, 0:n])
        t0 = 0
        while t0 < n:
            nt = min(128, n - t0)
            pt = self.PSM()
            op("pe", "transpose", out=pt[0:nt, 0:33], in_=lfm[0:33, t0:t0 + nt], identity=self.ident[0:33, 0:33])
            lt, mx, g1, g2 = self.SM(), self.SM(), self.SM(), self.SM()
            op("dve", "tensor_copy", out=mx[0:nt, 12:13], in_=pt[0:nt, 32:33])
            op("dve", "tensor_scalar", out=lt[0:nt, 0:8], in0=pt[0:nt, 0:8], scalar1=mx[0:nt, 12:13], scalar2=None, op0=ALU.mult)
            op("dve", "max", out=mx[0:nt, 0:8], in_=lt[0:nt, 0:8])
            op("dve", "tensor_tensor", out=mx[0:nt, 8:9], in0=mx[0:nt, 1:2], in1=mx[0:nt, 0:1], op=ALU.subtract)
            op("act", "activation", out=mx[0:nt, 8:9], in_=mx[0:nt, 8:9], func=AF.Exp)
            op("dve", "tensor_scalar", out=mx[0:nt, 9:10], in0=mx[0:nt, 8:9], scalar1=1.0, scalar2=None, op0=ALU.add)
            op("dve", "reciprocal", out=mx[0:nt, 9:10], in_=mx[0:nt, 9:10])
            op("dve", "tensor_tensor", out=mx[0:nt, 10:11], in0=mx[0:nt, 8:9], in1=mx[0:nt, 9:10], op=ALU.mult)
            op("dve", "tensor_scalar", out=g1[0:nt, 0:8], in0=lt[0:nt, 0:8], scalar1=mx[0:nt, 0:1], scalar2=mx[0:nt, 9:10], op0=ALU.is_equal, op1=ALU.mult)
            op("dve", "tensor_scalar", out=g2[0:nt, 0:8], in0=lt[0:nt, 0:8], scalar1=mx[0:nt, 1:2], scalar2=mx[0:nt, 10:11], op0=ALU.is_equal, op1=ALU.mult)
            op("dve", "tensor_tensor", out=g1[0:nt, 0:8], in0=g1[0:nt, 0:8], in1=g2[0:nt, 0:8], op=ALU.add)
            pt2 = self.PSM()
            op("pe", "transpose", out=pt2[0:8, 0:nt], in_=g1[0:nt, 0:8], identity=self.ident[0:nt, 0:nt])
            op("dve", "tensor_copy", out=gfm[0:8, t0:t0 + nt], in_=pt2[0:8, 0:nt])
            t0 += nt
        gkeep = self.gkeep
        op("dve", "tensor_copy", out=gkeep[0:8, 0:n], in_=gfm[0:8, 0:n])
        for e in range(NEXP):
            gm = self.T()
            op("dve", "tensor_scalar", out=gm[0:8, 0:n], in0=gkeep[0:8, 0:n], scalar1=self.ident[0:8, e:e + 1], scalar2=None, op0=ALU.mult)
            pb = self.PSM()
            op("pe", "matmul", out=pb[:, 0:n], lhsT=self.ones8[:, :], rhs=gm[0:8, 0:n], start=True, stop=True)
            gbc = self.gbc
            op("act", "copy", out=gbc[:, 0:n], in_=pb[:, 0:n])
            self.swiglu_dense(n, "od_moe_w_gate", "od_moe_w_up", "od_moe_w_down", 28, 4,
                              row0_gu=e * 1024, row0_d=e * DFE, gate_bc=gbc)


def make_consts():
    ident = np.eye(128, dtype=np.float32)
    ones = np.ones((128, 128), np.float32)
    p = np.arange(128)
    maskpar = np.stack([((p // 16) % 2 == 0), ((p // 16) % 2 == 1), (p >= 96)], axis=1).astype(np.float32)
    s = np.arange(64)
    triu = (s[:, None] <= s[None, :]).astype(np.float32)
    rm = np.ones((128, 528), np.float32)
    rm[:, 0] = 0
    for c in range(16, 528, 64):
        rm[:, c] = 0
    return {"c_ident": ident, "c_onesr": ones, "c_maskpar": maskpar, "c_triu": triu, "c_rmask": rm}


def make_weight_map(inp):
    f = lambda a: np.ascontiguousarray(np.asarray(a, dtype=np.float32))
    m = {}
    m["ev_w_in"] = f(inp["ev_w_in"][0])
    m["ev_gate_a_w"] = f(inp["ev_gate_a_w"][0].reshape(1024, 256))
    m["ev_gate_x_w"] = f(inp["ev_gate_x_w"][0].reshape(1024, 256))
    m["ev_ssm_glu_w"] = f(inp["ev_ssm_glu_w"][0])
    m["ev_w_out"] = f(inp["ev_w_out"][0])
    m["ev_ffn_w_gate"] = f(inp["ev_ffn_w_gate"][0])
    m["ev_ffn_w_up"] = f(inp["ev_ffn_w_up"][0])
    m["ev_ffn_w_down"] = f(inp["ev_ffn_w_down"][0])
    m["od_w_in"] = f(inp["od_w_in"][0])
    m["od_gla_gate_w2"] = f(inp["od_gla_gate_w2"][0])
    m["od_w_out"] = f(inp["od_w_out"][0])
    m["od_moe_w_gate"] = f(inp["od_moe_w_gate"][0].reshape(8 * 1024, 3584))
    m["od_moe_w_up"] = f(inp["od_moe_w_up"][0].reshape(8 * 1024, 3584))
    m["od_moe_w_down"] = f(inp["od_moe_w_down"][0].reshape(8 * 3584, 1024))
    for n in VEC1024:
        m[n] = f(inp[n]).reshape(1024)
    for n in VEC512:
        m[n] = f(inp[n]).reshape(512)
    m["ev_conv_w"] = f(inp["ev_conv_w"][0])
    m["od_router_w"] = f(inp["od_router_w"][0])
    for n in ("ev_ssm_lambda_re", "ev_ssm_lambda_im", "ev_ssm_b_re", "ev_ssm_b_im", "ev_ssm_c_re", "ev_ssm_c_im"):
        m[n] = f(inp[n][0])
    m["ev_ssm_log_dt"] = f(inp["ev_ssm_log_dt"][0])
    m.update(make_consts())
    return m


_CACHE = {}


def kernel(**inputs):
    x = np.asarray(inputs["x"], dtype=np.float32)
    meta = np.asarray(inputs["meta_tokens"], dtype=np.float32)
    nchunks = SEQ // 2 // 64
    half = SEQ // 2
    if "k" not in _CACHE:
        _CACHE["k"] = K(nchunks)
    k = _CACHE["k"]
    wm = make_weight_map(inputs)
    in_maps = []
    npass = NMETA + half
    for c in range(8):
        b, hf = c // 2, c % 2
        m = dict(wm)
        first = np.concatenate([meta, x[b, :half]], axis=0)
        if hf == 0:
            tok = np.concatenate([np.zeros((npass, D), np.float32), first], axis=0)
            flag = np.tile(np.array([[1.0, 0.0]], np.float32), (128, 1))
        else:
            tok = np.concatenate([first, x[b, half - NMETA:half], x[b, half:]], axis=0)
            flag = np.tile(np.array([[0.0, 1.0]], np.float32), (128, 1))
        m["tok"] = np.ascontiguousarray(tok)
        m["c_flag"] = flag
        in_maps.append(m)
    res = run_bass_kernel_spmd(k.nc, in_maps, core_ids=list(range(8)))
    out = np.empty((BATCH, SEQ, D), np.float32)
    for c in range(8):
        b, hf = c // 2, c % 2
        out[b, hf * half:(hf + 1) * half] = res.results[c]["out"]
    return out
```

```python
from contextlib import ExitStack
import math
import numpy as np
import concourse.bass as bass
import concourse.mybir as mybir
from concourse.bass_utils import run_bass_kernel_spmd

F32 = mybir.dt.float32
F32R = mybir.dt.float32r
I32 = mybir.dt.int32
AF = mybir.ActivationFunctionType
ALU = mybir.AluOpType
OUTKEYS = ("out", "accum_out", "ap")

D = 1024
KD = 8
NMETA = 16
SEQ = 8192
BATCH = 4
EPS = 1e-6
NEXP = 8
NSLOT = 5
RELAX_DVE_OWN = True
import os as _os
RELAX_ACT_OWN = True
DFE = 3584
DFF = 3072


class Sem:
    def __init__(self, P, name):
        self.h = P.ctx.enter_context(P.nc.semaphore(name))
        self.val = 0


class V:
    __slots__ = ("buf", "ap")

    def __init__(self, buf, ap):
        self.buf = buf
        self.ap = ap


class Buf:
    def __init__(self, P, name, shape, dtype, space="sbuf", alias=None):
        self.psum = (space == "psum")
        if alias is not None:
            self.t = alias
        elif space == "sbuf":
            self.t = P.ctx.enter_context(P.nc.sbuf_tensor(name, list(shape), dtype))
        else:
            self.t = P.ctx.enter_context(P.nc.psum_tensor(name, list(shape), dtype))
        self.lastw = None
        self.reads = []
        self.small_tk = set()

    @property
    def f(self):
        return _F32View(self)

    def __getitem__(self, idx):
        return V(self, self.t[idx])

    def v(self, ap):
        return V(self, ap)


class _F32View:
    def __init__(self, b):
        self.b = b

    def __getitem__(self, idx):
        return V(self.b, self.b.t[idx].bitcast(F32))


class Eng:
    def __init__(self, P, name):
        self.sem = Sem(P, "s_" + name)
        self.seen = {}
        self.q = []


class Prog:
    def __init__(self, nc):
        self.nc = nc
        self.ctx = ExitStack()
        self.E = {n: Eng(self, n) for n in ("pe", "dve", "act", "pool", "sp")}

    def buf(self, name, shape, dtype=F32, space="sbuf", alias=None):
        return Buf(self, name, shape, dtype, space, alias)

    def _waits(self, E, R, W, skip_own, relax_own=False):
        waits = {}
        own_needed = [False]

        def need(tk, buf=None):
            if tk is None:
                return
            if tk[0] is E.sem and relax_own and buf is not None and tk in buf.small_tk:
                own_needed[0] = True
            if waits.get(tk[0], 0) < tk[1]:
                waits[tk[0]] = tk[1]

        for v in R:
            need(v.buf.lastw, v.buf)
            if v.buf.psum:
                for t in v.buf.reads:
                    if t[0] is not E.sem:
                        need(t)
        for v in W:
            need(v.buf.lastw, v.buf)
            for t in v.buf.reads:
                need(t, v.buf)
        wl = []
        for s, val in waits.items():
            if s is E.sem:
                if skip_own:
                    continue
                if relax_own and not own_needed[0]:
                    continue
            if E.seen.get(s, 0) < val:
                E.seen[s] = val
                wl.append((s, val))
        return wl

    def op(self, eng, meth, *args, writes=(), reads=(), **kw):
        E = self.E[eng]
        W = [v for k, v in kw.items() if isinstance(v, V) and k in OUTKEYS] + list(writes)
        R = [v for k, v in kw.items() if isinstance(v, V) and k not in OUTKEYS] + list(reads)

        def fsize(v):
            n = 1
            for st_, cnt in list(v.ap.ap)[1:]:
                n *= cnt
            return n
        SCAL = ("scalar", "scalar1", "scalar2", "bias", "scale", "initial")
        mainv = [v for k, v in kw.items() if isinstance(v, V) and k not in SCAL] + list(writes) + list(reads)
        small = any(fsize(v) < 128 for v in mainv)
        relax = (eng == "dve" or (eng == "act" and RELAX_ACT_OWN)) and RELAX_DVE_OWN and not small
        wl = self._waits(E, R, W, eng == "pe", relax_own=relax)
        E.sem.val += 1
        tk = (E.sem, E.sem.val)
        kw2 = {k: (v.ap if isinstance(v, V) else v) for k, v in kw.items()}
        E.q.append((meth, args, kw2, wl, (E.sem, 1)))
        for v in R:
            v.buf.reads.append(tk)
            if small:
                v.buf.small_tk.add(tk)
        for v in W:
            v.buf.lastw = tk
            v.buf.reads = []
            v.buf.small_tk = {tk} if small else set()
        return tk

    def dma(self, eng, out, in_, sem, **kw):
        E = self.E[eng]
        W = [out] if isinstance(out, V) else []
        R = [in_] if isinstance(in_, V) else []
        wl = self._waits(E, R, W, False)
        sem.val += 16
        tk = (sem, sem.val)
        kw2 = dict(kw)
        kw2["out"] = out.ap if isinstance(out, V) else out
        kw2["in_"] = in_.ap if isinstance(in_, V) else in_
        E.q.append(("dma_start", (), kw2, wl, (sem, 16)))
        for v in R:
            v.buf.reads.append(tk)
        for v in W:
            v.buf.lastw = tk
            v.buf.reads = []
        return tk

    def wait(self, eng, tk):
        E = self.E[eng]
        if E.seen.get(tk[0], 0) < tk[1]:
            E.seen[tk[0]] = tk[1]
            E.q.append((None, (), {}, [tk], None))

    def build(self):
        nc = self.nc
        with nc.Block() as block:
            def mk(E):
                def body(e):
                    for meth, args, kw, wl, inc in E.q:
                        for s, val in wl:
                            e.wait_ge(s.h, val)
                        if meth is None:
                            continue
                        ins = getattr(e, meth)(*args, **kw)
                        if inc is not None:
                            ins.then_inc(inc[0].h, inc[1])
                return body
            for name, reg in (("pe", block.tensor), ("dve", block.vector), ("act", block.scalar),
                              ("pool", block.gpsimd), ("sp", block.sync)):
                if self.E[name].q:
                    reg(mk(self.E[name]))
        self.ctx.close()


BIGW = {
    "ev_w_in": (1024, 2560), "ev_gate_a_w": (1024, 256), "ev_gate_x_w": (1024, 256),
    "ev_ssm_glu_w": (512, 512), "ev_w_out": (1536, 1024),
    "ev_ffn_w_gate": (1024, 3072), "ev_ffn_w_up": (1024, 3072), "ev_ffn_w_down": (3072, 1024),
    "od_w_in": (1024, 3088), "od_gla_gate_w2": (16, 512), "od_w_out": (1024, 1024),
    "od_moe_w_gate": (8 * 1024, 3584), "od_moe_w_up": (8 * 1024, 3584), "od_moe_w_down": (8 * 3584, 1024),
}
VEC1024 = ["ev_norm_mix", "ev_conv_b", "ev_gate_a_b", "ev_gate_x_b", "ev_lru_lambda", "ev_norm_ffn",
           "od_norm_mix", "od_gla_norm", "od_norm_ffn", "final_norm"]
VEC512 = ["ev_ssm_d", "ev_ssm_glu_b", "od_gla_gate_b"]


def make_blocks(nch):
    first = min(7, nch)
    blocks = [[16] + [64] * first]
    rem = nch - first
    a = rem // 8
    while a >= 0 and (rem - 8 * a) % 7 != 0:
        a -= 1
    if a < 0:
        a, b7 = 0, 0
        blocks += [[64] * 7] * (rem // 7)
        if rem % 7:
            blocks.append([64] * (rem % 7))
        return blocks
    b7 = (rem - 8 * a) // 7
    blocks += [[64] * 8] * a + [[64] * 7] * b7
    return blocks


class K:
    def __init__(self, nchunks, stage_stop=99, dump_h=False):
        self.nchunks = nchunks
        self.stage_stop = stage_stop
        self.dump_h = dump_h
        self.blocks = make_blocks(nchunks)
        self.ntok = 2 * (NMETA + 64 * nchunks)
        self.npass = NMETA + 64 * nchunks
        nc = bass.Bass("TRN2", target_bir_lowering=False)
        nc.dge_precook = False
        self.nc = nc
        self.dr = {}
        dr = self.dr
        dr["tok"] = nc.dram_tensor("tok", [self.ntok, D], F32, kind="ExternalInput").ap()
        import os
        for n, s in BIGW.items():
            if os.environ.get("NOBIGW"):
                continue
            dr[n] = nc.dram_tensor(n, list(s), F32R, kind="ExternalInput").ap()
        for n in VEC1024:
            dr[n] = nc.dram_tensor(n, [1024], F32, kind="ExternalInput").ap()
        for n in VEC512:
            dr[n] = nc.dram_tensor(n, [512], F32, kind="ExternalInput").ap()
        dr["ev_conv_w"] = nc.dram_tensor("ev_conv_w", [4, 1024], F32, kind="ExternalInput").ap()
        dr["od_router_w"] = nc.dram_tensor("od_router_w", [1024, 8], F32, kind="ExternalInput").ap()
        for n in ("ev_ssm_lambda_re", "ev_ssm_lambda_im"):
            dr[n] = nc.dram_tensor(n, [32, 64], F32, kind="ExternalInput").ap()
        dr["ev_ssm_log_dt"] = nc.dram_tensor("ev_ssm_log_dt", [32], F32, kind="ExternalInput").ap()
        for n in ("ev_ssm_b_re", "ev_ssm_b_im"):
            dr[n] = nc.dram_tensor(n, [32, 64, 16], F32, kind="ExternalInput").ap()
        for n in ("ev_ssm_c_re", "ev_ssm_c_im"):
            dr[n] = nc.dram_tensor(n, [32, 16, 64], F32, kind="ExternalInput").ap()
        dr["c_ident"] = nc.dram_tensor("c_ident", [128, 128], F32, kind="ExternalInput").ap()
        dr["c_onesr"] = nc.dram_tensor("c_onesr", [128, 128], F32R, kind="ExternalInput").ap()
        dr["c_maskpar"] = nc.dram_tensor("c_maskpar", [128, 3], F32, kind="ExternalInput").ap()
        dr["c_triu"] = nc.dram_tensor("c_triu", [64, 64], F32, kind="ExternalInput").ap()
        dr["c_rmask"] = nc.dram_tensor("c_rmask", [128, 528], F32, kind="ExternalInput").ap()
        dr["c_flag"] = nc.dram_tensor("c_flag", [128, 2], F32, kind="ExternalInput").ap()
        dr["out"] = nc.dram_tensor("out", [64 * nchunks, D], F32, kind="ExternalOutput").ap()
        self.P = Prog(nc)
        self.alloc()
        self.prologue()
        pos = 0
        for bi, chunks in enumerate(self.blocks):
            self.block(pos, pos, chunks, hist=True, blend=False)
            pos += sum(chunks)
        self.main_start()
        mpos = 0
        for bi, chunks in enumerate(self.blocks):
            self.block(pos, mpos, chunks, hist=False, blend=(bi == 0))
            pos += sum(chunks)
            mpos += sum(chunks)
        for os_ in self.osem:
            self.P.wait("act", (os_, os_.val))
        self.P.build()

    def alloc(self):
        P = self.P
        b = P.buf
        ht = P.ctx.enter_context(P.nc.sbuf_tensor("h", [128, KD, 512], F32))
        self.ht = ht
        hnt = P.ctx.enter_context(P.nc.sbuf_tensor("hn", [128, KD, 512], F32R))
        import os
        NA = 8 if os.environ.get("SMALLA") else 28
        At = P.ctx.enter_context(P.nc.sbuf_tensor("A", [128, NA, 512], F32R))
        self.At = At
        self.h = [b(f"h{k}", None, None, alias=ht[:, k, :]) for k in range(KD)]
        self.hn = [b(f"hn{k}", None, None, alias=hnt[:, k, :]) for k in range(KD)]
        self.A = [b(f"A{k}", None, None, alias=At[:, k % NA, :]) for k in range(28)]
        self.slots = [b(f"slot{i}", [128, 2048], F32R) for i in range(NSLOT)]
        self.slot_sem = [Sem(P, f"slsem{i}") for i in range(NSLOT)]
        self.slot_i = 0
        self.ps = [b(f"ps{i}", [128, 512], F32, "psum") for i in range(8)]
        self.pool = [0, 1, 2, 3, 4, 5]
        self.ps_i = 0
        self.psm_i = 0
        self.tmp = [b(f"tmp{i}", [128, 512], F32) for i in range(6)]
        self.tmp_i = 0
        self.xt = [b(f"xt{i}", [128, 1024], F32) for i in range(2)]
        self.xt_sem = [Sem(P, f"xtsem{i}") for i in range(2)]
        self.osem = [Sem(P, f"osem{i}") for i in range(2)]
        self.psem = Sem(P, "psem")
        self.io_i = 0
        self.vec = {n: b("p_" + n, [128, 8], F32) for n in VEC1024}
        self.vec5 = {n: b("p_" + n, [128, 4], F32) for n in VEC512}
        self.convw = b("convw", [128, 4, 8], F32)
        self.rw = b("rw", [128, 8, 8], F32)
        self.rwg = b("rwg", [128, 8, 8], F32)
        self.ident = b("ident", [128, 128], F32)
        self.onesr = b("onesr", [128, 128], F32R)
        self.maskpar = b("maskpar", [128, 3], F32)
        self.triu = b("triu", [64, 64], F32)
        self.rmask = b("rmask", [128, 528], F32)
        self.w2 = b("w2", [16, 512], F32R)
        self.nc8 = b("nc8", [128, 8], F32)
        self.nc16 = b("nc16", [128, 8], F32)
        self.rec = b("rec", [128, 8, 3 + 512], F32)
        self.lru_state = b("lru_state", [128, 8], F32)
        self.Bc = b("Bc", [128, 4, 2, 128], F32R)
        self.Cc = b("Cc", [128, 16, 2, 64], F32)
        self.Bc3 = b("Bc3", [128, 4, 2, 128], F32R)
        self.lam_pw = b("lam_pw", [128, 16, 10, 3], F32)
        self.s5_state = b("s5_state", [128, 16, 2], F32)
        self.S = [b(f"S{i}", [128, 256], F32) for i in range(4)]
        self.Srab = [b(f"Srab{i}", [128, 256], F32R) for i in range(2)]
        self.S2 = b("Stmp2", [128, 256], F32)
        self.small = [b(f"small{i}", [128, 16], F32) for i in range(8)]
        self.small_i = 0
        self.epsb = b("epsb", [128, 1], F32)
        self.oneb = b("oneb", [128, 1], F32)
        self.ones8 = b("ones8", [8, 128], F32)
        self.vt = b("vt", None, None, alias=self.At[0:64, 12:16, :].rearrange("p a b -> p (a b)").rearrange("p (c v) -> p c v", c=8))
        self.qt = self.A[10]
        self.kt = self.A[11]
        self.att2 = [b(f"att{i}", [64, 64], F32R) for i in range(2)]
        self.ktT = b("ktT", None, None, alias=self.At[0:64, 16:18, :].rearrange("p a b -> p (a b)").rearrange("p (c v) -> p c v", c=8))
        self.gbc = b("gbc", [128, 512], F32)
        self.flag = b("flag", [128, 2], F32)
        self.gkeep = b("gkeep", [8, 512], F32)
        self.prev_n = None
        self.scr_off = 0

    def scratch(self, name, npart, width, dtype=F32):
        regs = [(self.ht[:, :, :].rearrange("p a b -> p (a b)"), 4096),
                (self.rec.t[:, :, :].rearrange("p a b -> p (a b)"), 8 * 515)]
        if not hasattr(self, "scr_offs"):
            self.scr_offs = [0, 0]
        for ri, (flat, cap) in enumerate(regs):
            off = self.scr_offs[ri]
            if off + width <= cap:
                self.scr_offs[ri] = off + width
                ap = flat[0:npart, off:off + width]
                if dtype is not F32:
                    ap = ap.bitcast(dtype)
                bf = self.P.buf(name, None, None, alias=ap)
                self.scr_list.append(bf)
                return bf
        raise AssertionError("scratch full")

    def T(self):
        t = self.tmp[self.tmp_i % len(self.tmp)]
        self.tmp_i += 1
        return t

    def SM(self):
        t = self.small[self.small_i % len(self.small)]
        self.small_i += 1
        return t

    def PS(self):
        t = self.ps[self.pool[self.ps_i % len(self.pool)]]
        self.ps_i += 1
        return t

    def PSM(self):
        t = self.ps[6 + self.psm_i % 2]
        self.psm_i += 1
        return t

    def next_slot(self):
        i = self.slot_i % NSLOT
        self.slot_i += 1
        return self.slots[i], self.slot_sem[i]

    def kslab(self, name, k0, kc, c0, ncols, row0=0):
        assert kc * ncols <= 2048
        w = self.dr[name]
        rows = w[row0 + k0 * 128: row0 + (k0 + kc) * 128, c0:c0 + ncols].rearrange("(k p) c -> p k c", p=128)
        s, sem = self.next_slot()
        sv = s.t[:, 0:kc * ncols].rearrange("p (k c) -> p k c", k=kc)
        self.P.dma("sp", s.v(sv), rows, sem)
        return s, sv

    def prologue(self):
        P, dr = self.P, self.dr
        ps = self.psem
        self.scr_list = []
        loaded = []
        import os
        if os.environ.get("MINPRO"):
            P.dma("sp", self.ident[:, :], dr["c_ident"], ps)
            P.dma("sp", self.onesr[:, :], dr["c_onesr"], ps)
            self.ident.lastw = (ps, ps.val)
            self.onesr.lastw = (ps, ps.val)
            return

        def ld(dst, src, **kw):
            P.dma("sp", dst, src, ps, **kw)
            loaded.append(dst.buf)
        nrow = 8 * len(VEC1024) + 4 * len(VEC512) + 32
        vstage = self.scratch("vstage", nrow, 128)
        r0 = 0
        self.vrows = {}
        for n in VEC1024:
            ld(vstage[r0:r0 + 8, :], dr[n].rearrange("(k p) -> k p", p=128))
            self.vrows[n] = (r0, 8)
            r0 += 8
        for n in VEC512:
            ld(vstage[r0:r0 + 4, :], dr[n].rearrange("(k p) -> k p", p=128))
            self.vrows[n] = (r0, 4)
            r0 += 4
        ld(vstage[r0:r0 + 32, :], dr["ev_conv_w"].rearrange("t (k p) -> (t k) p", p=128))
        self.vrows["ev_conv_w"] = (r0, 32)
        self.vstage = vstage
        ld(self.rw[:, :, :], dr["od_router_w"].rearrange("(k p) e -> p k e", p=128))
        ld(self.ident[:, :], dr["c_ident"])
        ld(self.onesr[:, :], dr["c_onesr"])
        ld(self.maskpar[:, :], dr["c_maskpar"])
        ld(self.triu[:, :], dr["c_triu"])
        ld(self.rmask[:, :], dr["c_rmask"])
        ld(self.w2[:, :], dr["od_gla_gate_w2"])
        ld(self.flag[:, :], dr["c_flag"])
        g = {}
        for n in ("ev_ssm_lambda_re", "ev_ssm_lambda_im"):
            t = self.scratch("g32_" + n, 32, 64)
            ld(t[:, :], dr[n])
            g[n] = t
            t2 = self.scratch("p16_" + n, 16, 128)
            ld(t2[:, :], dr[n].rearrange("(q r) n -> q (r n)", r=2))
            g[n + "_p16"] = t2
        self.g32 = g
        ldt32 = self.scratch("ldt32", 32, 1)
        ld(ldt32[:, :], dr["ev_ssm_log_dt"].rearrange("(g o) -> g o", o=1))
        ldt16 = self.scratch("ldt16", 16, 2)
        ld(ldt16[:, :], dr["ev_ssm_log_dt"].rearrange("(q r) -> q r", r=2))
        bre = self.scratch("bre", 64, 512)
        bim = self.scratch("bim", 64, 512)
        ld(bre.v(bre.t.rearrange("p (g h) -> p g h", g=32)), dr["ev_ssm_b_re"].rearrange("g n h -> n g h"))
        ld(bim.v(bim.t.rearrange("p (g h) -> p g h", g=32)), dr["ev_ssm_b_im"].rearrange("g n h -> n g h"))
        cnat = self.scratch("cnat", 128, 512)
        cv = cnat.t.rearrange("p (c j n) -> p c j n", c=4, j=2)
        ld(cnat.v(cv[:, :, 0, :]), dr["ev_ssm_c_re"].rearrange("(c g) h n -> (g h) c n", c=4))
        ld(cnat.v(cv[:, :, 1, :]), dr["ev_ssm_c_im"].rearrange("(c g) h n -> (g h) c n", c=4))
        fin = (ps, ps.val)
        for bf in loaded:
            bf.lastw = fin

        op = P.op
        op("dve", "memset", ap=self.epsb[:, :], constant=EPS)
        op("dve", "memset", ap=self.oneb[:, :], constant=1.0)
        op("dve", "memset", ap=self.ones8[:, :], constant=1.0)
        op("dve", "memset", ap=self.lru_state[:, :], constant=0.0)
        op("dve", "memset", ap=self.s5_state[:, :, :], constant=0.0)
        for i in range(4):
            op("dve", "memset", ap=self.S[i][:, :], constant=0.0)
        pv = self.PSM()
        nrow = 8 * len(VEC1024) + 4 * len(VEC512) + 32
        op("pe", "transpose", out=pv[:, 0:nrow], in_=self.vstage[:, :], identity=self.ident[0:nrow, 0:nrow])
        for n in VEC1024:
            r0, nr = self.vrows[n]
            op("dve", "tensor_copy", out=self.vec[n][:, :], in_=pv[:, r0:r0 + nr])
        for n in VEC512:
            r0, nr = self.vrows[n]
            op("dve", "tensor_copy", out=self.vec5[n][:, :], in_=pv[:, r0:r0 + nr])
        r0, nr = self.vrows["ev_conv_w"]
        op("dve", "tensor_copy", out=self.convw[:, :, :], in_=pv.v(pv.t[:, r0:r0 + 32].rearrange("p (t k) -> p t k", t=4)))
        gff = self.vec["od_norm_ffn"]
        for k in range(8):
            op("dve", "tensor_scalar", out=self.rwg[:, k, :], in0=self.rw[:, k, :], scalar1=gff[:, k:k + 1], scalar2=None, op0=ALU.mult)
        lam = self.vec["ev_lru_lambda"]
        t1, t2 = self.SM(), self.SM()
        op("act", "activation", out=t1[:, 0:8], in_=lam[:, :], func=AF.Abs)
        op("act", "activation", out=t1[:, 0:8], in_=t1[:, 0:8], func=AF.Exp, scale=-1.0)
        op("act", "activation", out=t1[:, 0:8], in_=t1[:, 0:8], func=AF.Ln, bias=self.oneb[:, 0:1], scale=1.0)
        op("dve", "tensor_scalar", out=t2[:, 0:8], in0=lam[:, :], scalar1=-1.0, scalar2=0.0, op0=ALU.mult, op1=ALU.max)
        op("dve", "tensor_tensor", out=t1[:, 0:8], in0=t1[:, 0:8], in1=t2[:, 0:8], op=ALU.add)
        op("dve", "tensor_scalar", out=self.nc8[:, :], in0=t1[:, 0:8], scalar1=-8.0, scalar2=None, op0=ALU.mult)
        op("dve", "tensor_scalar", out=self.nc16[:, :], in0=t1[:, 0:8], scalar1=-16.0, scalar2=None, op0=ALU.mult)
        import os
        if not os.environ.get('SKIP_S5P'):
            self.s5_prologue(ldt32, ldt16, bre, bim, cnat)
        for bf in self.scr_list:
            for a in self.h + [self.rec]:
                if bf.lastw is not None:
                    a.reads.append(bf.lastw)
                a.reads.extend(bf.reads)
        op("dve", "memset", ap=self.rec[:, :, :], constant=0.0)

    def lbar(self, lr, li, dtcol, npart, width, name):
        P = self.P
        op = P.op
        mk = lambda s: self.scratch(f"{name}_{s}", npart, width)
        dlr, dli, mag, sn, cs, ar, ai = mk("dlr"), mk("dli"), mk("mag"), mk("sn"), mk("cs"), mk("ar"), mk("ai")
        for sl, sc in dtcol:
            op("dve", "tensor_scalar", out=dlr[:, sl], in0=lr[:, sl], scalar1=sc, scalar2=None, op0=ALU.mult)
            op("dve", "tensor_scalar", out=dli[:, sl], in0=li[:, sl], scalar1=sc, scalar2=None, op0=ALU.mult)
        op("act", "activation", out=mag[:, :], in_=dlr[:, :], func=AF.Exp)
        ki = self.scratch(name + "_ki", npart, width, dtype=I32)
        kf = mk("kf")
        op("dve", "tensor_scalar", out=kf[:, :], in0=dli[:, :], scalar1=1.0 / (2 * math.pi), scalar2=None, op0=ALU.mult)
        op("dve", "tensor_copy", out=ki[:, :], in_=kf[:, :])
        op("dve", "tensor_copy", out=kf[:, :], in_=ki[:, :])
        r = mk("r")
        op("dve", "scalar_tensor_tensor", out=r[:, :], in0=kf[:, :], scalar=-2 * math.pi, in1=dli[:, :], op0=ALU.mult, op1=ALU.add)

        def wrap(tag, src, shift):
            y, m = mk("wy" + tag), mk("wm" + tag)
            op("dve", "tensor_scalar", out=y[:, :], in0=src[:, :], scalar1=shift, scalar2=None, op0=ALU.add)
            for _ in range(2):
                op("dve", "tensor_scalar", out=m[:, :], in0=y[:, :], scalar1=math.pi, scalar2=-2 * math.pi, op0=ALU.is_gt, op1=ALU.mult)
                op("dve", "tensor_tensor", out=y[:, :], in0=y[:, :], in1=m[:, :], op=ALU.add)
                op("dve", "tensor_scalar", out=m[:, :], in0=y[:, :], scalar1=-math.pi, scalar2=2 * math.pi, op0=ALU.is_lt, op1=ALU.mult)
                op("dve", "tensor_tensor", out=y[:, :], in0=y[:, :], in1=m[:, :], op=ALU.add)
            op("dve", "tensor_scalar", out=y[:, :], in0=y[:, :], scalar1=math.pi, scalar2=-math.pi, op0=ALU.min, op1=ALU.max)
            return y
        ys = wrap("s", r, 0.0)
        yc = wrap("c", r, math.pi / 2)
        op("act", "activation", out=sn[:, :], in_=ys[:, :], func=AF.Sin)
        op("act", "activation", out=cs[:, :], in_=yc[:, :], func=AF.Sin)
        op("dve", "tensor_tensor", out=ar[:, :], in0=mag[:, :], in1=cs[:, :], op=ALU.mult)
        op("dve", "tensor_tensor", out=ai[:, :], in0=mag[:, :], in1=sn[:, :], op=ALU.mult)
        return ar, ai

    def s5_prologue(self, ldt32, ldt16, bre, bim, cnat):
        P = self.P
        op = P.op
        g = self.g32
        dt32 = self.scratch("dt32", 32, 1)
        dt16 = self.scratch("dt16", 16, 2)
        op("act", "activation", out=dt32[:, :], in_=ldt32[:, :], func=AF.Exp)
        op("act", "activation", out=dt16[:, :], in_=ldt16[:, :], func=AF.Exp)
        lr, li = g["ev_ssm_lambda_re"], g["ev_ssm_lambda_im"]
        ar, ai = self.lbar(lr, li, [(slice(0, 64), dt32[:, 0:1])], 32, 64, "L32")
        mk = lambda s: self.scratch("z32_" + s, 32, 64)
        den, t, nr, zr, zi = mk("den"), mk("t"), mk("nr"), mk("zr"), mk("zi")
        op("dve", "tensor_tensor", out=den[:, :], in0=lr[:, :], in1=lr[:, :], op=ALU.mult)
        op("dve", "tensor_tensor", out=t[:, :], in0=li[:, :], in1=li[:, :], op=ALU.mult)
        op("dve", "tensor_tensor", out=den[:, :], in0=den[:, :], in1=t[:, :], op=ALU.add)
        op("dve", "reciprocal", out=den[:, :], in_=den[:, :])
        op("dve", "tensor_scalar", out=nr[:, :], in0=ar[:, :], scalar1=-1.0, scalar2=None, op0=ALU.add)
        op("dve", "tensor_tensor", out=zr[:, :], in0=nr[:, :], in1=lr[:, :], op=ALU.mult)
        op("dve", "tensor_tensor", out=t[:, :], in0=ai[:, :], in1=li[:, :], op=ALU.mult)
        op("dve", "tensor_tensor", out=zr[:, :], in0=zr[:, :], in1=t[:, :], op=ALU.add)
        op("dve", "tensor_tensor", out=zr[:, :], in0=zr[:, :], in1=den[:, :], op=ALU.mult)
        op("dve", "tensor_tensor", out=zi[:, :], in0=ai[:, :], in1=lr[:, :], op=ALU.mult)
        op("dve", "tensor_tensor", out=t[:, :], in0=nr[:, :], in1=li[:, :], op=ALU.mult)
        op("dve", "tensor_tensor", out=zi[:, :], in0=zi[:, :], in1=t[:, :], op=ALU.subtract)
        op("dve", "tensor_tensor", out=zi[:, :], in0=zi[:, :], in1=den[:, :], op=ALU.mult)
        zT = self.scratch("zT", 64, 96)
        for j, src in enumerate((zr, zi)):
            pt = self.PSM()
            op("pe", "transpose", out=pt[0:64, 0:32], in_=src[:, :], identity=self.ident[0:32, 0:32])
            op("dve", "tensor_copy", out=zT[:, 32 * j:32 * j + 32], in_=pt[0:64, 0:32])
        op("dve", "tensor_scalar", out=zT[:, 64:96], in0=zT[:, 32:64], scalar1=-1.0, scalar2=None, op0=ALU.mult)
        Bre = self.scratch("Bre", 64, 512)
        Bim = self.scratch("Bim", 64, 512)
        tt = self.scratch("tt", 64, 16)
        for gi in range(32):
            gs = slice(16 * gi, 16 * gi + 16)
            op("dve", "tensor_scalar", out=tt[:, :], in0=bre[:, gs], scalar1=zT[:, gi:gi + 1], scalar2=None, op0=ALU.mult)
            op("dve", "scalar_tensor_tensor", out=Bre[:, gs], in0=bim[:, gs], scalar=zT[:, 64 + gi:65 + gi], in1=tt[:, :], op0=ALU.mult, op1=ALU.add)
            op("dve", "tensor_scalar", out=tt[:, :], in0=bim[:, gs], scalar1=zT[:, gi:gi + 1], scalar2=None, op0=ALU.mult)
            op("dve", "scalar_tensor_tensor", out=Bim[:, gs], in0=bre[:, gs], scalar=zT[:, 32 + gi:33 + gi], in1=tt[:, :], op0=ALU.mult, op1=ALU.add)
        for c in range(4):
            for j, src in enumerate((Bre, Bim)):
                pt = self.PSM()
                op("pe", "transpose", out=pt[:, 0:64], in_=src[:, 128 * c:128 * c + 128], identity=self.ident[0:64, 0:64])
                for par in range(2):
                    op("dve", "tensor_scalar", out=self.Bc[:, c, j, 64 * par:64 * par + 64], in0=pt[:, 0:64],
                       scalar1=self.maskpar[:, par:par + 1], scalar2=None, op0=ALU.mult)
        for c in range(4):
            for j in range(2):
                op("dve", "tensor_scalar", out=self.Bc3[:, c, j, :], in0=self.Bc.f[:, c, j, :],
                   scalar1=self.maskpar[:, 2:3], scalar2=None, op0=ALU.mult)
        op("dve", "memset", ap=self.Cc[:, :, :, :], constant=0.0)
        pre2 = self.scratch("pre2", 128, 128)
        for c in range(4):
            for j in range(2):
                for par in range(2):
                    op("dve", "tensor_scalar", out=pre2[:, 64 * par:64 * par + 64], in0=cnat[:, (c * 2 + j) * 64:(c * 2 + j) * 64 + 64],
                       scalar1=self.maskpar[:, par:par + 1], scalar2=(1.0 if j == 0 else -1.0), op0=ALU.mult, op1=ALU.mult)
                pt = self.PSM()
                op("pe", "transpose", out=pt[:, 0:128], in_=pre2[:, :], identity=self.ident[:, :])
                for r in range(4):
                    op("dve", "tensor_copy", out=self.Cc[:, 4 * c + r, j, 32 * (r % 2):32 * (r % 2) + 32],
                       in_=pt[:, 32 * r:32 * r + 32])
        lr16, li16 = g["ev_ssm_lambda_re_p16"], g["ev_ssm_lambda_im_p16"]
        ar16, ai16 = self.lbar(lr16, li16, [(slice(0, 64), dt16[:, 0:1]), (slice(64, 128), dt16[:, 1:2])], 16, 128, "L16")
        lp = self.lam_pw
        for j, src in enumerate((ar16, ai16)):
            pt = self.PSM()
            op("pe", "transpose", out=pt[:, 0:16], in_=src[:, :], identity=self.ident[0:16, 0:16])
            op("dve", "tensor_copy", out=lp[:, :, 0, j], in_=pt[:, 0:16])
        t1 = self.scratch("lp_t1", 128, 16)
        t2 = self.scratch("lp_t2", 128, 16)
        for lv in range(1, 10):
            op("dve", "tensor_tensor", out=t1[:, :], in0=lp[:, :, lv - 1, 0], in1=lp[:, :, lv - 1, 0], op=ALU.mult)
            op("dve", "tensor_tensor", out=t2[:, :], in0=lp[:, :, lv - 1, 1], in1=lp[:, :, lv - 1, 1], op=ALU.mult)
            op("dve", "tensor_tensor", out=lp[:, :, lv, 0], in0=t1[:, :], in1=t2[:, :], op=ALU.subtract)
            op("dve", "tensor_tensor", out=t1[:, :], in0=lp[:, :, lv - 1, 0], in1=lp[:, :, lv - 1, 1], op=ALU.mult)
            op("dve", "tensor_scalar", out=lp[:, :, lv, 1], in0=t1[:, :], scalar1=2.0, scalar2=None, op0=ALU.mult)
        op("dve", "tensor_scalar", out=lp[:, :, :, 2], in0=lp[:, :, :, 1], scalar1=-1.0, scalar2=None, op0=ALU.mult)

    def rmsnorm(self, n, gname, src=None, dst=None, dst_f32=False):
        P = self.P
        op = P.op
        src = src or self.h
        dst = dst or self.hn
        A = self.A
        for k in range(KD):
            op("act", "activation", out=A[20 + k][:, 0:n], in_=src[k][:, 0:n], func=AF.Square)
        pss = self.PSM()
        for k in range(KD):
            op("pe", "matmul", out=pss[:, 0:n], lhsT=self.onesr[:, :], rhs=A[20 + k][:, 0:n], start=(k == 0), stop=(k == KD - 1))
        rstd = self.T()
        op("act", "activation", out=rstd[:, 0:n], in_=pss[:, 0:n], func=AF.Sqrt, scale=1.0 / 1024.0, bias=self.epsb[:, 0:1])
        op("dve", "reciprocal", out=rstd[:, 0:n], in_=rstd[:, 0:n])
        g = self.vec[gname]
        for k in range(KD):
            o = dst[k].f[:, 0:n] if dst_f32 else dst[k][:, 0:n]
            op("dve", "scalar_tensor_tensor", out=o, in0=src[k][:, 0:n], scalar=g[:, k:k + 1],
               in1=rstd[:, 0:n], op0=ALU.mult, op1=ALU.mult)
        return rstd

    def load_tokens(self, pos, n):
        P = self.P
        op = P.op
        import os
        t0 = 0
        nlim = int(os.environ.get('LOAD_TOK', '100000'))
        while t0 < min(n, nlim):
            nt = min(128, n - t0)
            i = self.io_i % 2
            self.io_i += 1
            xt = self.xt[i]
            P.dma("act", xt[0:nt, :], self.dr["tok"][pos + t0:pos + t0 + nt, :], self.xt_sem[i])
            lmode = int(os.environ.get("LOAD_MODE", "0")) if t0 > 0 else 0
            for half in range(2):
                if lmode == 1:
                    continue
                pt = self.PS()
                for j in range(4):
                    k = half * 4 + j
                    op("pe", "transpose", out=pt[:, j * 128:j * 128 + nt], in_=xt[0:nt, k * 128:(k + 1) * 128],
                       identity=self.ident[0:nt, 0:nt])
                if lmode == 2:
                    continue
                for j in range(4):
                    k = half * 4 + j
                    dstv = self.h[k][:, t0:t0 + nt]
                    if lmode == 3:
                        dstv = self.tmp[k % 6][:, 0:nt]
                    if lmode == 4:
                        dstv = self.h[k][:, 0:nt]
                    if lmode == 5 and j % 2 == 1:
                        continue
                    if lmode == 6 and j % 2 == 0:
                        continue
                    use_dve = (half == 0)
                    if use_dve:
                        op("dve", "tensor_copy", out=dstv, in_=pt[:, j * 128:j * 128 + nt])
                    else:
                        op("act", "copy", out=dstv, in_=pt[:, j * 128:j * 128 + nt])
            t0 += nt

    def store_tokens(self, src, pos, n, f32view=False):
        P = self.P
        op = P.op
        import os
        t0 = 0
        nlim = int(os.environ.get('STORE_TOK', '100000'))
        while t0 < min(n, nlim):
            nt = min(128, n - t0)
            i = self.io_i % 2
            self.io_i += 1
            ot = self.xt[i]
            for half in range(2):
                pt = self.PS()
                for j in range(4):
                    k = half * 4 + j
                    sv = src[k].f[:, t0:t0 + nt] if f32view else src[k][:, t0:t0 + nt]
                    op("pe", "transpose", out=pt[0:nt, j * 128:(j + 1) * 128], in_=sv, identity=self.ident[:, :])
                if half == 0:
                    op("dve", "tensor_copy", out=ot[0:nt, 0:512], in_=pt[0:nt, :])
                else:
                    op("act", "copy", out=ot[0:nt, 512:1024], in_=pt[0:nt, :])
            lo = pos + t0
            skip = max(0, NMETA - lo)
            if skip < nt:
                a = skip
                while a < nt:
                    bnd = min(nt, (a // 32 + 1) * 32) if a % 32 else nt
                    P.dma("act", self.dr["out"][lo + a - NMETA: lo + bnd - NMETA, :], ot[a:bnd, :], self.osem[i])
                    a = bnd
            t0 += nt

    def main_start(self):
        op = self.P.op
        g = self.flag[:, 1:2]
        op("dve", "tensor_scalar", out=self.lru_state[:, :], in0=self.lru_state[:, :], scalar1=g, scalar2=None, op0=ALU.mult)
        op("dve", "tensor_scalar", out=self.s5_state[:, :, :], in0=self.s5_state[:, :, :], scalar1=g, scalar2=None, op0=ALU.mult)
        for i in range(4):
            op("dve", "tensor_scalar", out=self.S[i][:, :], in0=self.S[i][:, :], scalar1=g, scalar2=None, op0=ALU.mult)
        pn = self.prev_n
        op("dve", "tensor_scalar", out=self.rec[:, :, pn:pn + 3], in0=self.rec[:, :, pn:pn + 3], scalar1=g, scalar2=None, op0=ALU.mult)

    def block(self, pos, mpos, chunks, hist, blend):
        n = sum(chunks)
        self.load_tokens(pos, n)
        if self.stage_stop >= 1:
            self.layer0_mixer(n, blend)
        if self.stage_stop >= 2:
            self.ffn0(n)
        if self.stage_stop >= 3:
            self.gla(n, chunks, hist, blend)
        if hist:
            return
        if self.stage_stop >= 4:
            self.moe(n)
        if not self.dump_h:
            self.rmsnorm(n, "final_norm", dst=self.h)
        self.store_tokens(self.h, mpos, n)

    def mm_out(self, n, lhs_list, rhs_list, ps=None):
        ps = ps or self.PS()
        nk = len(lhs_list)
        for k in range(nk):
            self.P.op("pe", "matmul", out=ps[:, 0:n], lhsT=lhs_list[k], rhs=rhs_list[k], start=(k == 0), stop=(k == nk - 1))
        return ps

    def layer0_mixer(self, n, blend=False):
        P = self.P
        op = P.op
        A, hn, h = self.A, self.hn, self.h
        self.rmsnorm(n, "ev_norm_mix")
        rec = self.rec
        if self.prev_n is not None:
            op("act", "copy", out=rec[:, :, 0:3], in_=rec[:, :, self.prev_n:self.prev_n + 3])
        self.prev_n = n
        rhs = [hn[k][:, 0:n] for k in range(8)]
        for sl in (8, 9, 0, 1, 2, 3, 4, 5, 6, 7):
            s, sv = self.kslab("ev_w_in", 0, 8, sl * 256, 256)
            for m in range(2):
                mc = sl * 2 + m
                ps = self.mm_out(n, [s.v(sv[:, k, m * 128:(m + 1) * 128]) for k in range(8)], rhs)
                if mc < 8:
                    op("act", "activation", out=A[mc][:, 0:n], in_=ps[:, 0:n], func=AF.Gelu)
                elif mc < 16:
                    op("dve", "tensor_copy", out=rec[:, mc - 8, 3:3 + n], in_=ps[:, 0:n])
                else:
                    op("dve", "tensor_copy", out=A[8 + mc - 16][:, 0:n], in_=ps[:, 0:n])
        cw, cb = self.convw, self.vec["ev_conv_b"]
        for k in range(KD):
            t = self.T()
            op("dve", "tensor_scalar", out=t[:, 0:n], in0=rec[:, k, 0:n], scalar1=cw[:, 0, k:k + 1], scalar2=cb[:, k:k + 1], op0=ALU.mult, op1=ALU.add)
            op("dve", "scalar_tensor_tensor", out=t[:, 0:n], in0=rec[:, k, 1:1 + n], scalar=cw[:, 1, k:k + 1], in1=t[:, 0:n], op0=ALU.mult, op1=ALU.add)
            op("dve", "scalar_tensor_tensor", out=t[:, 0:n], in0=rec[:, k, 2:2 + n], scalar=cw[:, 2, k:k + 1], in1=t[:, 0:n], op0=ALU.mult, op1=ALU.add)
            op("dve", "scalar_tensor_tensor", out=A[12 + k][:, 0:n], in0=rec[:, k, 3:3 + n], scalar=cw[:, 3, k:k + 1], in1=t[:, 0:n], op0=ALU.mult, op1=ALU.add)
        sa, sav = self.kslab("ev_gate_a_w", 0, 8, 0, 256)
        sx, sxv = self.kslab("ev_gate_x_w", 0, 8, 0, 256)
        ba, bx = self.vec["ev_gate_a_b"], self.vec["ev_gate_x_b"]
        for j in range(KD):
            hb = j // 2
            cols = slice((j % 2) * 128, (j % 2) * 128 + 128)
            xin = [A[12 + 2 * hb + kk][:, 0:n] for kk in range(2)]
            psa = self.mm_out(n, [sa.v(sav[:, 2 * hb + kk, cols]) for kk in range(2)], xin)
            psx = self.mm_out(n, [sx.v(sxv[:, 2 * hb + kk, cols]) for kk in range(2)], xin)
            r, a_t, s_t, i_t = self.T(), self.T(), self.T(), self.T()
            op("act", "activation", out=r[:, 0:n], in_=psa[:, 0:n], func=AF.Sigmoid, bias=ba[:, j:j + 1], scale=1.0)
            op("act", "activation", out=i_t[:, 0:n], in_=psx[:, 0:n], func=AF.Sigmoid, bias=bx[:, j:j + 1], scale=1.0)
            op("act", "activation", out=a_t[:, 0:n], in_=r[:, 0:n], func=AF.Exp, scale=self.nc8[:, j:j + 1])
            op("act", "activation", out=s_t[:, 0:n], in_=r[:, 0:n], func=AF.Exp, scale=self.nc16[:, j:j + 1])
            op("act", "activation", out=s_t[:, 0:n], in_=s_t[:, 0:n], func=AF.Sqrt, scale=-1.0, bias=self.oneb[:, 0:1])
            op("dve", "tensor_tensor", out=i_t[:, 0:n], in0=i_t[:, 0:n], in1=A[12 + j].f[:, 0:n], op=ALU.mult)
            op("dve", "tensor_tensor", out=i_t[:, 0:n], in0=i_t[:, 0:n], in1=s_t[:, 0:n], op=ALU.mult)
            ls = self.lru_state[:, j:j + 1]
            if blend:
                op("dve", "tensor_tensor_scan", out=r[:, 0:16], data0=a_t[:, 0:16], data1=i_t[:, 0:16],
                   initial=ls, op0=ALU.mult, op1=ALU.add)
                tb = self.SM()
                op("dve", "tensor_scalar", out=tb[:, 0:1], in0=r[:, 15:16], scalar1=self.flag[:, 0:1], scalar2=None, op0=ALU.mult)
                op("dve", "scalar_tensor_tensor", out=ls, in0=ls, scalar=self.flag[:, 1:2], in1=tb[:, 0:1], op0=ALU.mult, op1=ALU.add)
                op("dve", "tensor_tensor_scan", out=r[:, 16:n], data0=a_t[:, 16:n], data1=i_t[:, 16:n],
                   initial=ls, op0=ALU.mult, op1=ALU.add)
            else:
                op("dve", "tensor_tensor_scan", out=r[:, 0:n], data0=a_t[:, 0:n], data1=i_t[:, 0:n],
                   initial=ls, op0=ALU.mult, op1=ALU.add)
            op("act", "copy", out=self.lru_state[:, j:j + 1], in_=r[:, n - 1:n])
            op("dve", "tensor_tensor", out=A[j][:, 0:n], in0=A[j].f[:, 0:n], in1=r[:, 0:n], op=ALU.mult)
        self.s5(n, blend)
        mixr = [A[k][:, 0:n] for k in range(8)] + [A[20 + k][:, 0:n] for k in range(4)]
        for cg in range(4):
            pss = [self.PS(), self.PS()]
            for part in range(2):
                s, sv = self.kslab("ev_w_out", part * 6, 6, cg * 256, 256)
                for mm in range(2):
                    for k in range(6):
                        kk = part * 6 + k
                        op("pe", "matmul", out=pss[mm][:, 0:n], lhsT=s.v(sv[:, k, mm * 128:(mm + 1) * 128]), rhs=mixr[kk],
                           start=(kk == 0), stop=(kk == 11))
            for mm in range(2):
                m = cg * 2 + mm
                op("dve", "tensor_tensor", out=h[m][:, 0:n], in0=pss[mm][:, 0:n], in1=h[m][:, 0:n], op=ALU.add)

    def s5(self, n, blend=False):
        P = self.P
        op = P.op
        A = self.A
        lp = self.lam_pw
        nlev = 0
        while (1 << nlev) < n:
            nlev += 1
        yps = self.ps[0:4]
        saved_pool = self.pool
        self.pool = [4, 5]
        st = self.s5_state
        for pi in range(16):
            c, r = pi // 4, pi % 4
            prow = slice(32 * r, 32 * r + 32)
            za = [self.T(), self.T()]
            zb = [self.T(), self.T()]
            for j in range(2):
                psx = self.PS()
                if r < 3:
                    op("pe", "matmul", out=psx[:, 0:n], lhsT=self.Bc[prow, c, j, :], rhs=A[8 + c][prow, 0:n], start=True, stop=True)
                else:
                    op("pe", "matmul", out=psx[:, 0:n], lhsT=self.Bc3[64:128, c, j, :], rhs=A[8 + c][64:128, 0:n], start=True, stop=True)
                op("act", "copy", out=za[j][:, 0:n], in_=psx[:, 0:n])
            def seg(c0, c1):
                op("dve", "scalar_tensor_tensor", out=za[0][:, c0:c0 + 1], in0=st[:, pi, 0:1], scalar=lp[:, pi, 0, 0:1], in1=za[0][:, c0:c0 + 1], op0=ALU.mult, op1=ALU.add)
                op("dve", "scalar_tensor_tensor", out=za[0][:, c0:c0 + 1], in0=st[:, pi, 1:2], scalar=lp[:, pi, 0, 2:3], in1=za[0][:, c0:c0 + 1], op0=ALU.mult, op1=ALU.add)
                op("dve", "scalar_tensor_tensor", out=za[1][:, c0:c0 + 1], in0=st[:, pi, 1:2], scalar=lp[:, pi, 0, 0:1], in1=za[1][:, c0:c0 + 1], op0=ALU.mult, op1=ALU.add)
                op("dve", "scalar_tensor_tensor", out=za[1][:, c0:c0 + 1], in0=st[:, pi, 0:1], scalar=lp[:, pi, 0, 1:2], in1=za[1][:, c0:c0 + 1], op0=ALU.mult, op1=ALU.add)
                src, dst = za, zb
                lv = 0
                while (1 << lv) < (c1 - c0):
                    d = 1 << lv
                    pr, pim, npi = lp[:, pi, lv, 0:1], lp[:, pi, lv, 1:2], lp[:, pi, lv, 2:3]
                    op("act", "copy", out=dst[0][:, c0:c0 + d], in_=src[0][:, c0:c0 + d])
                    op("act", "copy", out=dst[1][:, c0:c0 + d], in_=src[1][:, c0:c0 + d])
                    op("dve", "scalar_tensor_tensor", out=dst[0][:, c0 + d:c1], in0=src[0][:, c0:c1 - d], scalar=pr, in1=src[0][:, c0 + d:c1], op0=ALU.mult, op1=ALU.add)
                    op("dve", "scalar_tensor_tensor", out=dst[0][:, c0 + d:c1], in0=src[1][:, c0:c1 - d], scalar=npi, in1=dst[0][:, c0 + d:c1], op0=ALU.mult, op1=ALU.add)
                    op("dve", "scalar_tensor_tensor", out=dst[1][:, c0 + d:c1], in0=src[1][:, c0:c1 - d], scalar=pr, in1=src[1][:, c0 + d:c1], op0=ALU.mult, op1=ALU.add)
                    op("dve", "scalar_tensor_tensor", out=dst[1][:, c0 + d:c1], in0=src[0][:, c0:c1 - d], scalar=pim, in1=dst[1][:, c0 + d:c1], op0=ALU.mult, op1=ALU.add)
                    src, dst = dst, src
                    lv += 1
                return src
            if blend:
                sa = seg(0, 16)
                for j in range(2):
                    tb = self.SM()
                    op("dve", "tensor_scalar", out=tb[:, 0:1], in0=sa[j][:, 15:16], scalar1=self.flag[:, 0:1], scalar2=None, op0=ALU.mult)
                    op("dve", "scalar_tensor_tensor", out=st[:, pi, j:j + 1], in0=st[:, pi, j:j + 1], scalar=self.flag[:, 1:2], in1=tb[:, 0:1], op0=ALU.mult, op1=ALU.add)
                src = seg(16, n)
                if sa is not src:
                    for j in range(2):
                        op("act", "copy", out=src[j][:, 0:16], in_=sa[j][:, 0:16])
            else:
                src = seg(0, n)
            op("act", "copy", out=st[:, pi, 0:1], in_=src[0][:, n - 1:n])
            op("act", "copy", out=st[:, pi, 1:2], in_=src[1][:, n - 1:n])
            orow = slice(64 * (r // 2), 64 * (r // 2) + 64)
            for j in range(2):
                op("pe", "matmul", out=yps[c][orow, 0:n], lhsT=self.Cc[:, pi, j, :], rhs=src[j][:, 0:n],
                   start=(j == 0 and r % 2 == 0), stop=(j == 1 and r % 2 == 1))
        self.pool = saved_pool
        dv = self.vec5["ev_ssm_d"]
        for c in range(4):
            t = self.T()
            op("dve", "scalar_tensor_tensor", out=t[:, 0:n], in0=A[8 + c].f[:, 0:n], scalar=dv[:, c:c + 1], in1=yps[c][:, 0:n], op0=ALU.mult, op1=ALU.add)
            op("act", "activation", out=A[24 + c][:, 0:n], in_=t[:, 0:n], func=AF.Gelu)
        s, sv = self.kslab("ev_ssm_glu_w", 0, 4, 0, 512)
        gb = self.vec5["ev_ssm_glu_b"]
        for m in range(4):
            ps = self.mm_out(n, [s.v(sv[:, k, m * 128:(m + 1) * 128]) for k in range(4)], [A[24 + k][:, 0:n] for k in range(4)])
            t = self.T()
            op("act", "activation", out=t[:, 0:n], in_=ps[:, 0:n], func=AF.Sigmoid, bias=gb[:, m:m + 1], scale=1.0)
            op("dve", "tensor_tensor", out=A[20 + m][:, 0:n], in0=A[24 + m].f[:, 0:n], in1=t[:, 0:n], op=ALU.mult)

    def swiglu_dense(self, n, wg, wu, wd, nf, nparts, row0_gu=0, row0_d=0, gate_bc=None):
        P = self.P
        op = P.op
        A, hn, h = self.A, self.hn, self.h
        rhs = [hn[k][:, 0:n] for k in range(8)]
        for f0 in range(0, nf, 2):
            sg, sgv = self.kslab(wg, 0, 8, f0 * 128, 256, row0=row0_gu)
            su, suv = self.kslab(wu, 0, 8, f0 * 128, 256, row0=row0_gu)
            for m in range(2):
                f = f0 + m
                pg = self.mm_out(n, [sg.v(sgv[:, k, m * 128:(m + 1) * 128]) for k in range(8)], rhs)
                pu = self.mm_out(n, [su.v(suv[:, k, m * 128:(m + 1) * 128]) for k in range(8)], rhs)
                t = self.T()
                op("act", "activation", out=t[:, 0:n], in_=pg[:, 0:n], func=AF.Silu)
                op("dve", "tensor_tensor", out=A[f][:, 0:n], in0=pu[:, 0:n], in1=t[:, 0:n], op=ALU.mult)
        kp = nf // nparts
        for cg in range(4):
            pss = [self.PS(), self.PS()]
            for part in range(nparts):
                s, sv = self.kslab(wd, part * kp, kp, cg * 256, 256, row0=row0_d)
                for mm in range(2):
                    for k in range(kp):
                        f = part * kp + k
                        op("pe", "matmul", out=pss[mm][:, 0:n], lhsT=s.v(sv[:, k, mm * 128:(mm + 1) * 128]), rhs=A[f][:, 0:n],
                           start=(f == 0), stop=(f == nf - 1))
            for mm in range(2):
                m = cg * 2 + mm
                if gate_bc is None:
                    op("dve", "tensor_tensor", out=h[m][:, 0:n], in0=pss[mm][:, 0:n], in1=h[m][:, 0:n], op=ALU.add)
                else:
                    t = self.T()
                    op("dve", "tensor_tensor", out=t[:, 0:n], in0=pss[mm][:, 0:n], in1=gate_bc[:, 0:n], op=ALU.mult)
                    op("dve", "tensor_tensor", out=h[m][:, 0:n], in0=t[:, 0:n], in1=h[m][:, 0:n], op=ALU.add)

    def ffn0(self, n):
        self.rmsnorm(n, "ev_norm_ffn")
        self.swiglu_dense(n, "ev_ffn_w_gate", "ev_ffn_w_up", "ev_ffn_w_down", 24, 3)

    def gla(self, n, chunks, hist=False, blend=False):
        P = self.P
        op = P.op
        A, hn, h = self.A, self.hn, self.h
        self.rmsnorm(n, "od_norm_mix")
        rhs = [hn[k][:, 0:n] for k in range(8)]
        s, sv = self.kslab("od_w_in", 0, 8, 3072, 16)
        psg = self.PSM()
        for k in range(8):
            op("pe", "matmul", out=psg[0:16, 0:n], lhsT=s.v(sv[:, k, :]), rhs=rhs[k], start=(k == 0), stop=(k == 7))
        glr = A[27]
        op("dve", "tensor_copy", out=glr[0:16, 0:n], in_=psg[0:16, 0:n])
        gbv = self.vec5["od_gla_gate_b"]
        hnorm = self.vec["od_gla_norm"]
        rm0 = 0 if chunks[0] == 16 else 16
        saved_pool = self.pool
        for hd in range(4):
            po = [self.ps[(hd % 2) * 2], self.ps[(hd % 2) * 2 + 1]]
            self.pool = [4, 5] + ([2, 3] if hd % 2 == 0 else [0, 1])
            sqk, sem = self.next_slot()
            sqkv = sqk.t[:, 0:2048].rearrange("p (k c) -> p k c", k=8)
            w = self.dr["od_w_in"]
            if not hist:
                P.dma("sp", sqk.v(sqkv[:, :, 0:128]), w[:, hd * 128:hd * 128 + 128].rearrange("(k p) c -> p k c", p=128), sem)
            P.dma("sp", sqk.v(sqkv[:, :, 128:256]), w[:, 512 + hd * 128:512 + hd * 128 + 128].rearrange("(k p) c -> p k c", p=128), sem)
            svv, svvv = self.kslab("od_w_in", 0, 8, 1024 + hd * 256, 256)
            pq = None if hist else self.mm_out(n, [sqk.v(sqkv[:, k, 0:128]) for k in range(8)], rhs)
            pk = self.mm_out(n, [sqk.v(sqkv[:, k, 128:256]) for k in range(8)], rhs)
            pl = self.PS()
            op("pe", "matmul", out=pl[:, 0:n], lhsT=self.w2[:, hd * 128:(hd + 1) * 128], rhs=glr[0:16, 0:n], start=True, stop=True)
            xs, ax, mn, eb = self.T(), self.T(), self.T(), self.T()
            op("dve", "tensor_scalar", out=xs[:, 0:n], in0=pl[:, 0:n], scalar1=gbv[:, hd:hd + 1], scalar2=None, op0=ALU.add)
            op("act", "activation", out=ax[:, 0:n], in_=xs[:, 0:n], func=AF.Abs)
            op("act", "activation", out=ax[:, 0:n], in_=ax[:, 0:n], func=AF.Exp, scale=-1.0)
            op("act", "activation", out=ax[:, 0:n], in_=ax[:, 0:n], func=AF.Ln, bias=self.oneb[:, 0:1], scale=1.0)
            op("dve", "tensor_scalar", out=mn[:, 0:n], in0=xs[:, 0:n], scalar1=0.0, scalar2=None, op0=ALU.min)
            op("dve", "tensor_tensor", out=mn[:, 0:n], in0=mn[:, 0:n], in1=ax[:, 0:n], op=ALU.subtract)
            b16 = xs
            op("dve", "tensor_tensor_scan", out=b16[:, 0:n], data0=self.rmask[:, rm0:rm0 + n], data1=mn[:, 0:n], initial=0.0, op0=ALU.mult, op1=ALU.add)
            enb = ax
            op("act", "activation", out=eb[:, 0:n], in_=b16[:, 0:n], func=AF.Exp, scale=1.0 / 16.0)
            op("act", "activation", out=enb[:, 0:n], in_=b16[:, 0:n], func=AF.Exp, scale=-1.0 / 16.0)
            qt, kt = self.qt, self.kt
            if not hist:
                op("dve", "scalar_tensor_tensor", out=qt[:, 0:n], in0=pq[:, 0:n], scalar=128.0 ** -0.5, in1=eb[:, 0:n], op0=ALU.mult, op1=ALU.mult)
            op("dve", "tensor_tensor", out=kt[:, 0:n], in0=pk[:, 0:n], in1=enb[:, 0:n], op=ALU.mult)
            vt = self.vt
            c0 = 0
            for ci, cs in enumerate(chunks):
                pv = self.PS()
                for k in range(8):
                    op("pe", "matmul", out=pv[0:cs, 0:256], lhsT=hn[k][:, c0:c0 + cs], rhs=svv.v(svvv[:, k, :]), start=(k == 0), stop=(k == 7))
                op("act", "copy", out=vt[0:cs, ci, :], in_=pv[0:cs, 0:256])
                c0 += cs
            S = self.S[hd]
            Srab, ktT, S2 = self.Srab, self.ktT, self.S2
            c0 = 0
            for ci, cs in enumerate(chunks):
                pt = self.PS()
                op("pe", "transpose", out=pt[0:cs, 0:128], in_=kt.f[:, c0:c0 + cs], identity=self.ident[:, :])
                op("act", "copy", out=ktT[0:cs, ci, :], in_=pt[0:cs, 0:128])
                c0 += cs
            cur = 0
            op("dve", "tensor_copy", out=Srab[0][:, :], in_=S[:, :])
            c0 = 0
            for ci, cs in enumerate(chunks):
                cl = slice(c0, c0 + cs)
                last = eb[:, c0 + cs - 1:c0 + cs]
                if not hist:
                    pa = self.PS()
                    op("pe", "matmul", out=pa[0:cs, 0:cs], lhsT=kt[:, cl], rhs=qt[:, cl], start=True, stop=True)
                    att = self.att2[ci % 2]
                    op("dve", "tensor_tensor", out=att[0:cs, 0:cs], in0=pa[0:cs, 0:cs], in1=self.triu[0:cs, 0:cs], op=ALU.mult)
                    for vc in range(2):
                        op("pe", "matmul", out=po[vc][:, cl], lhsT=vt[0:cs, ci, vc * 128:(vc + 1) * 128], rhs=att[0:cs, 0:cs], start=True, stop=False)
                        op("pe", "matmul", out=po[vc][:, cl], lhsT=Srab[cur][:, vc * 128:(vc + 1) * 128], rhs=qt[:, cl], start=False, stop=True)
                pS = self.PS()
                op("pe", "matmul", out=pS[:, 0:256], lhsT=ktT[0:cs, ci, :], rhs=vt[0:cs, ci, :], start=True, stop=True)
                op("dve", "tensor_tensor", out=S2[:, :], in0=pS[:, 0:256], in1=S[:, :], op=ALU.add)
                if blend and ci == 0:
                    op("dve", "tensor_scalar", out=S2[:, :], in0=S2[:, :], scalar1=last, scalar2=self.flag[:, 0:1], op0=ALU.mult, op1=ALU.mult)
                    op("dve", "scalar_tensor_tensor", out=S[:, :], in0=S[:, :], scalar=self.flag[:, 1:2], in1=S2[:, :], op0=ALU.mult, op1=ALU.add)
                else:
                    op("dve", "tensor_scalar", out=S[:, :], in0=S2[:, :], scalar1=last, scalar2=None, op0=ALU.mult)
                cur ^= 1
                op("dve", "tensor_copy", out=Srab[cur][:, :], in_=S[:, :])
                c0 += cs
            if hist:
                continue
            for vc in range(2):
                op("act", "activation", out=A[20 + vc][:, 0:n], in_=po[vc][:, 0:n], func=AF.Square)
            pss = self.PSM()
            for vc in range(2):
                op("pe", "matmul", out=pss[:, 0:n], lhsT=self.onesr[:, :], rhs=A[20 + vc][:, 0:n], start=(vc == 0), stop=(vc == 1))
            rs = self.T()
            op("act", "activation", out=rs[:, 0:n], in_=pss[:, 0:n], func=AF.Sqrt, scale=1.0 / 256.0, bias=self.epsb[:, 0:1])
            op("dve", "reciprocal", out=rs[:, 0:n], in_=rs[:, 0:n])
            sg, sgv = self.kslab("od_w_in", 0, 8, 2048 + hd * 256, 256)
            for vc in range(2):
                m = hd * 2 + vc
                on = self.T()
                op("dve", "scalar_tensor_tensor", out=on[:, 0:n], in0=po[vc][:, 0:n], scalar=hnorm[:, m:m + 1], in1=rs[:, 0:n], op0=ALU.mult, op1=ALU.mult)
                pg = self.mm_out(n, [sg.v(sgv[:, k, vc * 128:(vc + 1) * 128]) for k in range(8)], rhs)
                sgt = self.T()
                op("act", "activation", out=sgt[:, 0:n], in_=pg[:, 0:n], func=AF.Silu)
                op("dve", "tensor_tensor", out=A[m][:, 0:n], in0=on[:, 0:n], in1=sgt[:, 0:n], op=ALU.mult)
        self.pool = saved_pool
        if hist:
            return
        for cg in range(4):
            s, sv = self.kslab("od_w_out", 0, 8, cg * 256, 256)
            for mm in range(2):
                m = cg * 2 + mm
                ps = self.mm_out(n, [s.v(sv[:, k, mm * 128:(mm + 1) * 128]) for k in range(8)], [A[k][:, 0:n] for k in range(8)])
                op("dve", "tensor_tensor", out=h[m][:, 0:n], in0=ps[:, 0:n], in1=h[m][:, 0:n], op=ALU.add)

    def moe(self, n):
        P = self.P
        op = P.op
        hn = self.hn
        rstd = self.rmsnorm(n, "od_norm_ffn")
        pl = self.PSM()
        for k in range(8):
            op("pe", "matmul", out=pl[0:8, 0:n], lhsT=self.rwg[:, k, :], rhs=self.h[k][:, 0:n], start=(k == 0), stop=(k == 7))
        lfm, gfm = self.T(), self.T()
        op("dve", "memset", ap=lfm[0:64, 0:n], constant=0.0)
        op("dve", "tensor_copy", out=lfm[0:8, 0:n], in_=pl[0:8, 0:n])
        op("dve", "tensor_copy", out=lfm[32:33, 0:n], in_=rstd[32:33, 0:n])
        t0 = 0
        while t0 < n:
            nt = min(128, n - t0)
            pt = self.PSM()
            op("pe", "transpose", out=pt[0:nt, 0:33], in_=lfm[0:33, t0:t0 + nt], identity=self.ident[0:33, 0:33])
            lt, mx, g1, g2 = self.SM(), self.SM(), self.SM(), self.SM()
            op("dve", "tensor_copy", out=mx[0:nt, 12:13], in_=pt[0:nt, 32:33])
            op("dve", "tensor_scalar", out=lt[0:nt, 0:8], in0=pt[0:nt, 0:8], scalar1=mx[0:nt, 12:13], scalar2=None, op0=ALU.mult)
            op("dve", "max", out=mx[0:nt, 0:8], in_=lt[0:nt, 0:8])
            op("dve", "tensor_tensor", out=mx[0:nt, 8:9], in0=mx[0:nt, 1:2], in1=mx[0:nt, 0:1], op=ALU.subtract)
            op("act", "activation", out=mx[0:nt, 8:9], in_=mx[0:nt, 8:9], func=AF.Exp)
            op("dve", "tensor_scalar", out=mx[0:nt, 9:10], in0=mx[0:nt, 8:9], scalar1=1.0, scalar2=None, op0=ALU.add)
            op("dve", "reciprocal", out=mx[0:nt, 9:10], in_=mx[0:nt, 9:10])
            op("dve", "tensor_tensor", out=mx[0:nt, 10:11], in0=mx[0:nt, 8:9], in1=mx[0:nt, 9:10], op=ALU.mult)
            op("dve", "tensor_scalar", out=g1[0:nt, 0:8], in0=lt[0:nt, 0:8], scalar1=mx[0:nt, 0:1], scalar2=mx[0:nt, 9:10], op0=ALU.is_equal, op1=ALU.mult)
            op("dve", "tensor_scalar", out=g2[0:nt, 0:8], in0=lt[0:nt, 0:8], scalar1=mx[0:nt, 1:2], scalar2=mx[0:nt, 10:11], op0=ALU.is_equal, op1=ALU.mult)
            op("dve", "tensor_tensor", out=g1[0:nt, 0:8], in0=g1[0:nt, 0:8], in1=g2[0:nt, 0:8], op=ALU.add)
            pt2 = self.PSM()
            op("pe", "transpose", out=pt2[0:8, 0:nt], in_=g1[0:nt, 0:8], identity=self.ident[0:nt, 0:nt])
            op("dve", "tensor_copy", out=gfm[0:8, t0:t0 + nt], in_=pt2[0:8, 0:nt])
            t0 += nt
        gkeep = self.gkeep
        op("dve", "tensor_copy", out=gkeep[0:8, 0:n], in_=gfm[0:8, 0:n])
        for e in range(NEXP):
            gm = self.T()
            op("dve", "tensor_scalar", out=gm[0:8, 0:n], in0=gkeep[0:8, 0:n], scalar1=self.ident[0:8, e:e + 1], scalar2=None, op0=ALU.mult)
            pb = self.PSM()
            op("pe", "matmul", out=pb[:, 0:n], lhsT=self.ones8[:, :], rhs=gm[0:8, 0:n], start=True, stop=True)
            gbc = self.gbc
            op("act", "copy", out=gbc[:, 0:n], in_=pb[:, 0:n])
            self.swiglu_dense(n, "od_moe_w_gate", "od_moe_w_up", "od_moe_w_down", 28, 4,
                              row0_gu=e * 1024, row0_d=e * DFE, gate_bc=gbc)


def make_consts():
    ident = np.eye(128, dtype=np.float32)
    ones = np.ones((128, 128), np.float32)
    p = np.arange(128)
    maskpar = np.stack([((p // 16) % 2 == 0), ((p // 16) % 2 == 1), (p >= 96)], axis=1).astype(np.float32)
    s = np.arange(64)
    triu = (s[:, None] <= s[None, :]).astype(np.float32)
    rm = np.ones((128, 528), np.float32)
    rm[:, 0] = 0
    for c in range(16, 528, 64):
        rm[:, c] = 0
    return {"c_ident": ident, "c_onesr": ones, "c_maskpar": maskpar, "c_triu": triu, "c_rmask": rm}


def make_weight_map(inp):
    f = lambda a: np.ascontiguousarray(np.asarray(a, dtype=np.float32))
    m = {}
    m["ev_w_in"] = f(inp["ev_w_in"][0])
    m["ev_gate_a_w"] = f(inp["ev_gate_a_w"][0].reshape(1024, 256))
    m["ev_gate_x_w"] = f(inp["ev_gate_x_w"][0].reshape(1024, 256))
    m["ev_ssm_glu_w"] = f(inp["ev_ssm_glu_w"][0])
    m["ev_w_out"] = f(inp["ev_w_out"][0])
    m["ev_ffn_w_gate"] = f(inp["ev_ffn_w_gate"][0])
    m["ev_ffn_w_up"] = f(inp["ev_ffn_w_up"][0])
    m["ev_ffn_w_down"] = f(inp["ev_ffn_w_down"][0])
    m["od_w_in"] = f(inp["od_w_in"][0])
    m["od_gla_gate_w2"] = f(inp["od_gla_gate_w2"][0])
    m["od_w_out"] = f(inp["od_w_out"][0])
    m["od_moe_w_gate"] = f(inp["od_moe_w_gate"][0].reshape(8 * 1024, 3584))
    m["od_moe_w_up"] = f(inp["od_moe_w_up"][0].reshape(8 * 1024, 3584))
    m["od_moe_w_down"] = f(inp["od_moe_w_down"][0].reshape(8 * 3584, 1024))
    for n in VEC1024:
        m[n] = f(inp[n]).reshape(1024)
    for n in VEC512:
        m[n] = f(inp[n]).reshape(512)
    m["ev_conv_w"] = f(inp["ev_conv_w"][0])
    m["od_router_w"] = f(inp["od_router_w"][0])
    for n in ("ev_ssm_lambda_re", "ev_ssm_lambda_im", "ev_ssm_b_re", "ev_ssm_b_im", "ev_ssm_c_re", "ev_ssm_c_im"):
        m[n] = f(inp[n][0])
    m["ev_ssm_log_dt"] = f(inp["ev_ssm_log_dt"][0])
    m.update(make_consts())
    return m


_CACHE = {}


def kernel(**inputs):
    x = np.asarray(inputs["x"], dtype=np.float32)
    meta = np.asarray(inputs["meta_tokens"], dtype=np.float32)
    nchunks = SEQ // 2 // 64
    half = SEQ // 2
    if "k" not in _CACHE:
        _CACHE["k"] = K(nchunks)
    k = _CACHE["k"]
    wm = make_weight_map(inputs)
    in_maps = []
    npass = NMETA + half
    for c in range(8):
        b, hf = c // 2, c % 2
        m = dict(wm)
        first = np.concatenate([meta, x[b, :half]], axis=0)
        if hf == 0:
            tok = np.concatenate([np.zeros((npass, D), np.float32), first], axis=0)
            flag = np.tile(np.array([[1.0, 0.0]], np.float32), (128, 1))
        else:
            tok = np.concatenate([first, x[b, half - NMETA:half], x[b, half:]], axis=0)
            flag = np.tile(np.array([[0.0, 1.0]], np.float32), (128, 1))
        m["tok"] = np.ascontiguousarray(tok)
        m["c_flag"] = flag
        in_maps.append(m)
    res = run_bass_kernel_spmd(k.nc, in_maps, core_ids=list(range(8)))
    out = np.empty((BATCH, SEQ, D), np.float32)
    for c in range(8):
        b, hf = c // 2, c % 2
        out[b, hf * half:(hf + 1) * half] = res.results[c]["out"]
    return out
```
